# Optimizing a Trainium2 kernel written in Bass

```python
import jax, jax.numpy as jnp
from jax import lax
import numpy as np

D_MODEL = 1024
BATCH = 4
SEQ = 4096
DEPTH = 2

GRID_W = 64
CTX_LEN = 256

DN_HEADS = 4
DN_DK = 128
DN_DV = 128
DN_WIDTH = DN_HEADS * DN_DV
CONV_K = 5
CHUNK = 64

MLA_HEADS = 8
Q_RANK = 384
KV_RANK = 256
NOPE_DIM = 64
ROPE_DIM = 32
V_DIM = 64
MLA_WIDTH = MLA_HEADS * V_DIM
QK_SCALE = (NOPE_DIM + ROPE_DIM) ** -0.5
ROPE_BASE = 10000.0
Q_BLOCK = 128

F_GROUPS = 4
F_GROUP_DIM = 128
F_WIDTH = F_GROUPS * F_GROUP_DIM

N_BRANCH = 3
BRANCH_WIDTH = 512

D_FF = 2816
N_EXPERTS = 8
TOP_K = 2
EXPERT_FF = 3584

RMS_EPS = 1e-6

IN_NAMES = ("dn_q", "dn_k", "dn_v", "dn_z", "dn_a", "dn_b", "mla_cq", "mla_ckv", "mla_kpe", "fourier", "gates")
IN_SIZES = (DN_WIDTH, DN_WIDTH, DN_WIDTH, DN_WIDTH, 2 * DN_HEADS, 2 * DN_HEADS, Q_RANK, KV_RANK, ROPE_DIM, F_WIDTH, N_BRANCH * D_MODEL)
IN_WIDTH = sum(IN_SIZES)

kernel_name = "hybrid_deltanet_mla_fourier_moe_dit"

F32 = jnp.float32


def rmsnorm(x, g):
    xf = x.astype(F32)
    y = xf * lax.rsqrt(jnp.mean(xf * xf, -1, keepdims=True) + RMS_EPS)
    return (y * g.astype(F32)).astype(x.dtype)


def l2norm(x):
    xf = x.astype(F32)
    return xf * lax.rsqrt(jnp.sum(xf * xf, -1, keepdims=True) + RMS_EPS)


def modulate(h, shift, scale):
    return h * (1 + scale) + shift


def split_cols(u):
    out, off = {}, 0
    for name, size in zip(IN_NAMES, IN_SIZES):
        out[name] = u[..., off:off + size]
        off += size
    return out


def centred_dwconv(x, w):
    k = w.shape[0]
    return lax.conv_general_dilated(x, w[:, None, :].astype(x.dtype), window_strides=(1,),
                                    padding=[(k // 2, k // 2)],
                                    dimension_numbers=("NWC", "WIO", "NWC"),
                                    feature_group_count=x.shape[-1])


def axial_rope_cs(n_tokens):
    rows = n_tokens // GRID_W
    r, col = jnp.meshgrid(jnp.arange(rows, dtype=F32), jnp.arange(GRID_W, dtype=F32), indexing="ij")
    axis_dim = ROPE_DIM // 2
    inv = ROPE_BASE ** (-jnp.arange(0, axis_dim, 2, dtype=F32) / axis_dim)
    ang_r = r.reshape(-1, 1, 1) * inv
    ang_c = col.reshape(-1, 1, 1) * inv
    return (jnp.cos(ang_r), jnp.sin(ang_r), jnp.cos(ang_c), jnp.sin(ang_c))


def rope_half(x, cos, sin):
    x1, x2 = jnp.split(x, 2, -1)
    return jnp.concatenate([x1 * cos - x2 * sin, x2 * cos + x1 * sin], -1)


def axial_rope(x, cs):
    cr, sr, cc, sc = cs
    xr, xcol = jnp.split(x.astype(F32), 2, -1)
    return jnp.concatenate([rope_half(xr, cr, sr), rope_half(xcol, cc, sc)], -1).astype(x.dtype)


def gated_delta_chunked(q, k, v, g, beta, s0, with_output):
    B, T, H, dk = k.shape
    dv = v.shape[-1]
    n = T // CHUNK

    def chunks(t):
        t = t.astype(F32).reshape(B, n, CHUNK, H, *t.shape[3:])
        return jnp.moveaxis(t, (1, 3), (0, 2))

    q, k, v, g, beta = (chunks(t) for t in (q, k, v, g, beta))
    gc = jnp.cumsum(g, -1)
    idx = jnp.arange(CHUNK)
    incl = idx[:, None] >= idx[None, :]
    strict = idx[:, None] > idx[None, :]
    decay = jnp.exp(jnp.where(incl, gc[..., :, None] - gc[..., None, :], -jnp.inf))
    kb = k * beta[..., None]
    a_mat = jnp.where(strict, jnp.einsum("nbhik,nbhjk->nbhij", kb, k) * decay, 0.0)
    eye = jnp.eye(CHUNK, dtype=F32)
    tmat = lax.linalg.triangular_solve(a_mat + eye, jnp.broadcast_to(eye, a_mat.shape),
                                       left_side=True, lower=True, unit_diagonal=True)
    u = tmat @ (v * beta[..., None])
    w = tmat @ (kb * jnp.exp(gc)[..., None])
    g_last = gc[..., -1]
    kd = k * jnp.exp(g_last[..., None] - gc)[..., None]
    gl = jnp.exp(g_last)
    if with_output:
        qs = q * (dk ** -0.5)
        attn = jnp.einsum("nbhik,nbhjk->nbhij", qs, k) * decay
        qg = qs * jnp.exp(gc)[..., None]
        xs = (u, w, kd, gl, qg, attn)
    else:
        xs = (u, w, kd, gl)

    def step(S, xs_i):
        u_i, w_i, kd_i, gl_i = xs_i[:4]
        v_new = u_i - jnp.einsum("bhck,bhkv->bhcv", w_i, S)
        S_next = S * gl_i[..., None, None] + jnp.einsum("bhck,bhcv->bhkv", kd_i, v_new)
        if with_output:
            qg_i, attn_i = xs_i[4:]
            o = jnp.einsum("bhck,bhkv->bhcv", qg_i, S) + jnp.einsum("bhcd,bhdv->bhcv", attn_i, v_new)
            return S_next, o
        return S_next, None

    s_fin, o = lax.scan(step, s0.astype(F32), xs)
    if not with_output:
        return None, s_fin
    o = jnp.moveaxis(o, (0, 2), (1, 3)).reshape(B, T, H, dv)
    return o, s_fin


def dn_inputs(s, conv_w, a_log, dt_bias):
    qkv = jax.nn.silu(centred_dwconv(jnp.concatenate([s["dn_q"], s["dn_k"], s["dn_v"]], -1), conv_w))
    B, T, _ = qkv.shape
    qkv = qkv.reshape(B, T, 3, DN_HEADS, DN_DK)
    q, k, v = l2norm(qkv[:, :, 0]), l2norm(qkv[:, :, 1]), qkv[:, :, 2]
    a = s["dn_a"].astype(F32).reshape(B, T, 2, DN_HEADS)
    b = s["dn_b"].astype(F32).reshape(B, T, 2, DN_HEADS)
    g = -jnp.exp(a_log.astype(F32)) * jax.nn.softplus(a + dt_bias.astype(F32))
    return q, k, v, g, jax.nn.sigmoid(b)


def flip_dir(t, d):
    return jnp.flip(t, 1) if d else t


def dn_out(o, z, gain):
    B, T = z.shape[:2]
    o = o.astype(z.dtype)
    y = rmsnorm(o, gain) * jax.nn.silu(z.reshape(B, T, DN_HEADS, DN_DV))
    return y.reshape(B, T, DN_WIDTH)


def mla_q(s, p, rope_cs):
    B, T, _ = s["mla_cq"].shape
    q = (rmsnorm(s["mla_cq"], p["mla_q_norm"]) @ p["mla_w_qup"]).reshape(B, T, MLA_HEADS, NOPE_DIM + ROPE_DIM)
    q_pe = q[..., NOPE_DIM:]
    if rope_cs is not None:
        q_pe = axial_rope(q_pe, rope_cs)
    return jnp.concatenate([q[..., :NOPE_DIM], q_pe], -1)


def mla_kv(s, p, rope_cs):
    B, T, _ = s["mla_ckv"].shape
    kv = (rmsnorm(s["mla_ckv"], p["mla_kv_norm"]) @ p["mla_w_kvup"]).reshape(B, T, MLA_HEADS, NOPE_DIM + V_DIM)
    k_pe = s["mla_kpe"][:, :, None, :]
    if rope_cs is not None:
        k_pe = axial_rope(k_pe, rope_cs)
    k = jnp.concatenate([kv[..., :NOPE_DIM], jnp.broadcast_to(k_pe, (B, T, MLA_HEADS, ROPE_DIM))], -1)
    return k, kv[..., NOPE_DIM:]


def block_attention(q, k, v):
    B, Tq, H, d = q.shape
    nb = Tq // Q_BLOCK
    qb = jnp.swapaxes(q.reshape(B, nb, Q_BLOCK, H, d), 0, 1)

    def one(qi):
        s = jnp.einsum("bqhd,bkhd->bhqk", qi, k).astype(F32) * QK_SCALE
        pr = jax.nn.softmax(s, -1).astype(v.dtype)
        return jnp.einsum("bhqk,bkhv->bqhv", pr, v)

    o = lax.map(one, qb)
    return jnp.swapaxes(o, 0, 1).reshape(B, Tq, H * v.shape[-1])


def fourier_mix(u):
    B, T, _ = u.shape
    f = jnp.fft.fft2(u.reshape(B, T, F_GROUPS, F_GROUP_DIM).astype(F32), axes=(1, 3), norm="ortho").real
    return f.reshape(B, T, F_WIDTH).astype(u.dtype)


def merge_branches(branches, gates_raw, w_branch, w_out):
    y = jnp.stack(branches, -2)
    yd = jnp.einsum("btnc,ncd->btnd", y, w_branch)
    g = jax.nn.sigmoid(gates_raw.reshape(*gates_raw.shape[:-1], N_BRANCH, D_MODEL))
    return jnp.sum(g * yd, -2) @ w_out


def token_mixer(hx, hc, p, rope_cs, need_ctx):
    sx = split_cols(hx @ p["w_in"])
    sc = split_cols(hc @ p["w_in"])
    dx = dn_inputs(sx, p["dn_conv"], p["dn_a_log"], p["dn_dt_bias"])
    dc = dn_inputs(sc, p["dn_conv"], p["dn_a_log"], p["dn_dt_bias"])
    s0 = jnp.zeros((hx.shape[0], DN_HEADS, DN_DK, DN_DV), F32)
    ox_dirs, oc_dirs = [], []
    for d in range(2):
        o_c, s_c = gated_delta_chunked(flip_dir(dc[0], d), flip_dir(dc[1], d), flip_dir(dc[2], d),
                                       flip_dir(dc[3][:, :, d], d), flip_dir(dc[4][:, :, d], d), s0, need_ctx)
        o_x, _ = gated_delta_chunked(flip_dir(dx[0], d), flip_dir(dx[1], d), flip_dir(dx[2], d),
                                     flip_dir(dx[3][:, :, d], d), flip_dir(dx[4][:, :, d], d), s_c, True)
        ox_dirs.append(flip_dir(o_x, d))
        if need_ctx:
            oc_dirs.append(flip_dir(o_c, d))
    ya_x = dn_out(ox_dirs[0] + ox_dirs[1], sx["dn_z"], p["dn_norm"])
    kx, vx = mla_kv(sx, p, rope_cs)
    kc, vc = mla_kv(sc, p, None)
    yb_x = block_attention(mla_q(sx, p, rope_cs), jnp.concatenate([kx, kc], 1), jnp.concatenate([vx, vc], 1))
    yc_x = fourier_mix(sx["fourier"])
    out_x = merge_branches([ya_x, yb_x, yc_x], sx["gates"], p["w_branch"], p["w_out"])
    if not need_ctx:
        return out_x, None
    ya_c = dn_out(oc_dirs[0] + oc_dirs[1], sc["dn_z"], p["dn_norm"])
    yb_c = block_attention(mla_q(sc, p, None), kc, vc)
    yc_c = fourier_mix(sc["fourier"])
    out_c = merge_branches([ya_c, yb_c, yc_c], sc["gates"], p["w_branch"], p["w_out"])
    return out_x, out_c


def swiglu(h, wg, wu, wd):
    return (jax.nn.silu(h @ wg) * (h @ wu)) @ wd


def moe_swiglu(h, w_router, wg, wu, wd):
    logits = (h @ w_router).astype(F32)
    top_v, top_i = lax.top_k(logits, TOP_K)
    wts = jax.nn.softmax(top_v, -1)
    combine = jnp.sum(jax.nn.one_hot(top_i, N_EXPERTS, dtype=F32) * wts[..., None], -2).astype(h.dtype)
    out = jnp.zeros(h.shape[:-1] + (wd.shape[-1],), h.dtype)
    for e in range(N_EXPERTS):
        out = out + combine[..., e:e + 1] * swiglu(h, wg[e], wu[e], wd[e])
    return out


def setup_inputs(seed: int = 0) -> dict:
    key = jax.random.key(seed)
    keys = iter(jax.random.split(key, 40))

    def nrm(shape, scale):
        return jax.random.normal(next(keys), shape, F32) * scale

    L = DEPTH
    n_dense = (DEPTH + 1) // 2
    n_moe = DEPTH // 2
    D = D_MODEL
    return {
        "x": nrm((BATCH, SEQ, D), 1.0),
        "c": nrm((BATCH, D), 1.0),
        "ctx": nrm((BATCH, CTX_LEN, D), 1.0),
        "c_ctx": nrm((D,), 1.0),
        "w_mod": nrm((L, D, 6 * D), 0.5 * D ** -0.5),
        "b_mod": nrm((L, 6 * D), 0.02),
        "norm_mix": 1.0 + nrm((L, D), 0.02),
        "norm_ffn": 1.0 + nrm((L, D), 0.02),
        "w_in": nrm((L, D, IN_WIDTH), D ** -0.5),
        "dn_conv": nrm((L, CONV_K, 3 * DN_WIDTH), CONV_K ** -0.5),
        "dn_a_log": jnp.log(jax.random.uniform(next(keys), (L, 2, DN_HEADS), F32, 1.0, 16.0)),
        "dn_dt_bias": 1.0 + nrm((L, 2, DN_HEADS), 0.1),
        "dn_norm": 1.0 + nrm((L, DN_DV), 0.02),
        "mla_q_norm": 1.0 + nrm((L, Q_RANK), 0.02),
        "mla_kv_norm": 1.0 + nrm((L, KV_RANK), 0.02),
        "mla_w_qup": nrm((L, Q_RANK, MLA_HEADS * (NOPE_DIM + ROPE_DIM)), Q_RANK ** -0.5),
        "mla_w_kvup": nrm((L, KV_RANK, MLA_HEADS * (NOPE_DIM + V_DIM)), KV_RANK ** -0.5),
        "w_branch": nrm((L, N_BRANCH, BRANCH_WIDTH, D), BRANCH_WIDTH ** -0.5),
        "w_out": nrm((L, D, D), D ** -0.5),
        "ffn_w_gate": nrm((n_dense, D, D_FF), D ** -0.5),
        "ffn_w_up": nrm((n_dense, D, D_FF), D ** -0.5),
        "ffn_w_down": nrm((n_dense, D_FF, D), D_FF ** -0.5),
        "moe_router": nrm((n_moe, D, N_EXPERTS), D ** -0.5),
        "moe_w_gate": nrm((n_moe, N_EXPERTS, D, EXPERT_FF), D ** -0.5),
        "moe_w_up": nrm((n_moe, N_EXPERTS, D, EXPERT_FF), D ** -0.5),
        "moe_w_down": nrm((n_moe, N_EXPERTS, EXPERT_FF, D), EXPERT_FF ** -0.5),
        "final_norm": 1.0 + nrm((D,), 0.02),
    }


def reference(x, c, ctx, c_ctx, w_mod, b_mod, norm_mix, norm_ffn, w_in, dn_conv, dn_a_log, dn_dt_bias,
              dn_norm, mla_q_norm, mla_kv_norm, mla_w_qup, mla_w_kvup, w_branch, w_out,
              ffn_w_gate, ffn_w_up, ffn_w_down, moe_router, moe_w_gate, moe_w_up, moe_w_down, final_norm):
    rope_cs = axial_rope_cs(x.shape[1])
    xc = ctx
    c_act = jax.nn.silu(c)
    cc_act = jax.nn.silu(c_ctx)
    for l in range(DEPTH):
        last = l == DEPTH - 1
        mod_x = jnp.split((c_act @ w_mod[l] + b_mod[l])[:, None, :], 6, -1)
        mod_c = jnp.split(cc_act @ w_mod[l] + b_mod[l], 6, -1)
        p = {
            "w_in": w_in[l], "dn_conv": dn_conv[l], "dn_a_log": dn_a_log[l], "dn_dt_bias": dn_dt_bias[l],
            "dn_norm": dn_norm[l], "mla_q_norm": mla_q_norm[l], "mla_kv_norm": mla_kv_norm[l],
            "mla_w_qup": mla_w_qup[l], "mla_w_kvup": mla_w_kvup[l], "w_branch": w_branch[l], "w_out": w_out[l],
        }
        hx = modulate(rmsnorm(x, norm_mix[l]), mod_x[0], mod_x[1])
        hc = modulate(rmsnorm(xc, norm_mix[l]), mod_c[0], mod_c[1])
        mix_x, mix_c = token_mixer(hx, hc, p, rope_cs, not last)
        x = x + mod_x[2] * mix_x
        if l % 2 == 0:
            e = l // 2
            wts = (ffn_w_gate[e], ffn_w_up[e], ffn_w_down[e])
            channel_mixer = swiglu
        else:
            e = l // 2
            wts = (moe_router[e], moe_w_gate[e], moe_w_up[e], moe_w_down[e])
            channel_mixer = moe_swiglu
        hx = modulate(rmsnorm(x, norm_ffn[l]), mod_x[3], mod_x[4])
        x = x + mod_x[5] * channel_mixer(hx, *wts)
        if not last:
            xc = xc + mod_c[2] * mix_c
            hc = modulate(rmsnorm(xc, norm_ffn[l]), mod_c[3], mod_c[4])
            xc = xc + mod_c[5] * channel_mixer(hc, *wts)
    return rmsnorm(x, final_norm)
```

```python
from contextlib import ExitStack, contextmanager
import numpy as np
import ml_dtypes
import concourse.bass as bass
import concourse.mybir as mybir
from concourse.bass_utils import run_bass_kernel_spmd

F32 = mybir.dt.float32
BF16 = mybir.dt.bfloat16
ALU = mybir.AluOpType
AF = mybir.ActivationFunctionType

D = 1024
SEQ = 4096
CTX = 256
TALL = SEQ + CTX
NT = TALL // 128
NCT = CTX // 128
DEPTH = 2
INW = 6320
O_Q, O_K, O_V, O_Z, O_A, O_B, O_CQ, O_CKV, O_KPE, O_F, O_G = 0, 512, 1024, 1536, 2048, 2056, 2064, 2448, 2704, 2736, 3248
DFF = 2816
NEXP = 8
EFF = 3584
EPS = 1e-6
NDSEM = 96
LOCT = 16
LOC = LOCT * 128


class Obj:
    __slots__ = ("name", "h", "w", "r", "ds", "is_sb")

    def __init__(self, name, h, is_sb):
        self.name = name
        self.h = h
        self.w = []
        self.r = []
        self.ds = None
        self.is_sb = is_sb

    def __getitem__(self, idx):
        return self.h[idx]


class Scope:
    def __init__(self, S):
        self.S = S
        self.stack = ExitStack()
        self.objs = []

    def sb(self, name, shape, dt):
        S = self.S
        S.uid += 1
        o = Obj(name, self.stack.enter_context(S.nc.sbuf_tensor("%s_%d" % (name, S.uid), list(shape), dt)), True)
        self.objs.append(o)
        return o


class Sched:
    def __init__(self, nc, stack):
        self.nc = nc
        self.E = {}
        for nm, h in (("pe", nc.tensor), ("act", nc.scalar), ("dve", nc.vector), ("pool", nc.gpsimd), ("sp", nc.sync)):
            sem = stack.enter_context(nc.semaphore("sem_" + nm))
            self.E[nm] = dict(h=h, sem=sem, cnt=0, seen={})
        self.dpool = [dict(sem=stack.enter_context(nc.semaphore("dsem%d" % i)), cnt=0, free=True) for i in range(NDSEM)]
        self.ninst = 0
        self.uid = 0
        self.stack = stack

    def ps(self, name, shape, dt=F32):
        return Obj(name, self.stack.enter_context(self.nc.psum_tensor(name, list(shape), dt)), True)

    def dram(self, name, shape, dt, kind="Internal"):
        t = self.nc.dram_tensor(name, list(shape), dt, kind=kind)
        return Obj(name, t.ap(), False)

    @contextmanager
    def scope(self):
        sc = Scope(self)
        try:
            yield sc
        finally:
            self.barrier()
            for o in sc.objs:
                if o.ds is not None:
                    self.dpool[o.ds]["free"] = True
                    o.ds = None
            sc.stack.close()

    def _wait(self, e, tok):
        sem, val = tok
        k = id(sem)
        pe = self.E["pe"]
        if sem is pe["sem"] and val > pe["cnt"]:
            pe["h"].nop().then_inc(pe["sem"], 1)
            pe["cnt"] += 1
            self.ninst += 1
            self.nforced = getattr(self, "nforced", 0) + 1
        E = self.E[e]
        if E["seen"].get(k, 0) >= val:
            return
        E["seen"][k] = val
        E["h"].wait_ge(sem, val)
        self.ninst += 1

    def barrier(self):
        toks = [(E["sem"], E["cnt"]) for E in self.E.values() if E["cnt"] > 0]
        toks += [(d["sem"], d["cnt"]) for d in self.dpool if d["cnt"] > 0]
        for e in self.E:
            for tok in toks:
                self._wait(e, tok)

    def _deps(self, e, ins, outs):
        mysem = id(self.E[e]["sem"])
        for o in ins:
            if not o.is_sb:
                continue
            for tok in o.w:
                if e == "pe" and id(tok[0]) == mysem:
                    continue
                self._wait(e, tok)
        for o in outs:
            if not o.is_sb:
                continue
            for tok in o.w:
                if e == "pe" and id(tok[0]) == mysem:
                    continue
                self._wait(e, tok)
            for tok in o.r:
                if id(tok[0]) == mysem:
                    continue
                self._wait(e, tok)

    def _prune(self, toks):
        best = {}
        for s, v in toks:
            k = id(s)
            if k not in best or best[k][1] < v:
                best[k] = (s, v)
        return list(best.values())

    def op(self, e, fn, outs=(), ins=(), inc=True):
        E = self.E[e]
        self._deps(e, ins, outs)
        inst = fn(E["h"])
        self.ninst += 1
        tok = (E["sem"], E["cnt"] + 1)
        if inc:
            inst.then_inc(E["sem"], 1)
            E["cnt"] += 1
        for o in ins:
            if o.is_sb:
                o.r.append(tok)
                if len(o.r) > 16:
                    o.r = self._prune(o.r)
        for o in outs:
            if o.is_sb:
                o.w = [tok]
                o.r = []
        return inst

    def dma(self, q, out_obj, out_ap, in_obj, in_ap):
        E = self.E[q]
        sbo = out_obj if out_obj.is_sb else in_obj
        assert sbo.is_sb
        self._deps(q, [in_obj], [out_obj])
        if sbo.ds is None:
            for i, d in enumerate(self.dpool):
                if d["free"]:
                    d["free"] = False
                    sbo.ds = i
                    break
            else:
                raise RuntimeError("out of dma semaphores")
        d = self.dpool[sbo.ds]
        if d["cnt"]:
            self._wait(q, (d["sem"], d["cnt"]))
        inst = E["h"].dma_start(out=out_ap, in_=in_ap)
        d["cnt"] += 16
        inst.then_inc(d["sem"], 16)
        self.ninst += 1
        tok = (d["sem"], d["cnt"])
        if in_obj.is_sb:
            in_obj.r.append(tok)
            if len(in_obj.r) > 16:
                in_obj.r = self._prune(in_obj.r)
        if out_obj.is_sb:
            out_obj.w = [tok]
            out_obj.r = []
        return inst


def host_consts(flip):
    C = {}
    i = np.arange(128)
    m, j = np.meshgrid(i, i, indexing="ij")
    NEG = -30000.0
    C["ident_f"] = np.eye(128)
    C["ULE"] = (m <= j)
    C["LGE"] = (m >= j)
    C["SGT"] = (m > j)
    C["SLT"] = (m < j)
    C["NEGF"] = np.where(j <= m, 0.0, NEG)
    C["NEGB"] = np.where(j >= m, 0.0, NEG)
    C["OFFD"] = -(1.0 - np.eye(128))
    cm = np.concatenate([np.asarray(C[k], np.float32) for k in ("ident_f", "ULE", "LGE", "SGT", "SLT", "NEGF", "NEGB", "OFFD")], 1)
    out = {"cmask": np.ascontiguousarray(cm)}
    out["ident_b"] = np.eye(128).astype(ml_dtypes.bfloat16)
    ang = 2 * np.pi * np.outer(i, i) / 128.0
    out["dft_c"] = (np.concatenate([np.cos(ang), np.sin(ang)], 1) / np.sqrt(128.0)).astype(ml_dtypes.bfloat16)
    for nm, T in (("x", SEQ), ("c", CTX)):
        t = np.arange(T)
        if flip:
            t = t[::-1]
        ph = ((np.outer(t, t) % T).astype(np.float64) * (2 * np.pi / T)).astype(np.float32)
        for tag in ("C", "S"):
            M = (np.cos(ph) if tag == "C" else -np.sin(ph)) / np.float32(np.sqrt(T))
            KC = 512 if nm == "x" else 256
            M = M.reshape(T // 128, 128, T // KC, KC).transpose(2, 1, 0, 3)
            out["dft%s_%s" % (tag, nm)] = np.ascontiguousarray(M).astype(ml_dtypes.bfloat16)
        del ph
    inv = 10000.0 ** (-np.arange(0, 16, 2, dtype=np.float32) / 16)
    pos = np.arange(SEQ)
    if flip:
        pos = pos[::-1]
    ar = np.outer(inv, (pos // 64).astype(np.float32))
    ac = np.outer(inv, (pos % 64).astype(np.float32))
    cosx = np.concatenate([np.cos(ar), np.cos(ar), np.cos(ac), np.cos(ac)], 0)
    sinx = np.concatenate([-np.sin(ar), np.sin(ar), -np.sin(ac), np.sin(ac)], 0)
    cos = np.concatenate([np.ones((32, CTX)), cosx], 1)
    sin = np.concatenate([np.zeros((32, CTX)), sinx], 1)
    out["rope_cs"] = np.stack([cos, sin], 0).astype(np.float32)
    return out


M_ID, M_ULE, M_LGE, M_SGT, M_SLT, M_NEGF, M_NEGB, M_OFFD = range(8)


class Builder:
    def __init__(self, stop_after=None, dbg=()):
        self.stop_after = stop_after
        self.dbg = set(dbg)
        self.nc = bass.Bass("TRN2", target_bir_lowering=False)
        self.in_names = []
        self.out_names = ["y_out"]

    def inp(self, name, shape, dt=F32):
        self.in_names.append(name)
        return self.S.dram(name, shape, dt, kind="ExternalInput")

    def scratch(self, name, shape, dt):
        if name in self.dbg:
            self.out_names.append(name)
            return self.S.dram(name, shape, dt, kind="ExternalOutput")
        return self.S.dram(name, shape, dt, kind="Internal")

    def build(self):
        with ExitStack() as st:
            self.S = Sched(self.nc, st)
            self._build()
        return self.nc

    def mk(self, i):
        return self.masks[:, i * 128:(i + 1) * 128]

    def nps(self, exclude=None):
        while True:
            p = self.PS[self.ps_i % len(self.PS)]
            self.ps_i += 1
            if p is not exclude:
                return p

    def npb(self):
        p = self.PB[self.pb_i % len(self.PB)]
        self.pb_i += 1
        return p

    def mm(self, ps, out_ap, a, lhsT, b, rhs, start=True, stop=True, inc=None):
        self.S.op("pe", lambda e: e.matmul(out_ap, lhsT, rhs, start=start, stop=stop), outs=[ps], ins=[a, b], inc=stop if inc is None else inc)

    def tr(self, ps, out_ap, a, in_ap, ident):
        self.S.op("pe", lambda e: e.transpose(out_ap, in_ap, ident), outs=[ps], ins=[a, self.identb, self.masks])

    def act(self, out_o, out_ap, in_o, in_ap, func, ins=(), **kw):
        self.S.op("act", lambda e: e.activation(out_ap, in_ap, func, **kw), outs=[out_o], ins=[in_o] + list(ins))

    def tt(self, eng, out_o, out_ap, a, a_ap, b, b_ap, op):
        self.S.op(eng, lambda e: e.tensor_tensor(out_ap, a_ap, b_ap, op), outs=[out_o], ins=[a, b])

    def ts(self, eng, out_o, out_ap, a, a_ap, s1, s2, op0, op1=None, ins=()):
        if op1 is None:
            self.S.op(eng, lambda e: e.tensor_scalar(out_ap, a_ap, s1, None, op0), outs=[out_o], ins=[a] + list(ins))
        else:
            self.S.op(eng, lambda e: e.tensor_scalar(out_ap, a_ap, s1, s2, op0, op1), outs=[out_o], ins=[a] + list(ins))

    def stt(self, out_o, out_ap, a, a_ap, scalar, b, b_ap, op0, op1, ins=()):
        self.S.op("dve", lambda e: e.scalar_tensor_tensor(out_ap, a_ap, scalar, b_ap, op0, op1), outs=[out_o], ins=[a, b] + list(ins))

    def cp(self, eng, out_o, out_ap, in_o, in_ap):
        if eng == "act":
            self.S.op("act", lambda e: e.copy(out_ap, in_ap), outs=[out_o], ins=[in_o])
        else:
            self.S.op(eng, lambda e: e.tensor_copy(out_ap, in_ap), outs=[out_o], ins=[in_o])

    def recip(self, out_o, out_ap, in_o, in_ap):
        self.S.op("dve", lambda e: e.reciprocal(out_ap, in_ap), outs=[out_o], ins=[in_o])

    def ld(self, out_o, out_ap, in_o, in_ap, q="sp"):
        self.S.dma(q, out_o, out_ap, in_o, in_ap)

    def run_pipe(self, items, fn, max_active=None):
        import os
        if os.environ.get("NOPIPE"):
            for it in items:
                for _ in fn(it):
                    pass
            return
        active = []
        items = list(items)
        k = 0
        while k < len(items) or active:
            if k < len(items) and (max_active is None or len(active) < max_active):
                active.append(fn(items[k]))
                k += 1
            for g in list(active):
                try:
                    next(g)
                except StopIteration:
                    active.remove(g)

    def stop(self, l, name):
        return self.stop_after is not None and self.stop_after == (l, name)

    def _build(self):
        S = self.S
        I = self.inp
        self.x_in = I("x_in", [SEQ, D])
        self.ctx_in = I("ctx_in", [CTX, D])
        self.cT = I("cT", [128, 16])
        self.w_mod = I("w_mod", [DEPTH, D, 6 * D])
        self.b_mod = I("b_mod", [DEPTH, 6 * D])
        self.norm_mix = I("norm_mix", [DEPTH, D])
        self.norm_ffn = I("norm_ffn", [DEPTH, D])
        self.w_in = I("w_in", [DEPTH, D, INW])
        self.dn_convT = I("dn_convT", [DEPTH, 1536, 5])
        self.dn_a_log = I("dn_a_log", [DEPTH, 8])
        self.dn_dt_bias = I("dn_dt_bias", [DEPTH, 8])
        self.dn_norm = I("dn_norm", [DEPTH, 128])
        self.lat_gain = I("lat_gain", [DEPTH, 128, 5])
        self.mla_w_qup = I("mla_w_qup", [DEPTH, 384, 768])
        self.mla_w_kvup = I("mla_w_kvup", [DEPTH, 256, 1024])
        self.w_branch = I("w_branch", [DEPTH, 3, 512, D])
        self.w_out = I("w_out", [DEPTH, D, D])
        self.ffn_w_gate = I("ffn_w_gate", [1, D, DFF])
        self.ffn_w_up = I("ffn_w_up", [1, D, DFF])
        self.ffn_w_down = I("ffn_w_down", [1, DFF, D])
        self.moe_router = I("moe_router", [1, D, NEXP])
        self.moe_w_gate = I("moe_w_gate", [1, NEXP, D, EFF])
        self.moe_w_up = I("moe_w_up", [1, NEXP, D, EFF])
        self.moe_w_down = I("moe_w_down", [1, NEXP, EFF, D])
        self.final_norm = I("final_norm", [1, D])
        self.c_mask = I("cmask", [128, 8 * 128])
        self.c_identb = I("ident_b", [128, 128], BF16)
        self.c_dftc = I("dft_c", [128, 256], BF16)
        self.c_dft = {("C", "x"): I("dftC_x", [8, 128, 32, 512], BF16), ("S", "x"): I("dftS_x", [8, 128, 32, 512], BF16),
                      ("C", "c"): I("dftC_c", [1, 128, 2, 256], BF16), ("S", "c"): I("dftS_c", [1, 128, 2, 256], BF16)}
        self.c_rope = I("rope_cs", [2, 32, TALL])
        self.y_out = S.dram("y_out", [LOC, D], F32, kind="ExternalOutput")
        sc = self.scratch
        self.xres = sc("xres", [TALL, D], F32)
        self.mod_d = sc("mod_d", [2, 6 * D], F32)
        self.qT_d = sc("qT_d", [128, NT, 4, 128], BF16)
        self.kT_d = sc("kT_d", [128, NT, 4, 128], BF16)
        self.ktm_d = sc("ktm_d", [NT, 128, 4, 128], BF16)
        self.vtm_d = sc("vtm_d", [NT, 128, 4, 128], BF16)
        self.z_d = sc("z_d", [TALL, 512], BF16)
        self.lat_d = sc("lat_d", [5, 128, TALL], BF16)
        self.kpe_d = sc("kpe_d", [32, TALL], BF16)
        self.uT_d = sc("uT_d", [4, 128, TALL], BF16)
        self.gT_d = sc("gT_d", [24, 128, TALL], BF16)
        self.o_d = [sc("of_d", [TALL, 512], F32), sc("ob_d", [TALL, 512], F32)]
        self.yT_d = sc("yT_d", [3, 4, 128, TALL], BF16)
        self.hx_d = sc("hx_d", [TALL, D], BF16) if "hx_d" in self.dbg else None
        self.gb_d = sc("gb_d", [128, NT * 16], F32) if "gb_d" in self.dbg else None

        with S.scope() as G:
            self.masks = G.sb("masks", [128, 8 * 128], F32)
            self.ld(self.masks, self.masks[:], self.c_mask, self.c_mask[:])
            self.identb = G.sb("identb", [128, 128], BF16)
            self.ld(self.identb, self.identb[:], self.c_identb, self.c_identb[:])
            self.ones_b = G.sb("ones_b", [128, 128], BF16)
            S.op("dve", lambda e: e.memset(self.ones_b[:], 1.0), outs=[self.ones_b])
            self.ones_f = G.sb("ones_f", [128, 128], F32)
            S.op("dve", lambda e: e.memset(self.ones_f[:], 1.0), outs=[self.ones_f])
            self.cact = G.sb("cact", [128, 16], F32)
            self.ld(self.cact, self.cact[:], self.cT, self.cT[:])
            self.act(self.cact, self.cact[:], self.cact, self.cact[:], AF.Silu)
            self.gb = G.sb("gb", [128, NT, 16], F32)
            self.PS = [S.ps("psum%d" % i, [128, 1024]) for i in range(3)]
            self.PB = [S.ps("psumb%d" % i, [128, 1024], BF16) for i in range(2)]
            self.ps_i = 0
            self.pb_i = 0
            for l in range(DEPTH):
                if self.layer(l):
                    break

    def xsrc(self, l, t):
        if l == 0:
            if t < NCT:
                return self.ctx_in, self.ctx_in[t * 128:(t + 1) * 128, :]
            return self.x_in, self.x_in[(t - NCT) * 128:(t - NCT + 1) * 128, :]
        return self.xres, self.xres[t * 128:(t + 1) * 128, :]

    def layer(self, l):
        ns = self.nc.named_scope
        last = l == DEPTH - 1
        need_ctx = not last
        with ns("L%d_mod" % l):
            self.phase_mod(l)
        if self.stop(l, "mod"):
            return True
        with self.S.scope() as P:
            self.hT = P.sb("hT", [128, 8, TALL], BF16)
            with ns("L%d_norm" % l):
                self.phase_norm(l, "mix", list(range(NT)), self.hT, 0)
            with ns("L%d_qkv" % l):
                self.phase_qkv(l)
            with ns("L%d_zab" % l):
                self.phase_zab(l)
            with ns("L%d_lat" % l):
                self.phase_lat(l)
            with ns("L%d_fg" % l):
                self.phase_fg(l)
            if self.stop(l, "proj"):
                return True
        with ns("L%d_dn" % l):
            self.phase_dn(l, need_ctx)
        if self.stop(l, "dn"):
            return True
        with ns("L%d_dnout" % l):
            self.phase_dnout(l, need_ctx)
        with ns("L%d_mla" % l):
            self.phase_mla(l, need_ctx)
        with ns("L%d_fft" % l):
            self.phase_fft(l, need_ctx)
        if self.stop(l, "mix"):
            return True
        with ns("L%d_merge" % l):
            self.phase_merge(l, need_ctx)
        if self.stop(l, "merge"):
            return True
        with ns("L%d_ffn" % l):
            self.phase_ffn(l)
        return False

    def out_tiles(self, l, need_ctx):
        if l == DEPTH - 1:
            return list(range(NCT, NCT + LOCT))
        return list(range(0 if need_ctx else NCT, NT))

    def phase_mod(self, l):
        S = self.S
        with S.scope() as P:
            wm = [P.sb("wm%d" % i, [128, 8, 512], F32) for i in range(2)]
            bm = P.sb("bm", [2, 6 * D], F32)
            rows = P.sb("mrows", [2, 6 * D], F32)
            self.ld(bm, bm[:], self.b_mod, self.b_mod[l].partition_broadcast(2))
            wv = self.w_mod[l].rearrange("(kc p) n -> p kc n", p=128)
            for n in range(12):
                w = wm[n % 2]
                self.ld(w, w[:], self.w_mod, wv[:, :, n * 512:(n + 1) * 512])
                ps = self.nps()
                for kc in range(8):
                    self.mm(ps, ps[0:2, 0:512], self.cact, self.cact[:, kc * 2:kc * 2 + 2], w, w[:, kc, :], kc == 0, kc == 7)
                self.tt("dve", rows, rows[0:2, n * 512:(n + 1) * 512], ps, ps[0:2, 0:512], bm, bm[0:2, n * 512:(n + 1) * 512], ALU.add)
            self.ld(self.mod_d, self.mod_d[:, :], rows, rows[:], q="sp")

    def load_bc(self, P, l, r, idx, gain=None):
        t = P.sb("bc%d_%d" % (idx, r), [128, D], F32)
        self.ld(t, t[:], self.mod_d, self.mod_d[r, idx * D:(idx + 1) * D].partition_broadcast(128))
        if gain is not None:
            g = P.sb("bcg%d_%d" % (idx, r), [128, D], F32)
            self.ld(g, g[:], gain, gain[l].partition_broadcast(128))
            self.stt(t, t[:], t, t[:], 1.0, g, g[:], ALU.add, ALU.mult)
        return t

    def phase_norm(self, l, kind, tiles, hT, col0, router=None):
        S = self.S
        gi, si, gain = (1, 0, self.norm_mix) if kind == "mix" else (4, 3, self.norm_ffn)
        with S.scope() as P:
            G = {}
            SH = {}
            for r in sorted(set(1 if t < NCT else 0 for t in tiles)):
                G[r] = self.load_bc(P, l, r, gi, gain)
                SH[r] = self.load_bc(P, l, r, si)
            NB = 6
            xt = [P.sb("xt%d" % i, [128, D], F32) for i in range(NB)]
            junks = [P.sb("junk%d" % i, [128, D], F32) for i in range(2)]
            ss = [P.sb("ss%d" % i, [128, 1], F32) for i in range(NB)]
            hf = [P.sb("hf%d" % i, [128, D], F32) for i in range(NB)]
            hb = [P.sb("hb%d" % i, [128, D], BF16) for i in range(NB)]
            def tile_gen(it):
                i, t = it
                r = 1 if t < NCT else 0
                x, s, f, b = xt[i % NB], ss[i % NB], hf[i % NB], hb[i % NB]
                junk = junks[i % 2]
                so, sap = self.xsrc(l, t) if kind == "mix" else (self.xres, self.xres[t * 128:(t + 1) * 128, :])
                self.ld(x, x[:], so, sap)
                yield
                self.act(junk, junk[:], x, x[:], AF.Square)
                yield
                self.S.op("dve", lambda e: e.tensor_reduce(s[:], junk[:], mybir.AxisListType.X, ALU.add), outs=[s], ins=[junk])
                yield
                self.act(s, s[:], s, s[:], AF.Sqrt, bias=EPS, scale=1.0 / D)
                yield
                self.recip(s, s[:], s, s[:])
                self.stt(f, f[:], x, x[:], s[:, 0:1], G[r], G[r][:], ALU.mult, ALU.mult, ins=[s])
                yield
                if router is not None:
                    self.tt("pool", f, f[:], f, f[:], SH[r], SH[r][:], ALU.add)
                    yield
                    self.cp("act", b, b[:], f, f[:])
                    router(i, t, f)
                else:
                    self.tt("pool", b, b[:], f, f[:], SH[r], SH[r][:], ALU.add)
                if self.hx_d is not None and kind == "mix":
                    self.ld(self.hx_d, self.hx_d[t * 128:(t + 1) * 128, :], b, b[:])
                yield
                pb = self.npb()
                for kc in range(8):
                    self.tr(pb, pb[:, kc * 128:(kc + 1) * 128], b, b[:, kc * 128:(kc + 1) * 128], self.identb[:])
                yield
                c0 = col0 + i * 128
                self.cp("act", hT, hT[:, :, c0:c0 + 128], pb, pb[:].rearrange("p (k t) -> p k t", k=8))

            self.run_pipe(list(enumerate(tiles)), tile_gen)

    def tok_chunks(self):
        return [(0, CTX)] + [(CTX + i * 512, 512) for i in range(SEQ // 512)]

    def load_w(self, wt, src_obj, src_ap):
        self.ld(wt, wt, src_obj, src_ap, q="pool")

    def linear_fm(self, P, l, col0, nblk, epilogue, per_block_done=None, chunks=None):
        wts = [P.sb("wfm%d" % i, [128, 8, 512], BF16) for i in range(2)]
        wv = self.w_in[l].rearrange("(kc p) n -> p kc n", p=128)
        gi = 0
        for g0 in range(0, nblk, 4):
            nb = min(4, nblk - g0)
            wt = wts[gi % 2]
            gi += 1
            self.S.dma("pool", wt, wt[:, :, 0:nb * 128], self.w_in, wv[:, :, col0 + g0 * 128:col0 + (g0 + nb) * 128])
            for b in range(nb):
                for ci, (t0, n) in enumerate(self.tok_chunks() if chunks is None else chunks):
                    ps = self.nps()
                    for kc in range(8):
                        self.mm(ps, ps[:, 0:n], wt, wt[:, kc, b * 128:(b + 1) * 128], self.hT, self.hT[:, kc, t0:t0 + n], kc == 0, kc == 7)
                    epilogue(g0 + b, ci, ps, t0, n)
                if per_block_done is not None:
                    per_block_done(g0 + b)

    def phase_qkv(self, l):
        S = self.S
        W = TALL + 4
        wv = self.w_in[l].rearrange("(kc p) n -> p kc n", p=128)
        with S.scope() as P:
            U = P.sb("convU", [128, W + 4], F32)
            accs = [P.sb("convA%d" % i, [128, W], F32) for i in range(2)]
            sqbs = [P.sb("sqb%d" % i, [128, W], BF16) for i in range(2)]
            stgs = [P.sb("stg%d" % i, [128, W], BF16) for i in range(2)]
            cw = P.sb("cw", [128, 12, 5], F32)
            rn = [P.sb("rn%d" % i, [128, 512], F32) for i in range(2)]
            tms = [P.sb("tms%d" % i, [128, 8, 128], BF16) for i in range(2)]
            wts = [P.sb("wfm%d" % i, [128, 8, 512], BF16) for i in range(2)]
            self.ld(cw, cw[:], self.dn_convT, self.dn_convT[l].rearrange("(b p) k -> p b k", p=128))
            S.op("dve", lambda e: e.memset(U[:], 0.0), outs=[U])
            cnt = [0, 0]

            def blk(b):
                acc, sqb, stg = accs[b % 2], sqbs[b % 2], stgs[b % 2]
                wt = wts[(b // 4) % 2]
                if b % 4 == 0:
                    self.S.dma("pool", wt, wt[:], self.w_in, wv[:, :, O_Q + b * 128:O_Q + (b + 4) * 128])
                bb = b % 4
                for ci, (t0, n) in enumerate(self.tok_chunks()):
                    ps = self.nps()
                    for kc in range(8):
                        self.mm(ps, ps[:, 0:n], wt, wt[:, kc, bb * 128:(bb + 1) * 128], self.hT, self.hT[:, kc, t0:t0 + n], kc == 0, kc == 7)
                    off = 2 + t0 if t0 < CTX else 2 + 4 + t0
                    self.cp("act", U, U[:, off:off + n], ps, ps[:, 0:n])
                yield
                self.ts("dve", acc, acc[:], U, U[:, 0:W], cw[:, b, 0:1], None, ALU.mult, ins=[cw])
                for k in range(1, 5):
                    self.stt(acc, acc[:], U, U[:, k:k + W], cw[:, b, k:k + 1], acc, acc[:], ALU.mult, ALU.add, ins=[cw])
                yield
                self.act(acc, acc[:], acc, acc[:], AF.Silu)
                kind, h = b // 4, b % 4
                if kind < 2:
                    self.tt("pool", sqb, sqb[:], acc, acc[:], acc, acc[:], ALU.mult)
                    yield
                    for c0 in range(0, W, 512):
                        n = min(512, W - c0)
                        ps = self.nps()
                        self.mm(ps, ps[:, 0:n], self.ones_b, self.ones_b[:], sqb, sqb[:, c0:c0 + n])
                        r = rn[cnt[0] % 2]
                        cnt[0] += 1
                        self.act(r, r[:, 0:n], ps, ps[:, 0:n], AF.Sqrt, bias=EPS, scale=1.0)
                        self.recip(r, r[:, 0:n], r, r[:, 0:n])
                        self.tt("dve", stg, stg[:, c0:c0 + n], acc, acc[:, c0:c0 + n], r, r[:, 0:n], ALU.mult)
                        if (c0 // 512) % 3 == 2:
                            yield
                    dst = self.qT_d if kind == 0 else self.kT_d
                    self.ld(dst, dst[:, 0:NCT, h, :], stg, stg[:, 0:CTX].rearrange("p (t k) -> p t k", k=128))
                    self.ld(dst, dst[:, NCT:NT, h, :], stg, stg[:, CTX + 4:W].rearrange("p (t k) -> p t k", k=128))
                else:
                    self.cp("pool", stg, stg[:], acc, acc[:])
                yield
                if kind >= 1:
                    dst = self.ktm_d if kind == 1 else self.vtm_d
                    for t0 in range(0, NT, 8):
                        nt = min(8, NT - t0)
                        pb = self.npb()
                        for i in range(nt):
                            t = t0 + i
                            c0 = t * 128 if t < NCT else t * 128 + 4
                            self.tr(pb, pb[:, i * 128:(i + 1) * 128], stg, stg[:, c0:c0 + 128], self.identb[:])
                        tm = tms[cnt[1] % 2]
                        cnt[1] += 1
                        self.cp("act", tm, tm[:, 0:nt, :], pb, pb[:, 0:nt * 128].rearrange("p (t k) -> p t k", k=128))
                        self.ld(dst, dst[t0:t0 + nt, :, h, :].rearrange("t p k -> p t k"), tm, tm[:, 0:nt, :])
                        yield

            self.run_pipe(list(range(12)), blk, max_active=2)

    def phase_zab(self, l):
        S = self.S
        wv = self.w_in[l].rearrange("(kc p) n -> p kc n", p=128)
        with S.scope() as P:
            wz = P.sb("wz", [128, 8, 512], BF16)
            wab = P.sb("wab", [128, 8, 16], BF16)
            self.S.dma("pool", wz, wz[:], self.w_in, wv[:, :, O_Z:O_Z + 512])
            self.S.dma("pool", wab, wab[:], self.w_in, wv[:, :, O_A:O_A + 16])
            zs = [P.sb("zs%d" % i, [128, 512], BF16) for i in range(2)]
            for t in range(NT):
                ps = self.nps()
                for kc in range(8):
                    self.mm(ps, ps[:, 0:512], self.hT, self.hT[:, kc, t * 128:(t + 1) * 128], wz, wz[:, kc, :], kc == 0, kc == 7)
                z = zs[t % 2]
                self.act(z, z[:], ps, ps[:, 0:512], AF.Silu)
                self.ld(self.z_d, self.z_d[t * 128:(t + 1) * 128, :], z, z[:])
            ps = self.nps()
            for t in range(NT):
                for kc in range(8):
                    self.mm(ps, ps[:, t * 16:(t + 1) * 16], self.hT, self.hT[:, kc, t * 128:(t + 1) * 128], wab, wab[:, kc, :], kc == 0, kc == 7)
            al = P.sb("alog", [128, 8], F32)
            dtb = P.sb("dtb", [128, 8], F32)
            self.ld(al, al[:], self.dn_a_log, self.dn_a_log[l].partition_broadcast(128))
            self.ld(dtb, dtb[:], self.dn_dt_bias, self.dn_dt_bias[l].partition_broadcast(128))
            self.act(al, al[:], al, al[:], AF.Exp)
            tmp = P.sb("abtmp", [128, NT, 8], F32)
            pv = ps[:, 0:NT * 16].rearrange("p (t c) -> p t c", c=16)
            self.tt("dve", tmp, tmp[:], ps, pv[:, :, 0:8], dtb, dtb[:].unsqueeze(1).to_broadcast([128, NT, 8]), ALU.add)
            self.act(tmp, tmp[:], tmp, tmp[:], AF.Exp)
            self.act(tmp, tmp[:], tmp, tmp[:], AF.Ln, bias=1.0, scale=1.0)
            self.stt(self.gb, self.gb[:, :, 0:8], tmp, tmp[:], -1.0, al, al[:].unsqueeze(1).to_broadcast([128, NT, 8]), ALU.mult, ALU.mult)
            self.act(self.gb, self.gb[:, :, 8:16], ps, pv[:, :, 8:16], AF.Sigmoid)
            if self.gb_d is not None:
                self.ld(self.gb_d, self.gb_d[:], self.gb, self.gb[:].rearrange("p t c -> p (t c)"))

    def phase_lat(self, l):
        S = self.S
        wv = self.w_in[l].rearrange("(kc p) n -> p kc n", p=128)
        with S.scope() as P:
            wl = P.sb("wl", [128, 8, 640], BF16)
            self.S.dma("pool", wl, wl[:], self.w_in, wv[:, :, O_CQ:O_CQ + 640])
            wk = P.sb("wkpe", [128, 8, 64], BF16)
            self.S.dma("pool", wk, wk[:, :, 0:32], self.w_in, wv[:, :, O_KPE:O_KPE + 32])
            for (d0, s0) in ((0, 8), (8, 0), (16, 24), (24, 16)):
                self.S.dma("pool", wk, wk[:, :, 32 + d0:32 + d0 + 8], self.w_in, wv[:, :, O_KPE + s0:O_KPE + s0 + 8])
            gn = P.sb("latg", [128, 5], F32)
            self.ld(gn, gn[:], self.lat_gain, self.lat_gain[l])
            sets = [dict(raw=[P.sb("raw%d" % i, [128, 512], F32) for i in range(3)], sq=[P.sb("sq%d" % i, [128, 512], BF16) for i in range(3)],
                         rs=P.sb("rs", [128, 512], F32), outb=[P.sb("lout%d" % i, [128, 512], BF16) for i in range(5)],
                         cs=P.sb("ropecs", [32, 2, 512], F32), t1=P.sb("kt1", [32, 512], F32), t2=P.sb("kt2", [32, 512], F32), kb=P.sb("kpeb", [32, 512], BF16)) for _ in range(2)]

            def chunk_gen(it):
                ci, (t0, n) = it
                B_ = sets[ci % 2]
                raw, sq, rs, outb, cs, t1, t2, kb = B_["raw"], B_["sq"], B_["rs"], B_["outb"], B_["cs"], B_["t1"], B_["t2"], B_["kb"]
                self.ld(cs, cs[:, :, 0:n], self.c_rope, self.c_rope[:, :, t0:t0 + n].rearrange("c p t -> p c t"))
                for (b0, nb, R) in ((0, 3, 384.0), (3, 2, 256.0)):
                    for b in range(nb):
                        ps = self.nps()
                        for kc in range(8):
                            self.mm(ps, ps[:, 0:n], wl, wl[:, kc, (b0 + b) * 128:(b0 + b + 1) * 128], self.hT, self.hT[:, kc, t0:t0 + n], kc == 0, kc == 7)
                        self.cp("act", raw[b], raw[b][:, 0:n], ps, ps[:, 0:n])
                        self.tt("pool", sq[b], sq[b][:, 0:n], raw[b], raw[b][:, 0:n], raw[b], raw[b][:, 0:n], ALU.mult)
                    yield
                    ps = self.nps()
                    for b in range(nb):
                        self.mm(ps, ps[:, 0:n], self.ones_b, self.ones_b[:], sq[b], sq[b][:, 0:n], b == 0, b == nb - 1)
                    self.act(rs, rs[:, 0:n], ps, ps[:, 0:n], AF.Sqrt, bias=EPS, scale=1.0 / R)
                    yield
                    self.recip(rs, rs[:, 0:n], rs, rs[:, 0:n])
                    for b in range(nb):
                        o = outb[b0 + b]
                        self.stt(o, o[:, 0:n], raw[b], raw[b][:, 0:n], gn[:, b0 + b:b0 + b + 1], rs, rs[:, 0:n], ALU.mult, ALU.mult, ins=[gn])
                        self.ld(self.lat_d, self.lat_d[b0 + b, :, t0:t0 + n], o, o[:, 0:n])
                    yield
                psA = self.nps()
                for kc in range(8):
                    self.mm(psA, psA[0:32, 0:n], wk, wk[:, kc, 0:32], self.hT, self.hT[:, kc, t0:t0 + n], kc == 0, kc == 7)
                for kc in range(8):
                    self.mm(psA, psA[0:32, 512:512 + n], wk, wk[:, kc, 32:64], self.hT, self.hT[:, kc, t0:t0 + n], kc == 0, kc == 7)
                self.tt("dve", t1, t1[:, 0:n], psA, psA[0:32, 0:n], cs, cs[:, 0, 0:n], ALU.mult)
                self.tt("dve", t2, t2[:, 0:n], psA, psA[0:32, 512:512 + n], cs, cs[:, 1, 0:n], ALU.mult)
                yield
                self.tt("pool", kb, kb[:, 0:n], t1, t1[:, 0:n], t2, t2[:, 0:n], ALU.add)
                self.ld(self.kpe_d, self.kpe_d[:, t0:t0 + n], kb, kb[:, 0:n])

            self.run_pipe(list(enumerate(self.tok_chunks())), chunk_gen, max_active=2)

    def phase_fg(self, l):
        with self.S.scope() as P:
            st = [P.sb("fgs%d" % i, [128, 512], BF16) for i in range(3)]
            cnt = [0]

            def epi_f(b, ci, ps, t0, n):
                s = st[cnt[0] % 3]
                cnt[0] += 1
                self.cp("act", s, s[:, 0:n], ps, ps[:, 0:n])
                self.ld(self.uT_d, self.uT_d[b, :, t0:t0 + n], s, s[:, 0:n])

            def epi_g(b, ci, ps, t0, n):
                s = st[cnt[0] % 3]
                cnt[0] += 1
                self.act(s, s[:, 0:n], ps, ps[:, 0:n], AF.Sigmoid)
                self.ld(self.gT_d, self.gT_d[b, :, t0:t0 + n], s, s[:, 0:n])

            self.linear_fm(P, l, O_F, 4, epi_f)
            gch = [c for c in self.tok_chunks() if CTX <= c[0] < CTX + LOC] if l == DEPTH - 1 else None
            self.linear_fm(P, l, O_G, 24, epi_g, chunks=gch)


    def phase_dn(self, l, need_ctx):
        S = self.S
        order = [list(range(NT)), [1, 0] + list(range(NT - 1, NCT - 1, -1))]
        otiles = set(self.out_tiles(l, need_ctx))
        B3 = [128, 4, 128]

        def hb(ap):
            return ap.unsqueeze(2).to_broadcast(B3)

        def mb(ap):
            return ap.unsqueeze(1).to_broadcast(B3)

        with S.scope() as P:
            St = [P.sb("St%d" % d, B3, F32) for d in range(2)]
            Sb = [P.sb("Sb%d" % d, B3, BF16) for d in range(2)]
            for d in range(2):
                S.op("dve", lambda e: e.memset(St[d][:], 0.0), outs=[St[d]])
                S.op("dve", lambda e: e.memset(Sb[d][:], 0.0), outs=[Sb[d]])
            L = {}
            for d in range(2):
                for par in range(2):
                    k = (d, par)
                    L[k] = dict(
                        qt=P.sb("qt", B3, BF16), kt=P.sb("kt", B3, BF16), km=P.sb("km", B3, BF16), vm=P.sb("vm", B3, BF16),
                        sm=P.sb("sm", [128, 20], F32), sm2=P.sb("sm2", [128, 8], F32), gm=P.sb("gm", B3, F32),
                        tD=P.sb("tD", B3, F32), Es=P.sb("Es", B3, F32), EsN=P.sb("EsN", B3, F32), ET=P.sb("ET", B3, F32),
                        tN=P.sb("tN", B3, F32), Na=P.sb("Na", B3, BF16), Nb=P.sb("Nb", B3, BF16), Nta=P.sb("Nta", B3, BF16), Ntb=P.sb("Ntb", B3, BF16),
                        Ra=P.sb("Ra", B3, BF16), Rb=P.sb("Rb", B3, BF16), aT=P.sb("aT", B3, BF16),
                        vb=P.sb("vb", B3, F32), kd=P.sb("kd", B3, BF16), r2t=P.sb("r2t", B3, F32), r2=P.sb("r2", B3, BF16),
                        vn=P.sb("vn", B3, BF16), tS=P.sb("tS", B3, F32), avs=P.sb("avs", B3, F32), to=P.sb("to", B3, F32), o=P.sb("o", B3, F32))
            def lane_step(s, d):
                t = order[d][s]
                b = L[(d, s % 2)]
                want_o = t in otiles
                if l == DEPTH - 1 and d == 0 and t >= NCT + LOCT:
                    return
                qt, kt, km, vm, sm, sm2 = b["qt"], b["kt"], b["km"], b["vm"], b["sm"], b["sm2"]
                self.ld(qt, qt[:], self.qT_d, self.qT_d[:, t, :, :])
                self.ld(kt, kt[:], self.kT_d, self.kT_d[:, t, :, :])
                self.ld(km, km[:], self.ktm_d, self.ktm_d[t])
                self.ld(vm, vm[:], self.vtm_d, self.vtm_d[t])
                g = self.gb[:, t, d * 4:(d + 1) * 4]
                beta = self.gb[:, t, 8 + d * 4:8 + (d + 1) * 4]
                Mle = self.mk(M_ULE if d == 0 else M_LGE)
                Msg = self.mk(M_SGT if d == 0 else M_SLT)
                NEGd = self.mk(M_NEGF if d == 0 else M_NEGB)
                NEGt = self.mk(M_NEGB if d == 0 else M_NEGF)
                psS = self.nps()
                self.mm(psS, psS[:, 0:4], self.masks, Mle, self.gb, g)
                self.mm(psS, psS[:, 4:8], self.ones_f, self.ones_f[:], self.gb, g)
                self.cp("act", sm2, sm2[:], psS, psS[:, 0:8])
                self.act(sm, sm[:, 0:8], sm2, sm2[:], AF.Exp)
                self.tt("dve", sm2, sm2[:, 0:4], sm2, sm2[:, 4:8], sm2, sm2[:, 0:4], ALU.subtract)
                self.act(sm, sm[:, 8:12], sm2, sm2[:, 0:4], AF.Exp)
                self.stt(sm, sm[:, 12:16], sm, sm[:, 0:4], -1.0, self.gb, beta, ALU.mult, ALU.mult)
                self.ts("dve", sm, sm[:, 16:20], sm, sm[:, 0:4], 128.0 ** -0.5, None, ALU.mult)
                yield
                gm = b["gm"]
                self.tt("pool", gm, gm[:], self.masks, mb(Msg), self.gb, hb(g), ALU.mult)
                psD = self.nps()
                for h in range(4):
                    self.mm(psD, psD[:, h * 128:(h + 1) * 128], self.masks, Mle, gm, gm[:, h, :])
                    self.mm(psD, psD[:, 512 + h * 128:512 + (h + 1) * 128], gm, gm[:, h, :], self.masks, Mle)
                tD, Es, EsN, ET = b["tD"], b["Es"], b["EsN"], b["ET"]
                self.tt("dve", tD, tD[:], psD, psD[:, 0:512].rearrange("p (h k) -> p h k", h=4), self.masks, mb(NEGd), ALU.add)
                self.act(Es, Es[:], tD, tD[:], AF.Exp)
                self.tt("pool", EsN, EsN[:], Es, Es[:], self.masks, mb(self.mk(M_OFFD)), ALU.mult)
                self.tt("dve", tD, tD[:], psD, psD[:, 512:1024].rearrange("p (h k) -> p h k", h=4), self.masks, mb(NEGt), ALU.add)
                self.act(ET, ET[:], tD, tD[:], AF.Exp)
                yield
                psG = self.nps()
                for h in range(4):
                    self.mm(psG, psG[:, h * 128:(h + 1) * 128], kt, kt[:, h, :], kt, kt[:, h, :])
                    self.mm(psG, psG[:, 512 + h * 128:512 + (h + 1) * 128], kt, kt[:, h, :], qt, qt[:, h, :])
                tN, N, Nt, R, aT = b["tN"], b["Na"], b["Nta"], b["Ra"], b["aT"]
                N2, Nt2, R2 = b["Nb"], b["Ntb"], b["Rb"]
                self.tt("dve", tN, tN[:], psG, psG[:, 0:512].rearrange("p (h k) -> p h k", h=4), self.gb, hb(beta), ALU.mult)
                self.tt("pool", N, N[:], tN, tN[:], EsN, EsN[:], ALU.mult)
                self.stt(aT, aT[:], psG, psG[:, 512:1024].rearrange("p (h k) -> p h k", h=4), 128.0 ** -0.5, ET, ET[:], ALU.mult, ALU.mult)
                yield
                pb = self.npb()
                for h in range(4):
                    self.tr(pb, pb[:, h * 128:(h + 1) * 128], N, N[:, h, :], self.identb[:])
                self.cp("act", Nt, Nt[:], pb, pb[:, 0:512].rearrange("p (h k) -> p h k", h=4))
                self.tt("pool", R, R[:], Nt, Nt[:], self.identb, mb(self.identb[:]), ALU.add)
                for lev in range(6):
                    yield
                    ps1 = self.nps()
                    for h in range(4):
                        self.mm(ps1, ps1[:, h * 128:(h + 1) * 128], Nt, Nt[:, h, :], N, N[:, h, :])
                        if lev < 5:
                            self.mm(ps1, ps1[:, 512 + h * 128:512 + (h + 1) * 128], N, N[:, h, :], Nt, Nt[:, h, :])
                    self.cp("act", N2, N2[:], ps1, ps1[:, 0:512].rearrange("p (h k) -> p h k", h=4))
                    if lev < 5:
                        self.cp("dve", Nt2, Nt2[:], ps1, ps1[:, 512:1024].rearrange("p (h k) -> p h k", h=4))
                    yield
                    ps2 = self.nps()
                    for h in range(4):
                        self.mm(ps2, ps2[:, h * 128:(h + 1) * 128], N2, N2[:, h, :], R, R[:, h, :])
                    self.tt("dve", R2, R2[:], ps2, ps2[:, 0:512].rearrange("p (h k) -> p h k", h=4), R, R[:], ALU.add)
                    N, N2 = N2, N
                    Nt, Nt2 = Nt2, Nt
                    R, R2 = R2, R
                b["Rfin"] = R

            def scan_step(s, d):
                t = order[d][s]
                b = L[(d, s % 2)]
                want_o = t in otiles
                if l == DEPTH - 1 and d == 0 and t >= NCT + LOCT:
                    return
                qt, kt, km, vm, sm, aT, R = b["qt"], b["kt"], b["km"], b["vm"], b["sm"], b["aT"], b["Rfin"]
                beta = self.gb[:, t, 8 + d * 4:8 + (d + 1) * 4]
                vb, kd, r2t, r2, vn, tS, avs, to, o = b["vb"], b["kd"], b["r2t"], b["r2"], b["vn"], b["tS"], b["avs"], b["to"], b["o"]
                self.tt("pool", vb, vb[:], vm, vm[:], self.gb, hb(beta), ALU.mult)
                self.tt("pool", kd, kd[:], km, km[:], sm, hb(sm[:, 8:12]), ALU.mult)
                psK = self.nps()
                for h in range(4):
                    self.mm(psK, psK[:, h * 128:(h + 1) * 128], kt, kt[:, h, :], Sb[d], Sb[d][:, h, :])
                    if want_o:
                        self.mm(psK, psK[:, 512 + h * 128:512 + (h + 1) * 128], qt, qt[:, h, :], Sb[d], Sb[d][:, h, :])
                self.tt("dve", r2t, r2t[:], psK, psK[:, 0:512].rearrange("p (h k) -> p h k", h=4), sm, hb(sm[:, 12:16]), ALU.mult)
                if want_o:
                    self.tt("dve", to, to[:], psK, psK[:, 512:1024].rearrange("p (h k) -> p h k", h=4), sm, hb(sm[:, 16:20]), ALU.mult)
                self.tt("pool", r2, r2[:], r2t, r2t[:], vb, vb[:], ALU.add)
                yield
                psV = self.nps()
                for h in range(4):
                    self.mm(psV, psV[:, h * 128:(h + 1) * 128], R, R[:, h, :], r2, r2[:, h, :])
                self.cp("act", vn, vn[:], psV, psV[:, 0:512].rearrange("p (h k) -> p h k", h=4))
                yield
                psU = self.nps()
                for h in range(4):
                    self.mm(psU, psU[:, h * 128:(h + 1) * 128], kd, kd[:, h, :], vn, vn[:, h, :])
                    if want_o:
                        self.mm(psU, psU[:, 512 + h * 128:512 + (h + 1) * 128], aT, aT[:, h, :], vn, vn[:, h, :])
                self.tt("pool", tS, tS[:], St[d], St[d][:], sm, hb(sm[:, 4:8]), ALU.mult)
                self.tt("dve", St[d], St[d][:], tS, tS[:], psU, psU[:, 0:512].rearrange("p (h k) -> p h k", h=4), ALU.add)
                self.cp("act", Sb[d], Sb[d][:], St[d], St[d][:])
                if want_o:
                    self.cp("act", avs, avs[:], psU, psU[:, 512:1024].rearrange("p (h k) -> p h k", h=4))
                    self.tt("pool", o, o[:], to, to[:], avs, avs[:], ALU.add)
                    self.ld(self.o_d[d], self.o_d[d][t * 128:(t + 1) * 128, :], o, o[:].rearrange("p h k -> p (h k)"))

            for s in range(NT + 1):
                gens = ([scan_step(s - 1, 0), scan_step(s - 1, 1)] if s > 0 else []) + ([lane_step(s, 0), lane_step(s, 1)] if s < NT else [])
                while gens:
                    for g_ in list(gens):
                        try:
                            next(g_)
                        except StopIteration:
                            gens.remove(g_)

    def phase_dnout(self, l, need_ctx):
        S = self.S
        tiles = self.out_tiles(l, need_ctx)
        with S.scope() as P:
            gn = P.sb("dng", [128, 128], F32)
            self.ld(gn, gn[:], self.dn_norm, self.dn_norm[l].partition_broadcast(128))
            bufs = [dict(of=P.sb("of", [128, 512], F32), ob=P.sb("ob", [128, 512], F32), z=P.sb("z", [128, 512], BF16), sq=P.sb("sq", [128, 512], F32),
                         ss=P.sb("ss", [128, 4], F32), gz=P.sb("gz", [128, 512], F32), y=P.sb("y", [128, 512], BF16), yT=P.sb("yT", [128, 4, 128], BF16)) for _ in range(10)]
            def tile_gen(it):
                i, t = it
                b = bufs[i % 10]
                of, ob, z, sq, ss, gz, y, yT = b["of"], b["ob"], b["z"], b["sq"], b["ss"], b["gz"], b["y"], b["yT"]
                rows = slice(t * 128, (t + 1) * 128)
                self.ld(of, of[:], self.o_d[0], self.o_d[0][rows, :])
                self.ld(ob, ob[:], self.o_d[1], self.o_d[1][rows, :])
                self.ld(z, z[:], self.z_d, self.z_d[rows, :])
                yield
                self.tt("pool", of, of[:], of, of[:], ob, ob[:], ALU.add)
                self.tt("pool", gz, gz[:].rearrange("p (h k) -> p h k", h=4), z, z[:].rearrange("p (h k) -> p h k", h=4), gn, gn[:].unsqueeze(1).to_broadcast([128, 4, 128]), ALU.mult)
                yield
                self.act(sq, sq[:], of, of[:], AF.Square)
                yield
                self.S.op("dve", lambda e: e.tensor_reduce(ss[:], sq[:].rearrange("p (h k) -> p h k", h=4), mybir.AxisListType.X, ALU.add), outs=[ss], ins=[sq])
                yield
                self.act(ss, ss[:], ss, ss[:], AF.Sqrt, bias=EPS, scale=1.0 / 128)
                yield
                self.recip(ss, ss[:], ss, ss[:])
                self.tt("dve", sq, sq[:].rearrange("p (h k) -> p h k", h=4), of, of[:].rearrange("p (h k) -> p h k", h=4), ss, ss[:].unsqueeze(2).to_broadcast([128, 4, 128]), ALU.mult)
                yield
                self.tt("pool", y, y[:], sq, sq[:], gz, gz[:], ALU.mult)
                yield
                pb = self.npb()
                for c in range(4):
                    self.tr(pb, pb[:, c * 128:(c + 1) * 128], y, y[:, c * 128:(c + 1) * 128], self.identb[:])
                yield
                self.cp("act", yT, yT[:], pb, pb[:, 0:512].rearrange("p (c k) -> p c k", c=4))
                self.ld(self.yT_d, self.yT_d[0, :, :, rows].rearrange("c p k -> p c k"), yT, yT[:])

            self.run_pipe(list(enumerate(tiles)), tile_gen)

    def phase_mla(self, l, need_ctx):
        S = self.S
        SCALE = 96.0 ** -0.5
        with S.scope() as P:
            lat = [P.sb("lat%d" % i, [128, TALL], BF16) for i in range(5)]
            for i in range(5):
                self.ld(lat[i], lat[i][:], self.lat_d, self.lat_d[i])
            KT = [P.sb("KT%d" % h, [96, TALL], BF16) for h in range(8)]
            VP = P.sb("VP", [128, NT, 512], BF16)
            wq = P.sb("wq", [128, 3, 768], BF16)
            wqs = P.sb("wqs", [128, 3, 768], BF16)
            wkv = P.sb("wkv", [128, 2, 1024], BF16)
            qv = self.mla_w_qup[l].rearrange("(rc p) n -> p rc n", p=128)
            self.S.dma("pool", wq, wq[:], self.mla_w_qup, qv)
            self.S.dma("pool", wqs, wqs[:], self.mla_w_qup, qv)
            q4 = self.mla_w_qup[l].rearrange("(rc p) (h d) -> p rc h d", p=128, d=96)
            w4 = wqs[:].rearrange("p rc (h d) -> p rc h d", d=96)
            for (d0, s0) in ((0, 8), (8, 0), (16, 24), (24, 16)):
                for rc in range(3):
                    self.S.dma("pool", wqs, w4[:, rc, :, 64 + d0:64 + d0 + 8], self.mla_w_qup, q4[:, rc, :, 64 + s0:64 + s0 + 8])
            self.S.dma("pool", wkv, wkv[:], self.mla_w_kvup, self.mla_w_kvup[l].rearrange("(rc p) n -> p rc n", p=128))
            for h in range(8):
                self.ld(KT[h], KT[h][64:96, :], self.kpe_d, self.kpe_d[:, :])
            for (t0, n) in self.tok_chunks():
                for h in range(8):
                    ps = self.nps()
                    for rc in range(2):
                        self.mm(ps, ps[0:64, 0:n], wkv, wkv[:, rc, h * 128:h * 128 + 64], lat[3 + rc], lat[3 + rc][:, t0:t0 + n], rc == 0, rc == 1)
                    self.cp("act" if h % 2 else "dve", KT[h], KT[h][0:64, t0:t0 + n], ps, ps[0:64, 0:n])
            wv4 = wkv[:].rearrange("p rc (h d) -> p rc h d", d=128)
            for t in range(NT):
                ps = self.nps()
                for rc in range(2):
                    self.mm(ps, ps[:, 0:512].rearrange("p (h d) -> p h d", d=64), lat[3 + rc], lat[3 + rc][:, t * 128:(t + 1) * 128], wkv, wv4[:, rc, :, 64:128], rc == 0, rc == 1)
                self.cp("act" if t % 2 else "dve", VP, VP[:, t, :], ps, ps[:, 0:512])
            self.S.barrier()
            H = [Obj("psh%d" % i, self.PS[i // 2].h[:, (i % 2) * 512:(i % 2 + 1) * 512], True) for i in range(6)]
            psOo, psOd = H[0], H[1]
            QP = [Obj("psq%d" % i, self.PB[i].h[:].bitcast(F32), True) for i in range(2)]
            rot = H[2:6]
            ri = [0]

            def nh():
                p = rot[ri[0] % 4]
                ri[0] += 1
                return p
            css = [P.sb("qcs%d" % i, [96, 2, 512], F32) for i in range(2)]
            QT = [P.sb("QT%d" % i, [96, 512], BF16) for i in range(2)]
            t1s = [P.sb("qt1_%d" % i, [96, 512], F32) for i in range(2)]
            t2s = [P.sb("qt2_%d" % i, [96, 512], F32) for i in range(2)]
            PT = [P.sb("PT%d" % i, [128, 512], BF16) for i in range(3)]
            rec = P.sb("rec", [128, 512], F32)
            yb = [P.sb("yb%d" % i, [128, 512], BF16) for i in range(2)]
            nq = (LOC if l == DEPTH - 1 else SEQ) // 512
            chunks = ([(0, CTX, list(range(NCT)))] if need_ctx else []) + [(CTX + i * 512, 512, list(range(NT))) for i in range(nq)]
            items = [(ci, t0, n, ktiles, c, j) for ci, (t0, n, ktiles) in enumerate(chunks) for c in range(4) for j in range(2)]

            def build_q(k):
                ci, t0, n, ktiles, c, j = items[k]
                cs = css[ci % 2]
                if c == 0 and j == 0:
                    self.ld(cs, cs[64:96, :, 0:n], self.c_rope, self.c_rope[:, :, t0:t0 + n].rearrange("c p t -> p c t"))
                h = 2 * c + j
                q, t1, t2 = QT[k % 2], t1s[k % 2], t2s[k % 2]
                pA, pB = QP
                for rc in range(3):
                    self.mm(pA, pA[0:96, 0:n], wq, wq[:, rc, h * 96:(h + 1) * 96], lat[rc], lat[rc][:, t0:t0 + n], rc == 0, rc == 2)
                for rc in range(3):
                    self.mm(pB, pB[0:96, 0:n], wqs, wqs[:, rc, h * 96:(h + 1) * 96], lat[rc], lat[rc][:, t0:t0 + n], rc == 0, rc == 2)
                self.cp("act", q, q[0:64, 0:n], pA, pA[0:64, 0:n])
                self.tt("dve", t1, t1[64:96, 0:n], pA, pA[64:96, 0:n], cs, cs[64:96, 0, 0:n], ALU.mult)
                self.tt("dve", t2, t2[64:96, 0:n], pB, pB[64:96, 0:n], cs, cs[64:96, 1, 0:n], ALU.mult)
                self.tt("pool", q, q[64:96, 0:n], t1, t1[64:96, 0:n], t2, t2[64:96, 0:n], ALU.add)

            build_q(0)
            for k, (ci, t0, n, ktiles, c, j) in enumerate(items):
                h = 2 * c + j
                q = QT[k % 2]
                y = yb[c % 2]
                sts = {}

                def score(i):
                    kt = ktiles[i]
                    p = nh()
                    self.mm(p, p[:, 0:n], KT[h], KT[h][:, kt * 128:(kt + 1) * 128], q, q[:, 0:n])
                    sts[i] = p
                AHEAD = 3
                for i in range(min(AHEAD, len(ktiles))):
                    score(i)
                if k + 1 < len(items):
                    build_q(k + 1)
                for i, kt in enumerate(ktiles):
                    if i + AHEAD < len(ktiles):
                        score(i + AHEAD)
                    p = sts.pop(i)
                    pt = PT[i % 3]
                    self.act(pt, pt[:, 0:n], p, p[:, 0:n], AF.Exp, scale=SCALE)
                    last = i == len(ktiles) - 1
                    self.mm(psOo, psOo[:, 0:n], VP, VP[:, kt, c * 128:(c + 1) * 128], pt, pt[:, 0:n], i == 0, last, inc=last)
                    self.mm(psOd, psOd[:, 0:n], self.ones_b, self.ones_b[:], pt, pt[:, 0:n], i == 0, last, inc=True)
                r0 = j * 64
                self.recip(rec, rec[r0:r0 + 64, 0:n], psOd, psOd[r0:r0 + 64, 0:n])
                self.tt("dve", y, y[r0:r0 + 64, 0:n], psOo, psOo[r0:r0 + 64, 0:n], rec, rec[r0:r0 + 64, 0:n], ALU.mult)
                if j == 1:
                    self.ld(self.yT_d, self.yT_d[1, c, :, t0:t0 + n], y, y[:, 0:n])

    def phase_fft(self, l, need_ctx):
        S = self.S
        with S.scope() as P:
            dc = P.sb("dftc", [128, 256], BF16)
            self.ld(dc, dc[:], self.c_dftc, self.c_dftc[:])
            AB = P.sb("AB", [128, NT, 1024], BF16)
            ut = [P.sb("ut%d" % i, [128, 4, 128], BF16) for i in range(2)]
            tiles = list(range(0 if need_ctx else NCT, NT))
            for i, t in enumerate(tiles):
                u = ut[i % 2]
                self.ld(u, u[:], self.uT_d, self.uT_d[:, :, t * 128:(t + 1) * 128].rearrange("g p k -> p g k"))
                ps = self.nps()
                for g in range(4):
                    self.mm(ps, ps[:, g * 256:(g + 1) * 256], u, u[:, g, :], dc, dc[:])
                self.cp("act" if i % 2 else "dve", AB, AB[:, t, :], ps, ps[:])
            self.S.barrier()
            H = [Obj("fph%d" % i, self.PS[i // 2].h[:, (i % 2) * 512:(i % 2 + 1) * 512], True) for i in range(6)]
            DB = [P.sb("dft%d" % i, [128, 32, 512], BF16) for i in range(3)]
            yc = [P.sb("yc%d" % i, [128, 512], BF16) for i in range(2)]
            nkc = (LOC if l == DEPTH - 1 else SEQ) // 512
            jobs = ([("c", 0, 0, NCT, 0, 256)] if need_ctx else []) + [("x", kc, NCT, SEQ // 128, CTX, 512) for kc in range(nkc)]
            passes = [(ji, pi) for ji in range(len(jobs)) for pi in range(2)]

            def load(p):
                ji, pi = passes[p]
                nm, kc, tb, ntt, tok0, w = jobs[ji]
                Mb = DB[p % 3]
                src = self.c_dft[("C" if pi == 0 else "S", nm)]
                self.ld(Mb, Mb[:, 0:ntt, 0:w], src, src[kc])

            oi = 0
            load(0)
            accs = None
            for p, (ji, pi) in enumerate(passes):
                nm, kc, tb, ntt, tok0, w = jobs[ji]
                if p + 1 < len(passes):
                    load(p + 1)
                if pi == 0:
                    accs = [H[(4 * ji + g) % 6] for g in range(4)]
                Mb = DB[p % 3]
                for tt in range(ntt):
                    for g in range(4):
                        o0 = g * 256 + (0 if pi == 0 else 128)
                        self.mm(accs[g], accs[g][:, 0:w], AB, AB[:, tb + tt, o0:o0 + 128], Mb, Mb[:, tt, 0:w], pi == 0 and tt == 0, pi == 1 and tt == ntt - 1)
                if pi == 1:
                    for g in range(4):
                        y = yc[oi % 2]
                        oi += 1
                        self.cp("act" if oi % 2 else "dve", y, y[:, 0:w], accs[g], accs[g][:, 0:w])
                        self.ld(self.yT_d, self.yT_d[2, g, :, tok0 + kc * w:tok0 + (kc + 1) * w], y, y[:, 0:w])

    def phase_merge(self, l, need_ctx):
        S = self.S
        with S.scope() as P:
            wb = P.sb("wb", [128, 12, D], BF16)
            self.S.dma("pool", wb, wb[:], self.w_branch, self.w_branch[l].rearrange("n (cc p) d -> p (n cc) d", p=128))
            wo = P.sb("wo", [128, 8, D], BF16)
            self.S.dma("pool", wo, wo[:], self.w_out, self.w_out[l].rearrange("(kc p) d -> p kc d", p=128))
            GT = {r: self.load_bc(P, l, r, 2) for r in ([0, 1] if need_ctx else [0])}
            yT = [P.sb("myT%d" % i, [128, 12, 512], BF16) for i in range(2)]
            gT = [P.sb("mgT%d" % i, [128, 24, 512], BF16) for i in range(2)]
            mT = [P.sb("mmT%d" % i, [128, 8, 512], BF16) for i in range(2)]
            tas = [[[P.sb("mta%d_%d_%d" % (c_, k, i), [128, 512], F32) for i in range(3)] for k in range(2)] for c_ in range(2)]
            xt = [P.sb("mxt%d" % i, [128, D], F32) for i in range(4)]
            tx = [P.sb("mtx%d" % i, [128, D], F32) for i in range(4)]
            chunks = [c for c in self.tok_chunks() if need_ctx or c[0] >= CTX]
            if l == DEPTH - 1:
                chunks = [c for c in chunks if CTX <= c[0] < CTX + LOC]
            def chunk_gen(it):
                ci, (t0, n) = it
                r = 1 if t0 < CTX else 0
                y, g, m = yT[ci % 2], gT[ci % 2], mT[ci % 2]
                self.ld(y, y[:, :, 0:n], self.yT_d, self.yT_d[:, :, :, t0:t0 + n].rearrange("n c p k -> p (n c) k"))
                self.ld(g, g[:, :, 0:n], self.gT_d, self.gT_d[:, :, t0:t0 + n].rearrange("b p k -> p b k"))
                yield
                for db in range(8):
                    ta = tas[ci % 2][db % 2]
                    for nb in range(3):
                        ps = self.nps()
                        for cc in range(4):
                            self.mm(ps, ps[:, 0:n], wb, wb[:, nb * 4 + cc, db * 128:(db + 1) * 128], y, y[:, nb * 4 + cc, 0:n], cc == 0, cc == 3)
                        self.tt("dve", ta[nb], ta[nb][:, 0:n], ps, ps[:, 0:n], g, g[:, nb * 8 + db, 0:n], ALU.mult)
                    self.tt("pool", ta[0], ta[0][:, 0:n], ta[0], ta[0][:, 0:n], ta[1], ta[1][:, 0:n], ALU.add)
                    self.tt("pool", m, m[:, db, 0:n], ta[0], ta[0][:, 0:n], ta[2], ta[2][:, 0:n], ALU.add)
                    yield
                for tl in range(n // 128):
                    t = t0 // 128 + tl
                    x, txx = xt[(ci % 2) * 2 + tl % 2], tx[(ci % 2) * 2 + tl % 2]
                    so, sap = self.xsrc(l, t)
                    self.ld(x, x[:], so, sap)
                    ps = self.nps()
                    for nh in range(2):
                        for db in range(8):
                            self.mm(ps, ps[:, nh * 512:(nh + 1) * 512], m, m[:, db, tl * 128:(tl + 1) * 128], wo, wo[:, db, nh * 512:(nh + 1) * 512], db == 0, db == 7)
                    self.tt("dve", txx, txx[:], ps, ps[:], GT[r], GT[r][:], ALU.mult)
                    self.tt("pool", txx, txx[:], txx, txx[:], x, x[:], ALU.add)
                    self.ld(self.xres, self.xres[t * 128:(t + 1) * 128, :], txx, txx[:])
                    yield

            self.run_pipe(list(enumerate(chunks)), chunk_gen, max_active=2)

    def phase_ffn(self, l):
        S = self.S
        moe = l % 2 == 1
        ei = l // 2
        last = l == DEPTH - 1
        NE, FF = (NEXP, EFF) if moe else (1, DFF)
        nfb = FF // 128
        nun = (nfb + 6) // 7
        units = []
        f_ = 0
        for i in range(nun):
            k_ = (nfb - f_ + (nun - i) - 1) // (nun - i)
            units.append((f_, k_))
            f_ += k_
        UMAX = max(u[1] for u in units)
        tiles_all = list(range(NCT, NCT + LOCT)) if last else list(range(NT))
        CH = 8
        for c0 in range(0, len(tiles_all), CH):
            tiles = tiles_all[c0:c0 + CH]
            ntl = len(tiles)
            ntok = ntl * 128
            with S.scope() as P:
                hT = P.sb("fhT", [128, 8, CH * 128], BF16)
                acc = P.sb("facc", [128, CH, D], F32)
                comb = P.sb("comb", [128, CH, NEXP], F32)
                conds = sorted(set(1 if t < NCT else 0 for t in tiles))
                GT = {r: self.load_bc(P, l, r, 5) for r in conds}
                if moe:
                    wr = P.sb("wr", [128, 8, NEXP], F32)
                    self.ld(wr, wr[:], self.moe_router, self.moe_router[ei].rearrange("(kc p) e -> p kc e", p=128))
                    fT = P.sb("fT", [128, 8, 128], F32)
                    rt = P.sb("rt", [128, 8 * NEXP], F32)

                    def router(i, t, f):
                        ps = self.nps()
                        for kc in range(8):
                            self.S.op("pe", lambda e: e.transpose(ps[:, kc * 128:(kc + 1) * 128], f[:, kc * 128:(kc + 1) * 128], self.mk(M_ID)), outs=[ps], ins=[f, self.masks])
                        self.cp("act", fT, fT[:], ps, ps[:].rearrange("p (k t) -> p k t", k=8))
                        pl = self.nps()
                        for kc in range(8):
                            self.mm(pl, pl[:, 0:NEXP], fT, fT[:, kc, :], wr, wr[:, kc, :], kc == 0, kc == 7)
                        lg, m1, eq, l2, m2, sel, ex, sw = (rt[:, k * 8:(k + 1) * 8] for k in range(8))
                        self.cp("act", rt, lg, pl, pl[:, 0:NEXP])
                        self.S.op("dve", lambda e: e.tensor_reduce(m1[:, 0:1], lg, mybir.AxisListType.X, ALU.max), outs=[rt], ins=[rt])
                        self.ts("dve", rt, eq, rt, lg, m1[:, 0:1], None, ALU.is_equal)
                        self.stt(rt, l2, rt, eq, -1e30, rt, lg, ALU.mult, ALU.add)
                        self.S.op("dve", lambda e: e.tensor_reduce(m2[:, 0:1], l2, mybir.AxisListType.X, ALU.max), outs=[rt], ins=[rt])
                        self.ts("dve", rt, sel, rt, lg, m2[:, 0:1], None, ALU.is_ge)
                        self.ts("dve", rt, m1[:, 1:2], rt, m1[:, 0:1], -1.0, None, ALU.mult)
                        self.act(rt, ex, rt, lg, AF.Exp, bias=m1[:, 1:2], scale=1.0)
                        self.tt("dve", rt, ex, rt, ex, rt, sel, ALU.mult)
                        self.S.op("dve", lambda e: e.tensor_reduce(sw[:, 0:1], ex, mybir.AxisListType.X, ALU.add), outs=[rt], ins=[rt])
                        self.recip(rt, sw[:, 1:2], rt, sw[:, 0:1])
                        self.ts("dve", comb, comb[:, i, :], rt, ex, sw[:, 1:2], None, ALU.mult, ins=[rt])
                else:
                    router = None
                self.phase_norm(l, "ffn", tiles, hT, 0, router=router)
                tchunks = [(o, min(512, ntok - o)) for o in range(0, ntok, 512)]
                with S.scope() as Q:
                    actTs = [Q.sb("actT%d" % i, [128, UMAX, CH * 128], BF16) for i in range(2)]
                    wg = [Q.sb("wg%d" % i, [128, 8, 512], BF16) for i in range(2)]
                    wu = [Q.sb("wu%d" % i, [128, 8, 512], BF16) for i in range(2)]
                    wds = [Q.sb("wd%d" % i, [128, UMAX, D], BF16) for i in range(2)]
                    ui = 0
                    sg = [Q.sb("sg%d" % i, [128, 512], F32) for i in range(2)]
                    gi = 0
                    si = 0
                    first = True
                    for e in range(NE):
                        if moe:
                            Wg, Wu, Wd = self.moe_w_gate, self.moe_w_up, self.moe_w_down
                            wgv = Wg[ei, e].rearrange("(kc p) f -> p kc f", p=128)
                            wuv = Wu[ei, e].rearrange("(kc p) f -> p kc f", p=128)
                            wdv = Wd[ei, e]
                        else:
                            Wg, Wu, Wd = self.ffn_w_gate, self.ffn_w_up, self.ffn_w_down
                            wgv = Wg[ei].rearrange("(kc p) f -> p kc f", p=128)
                            wuv = Wu[ei].rearrange("(kc p) f -> p kc f", p=128)
                            wdv = Wd[ei]
                        for (fb0, nfbu) in units:
                            actT, wd = actTs[ui % 2], wds[ui % 2]
                            ui += 1
                            self.S.dma("pool", wd, wd[:, 0:nfbu, :], Wd, wdv[fb0 * 128:(fb0 + nfbu) * 128, :].rearrange("(fb p) d -> p fb d", p=128))
                            for g0 in range(0, nfbu, 4):
                                nb = min(4, nfbu - g0)
                                g_, u_ = wg[gi % 2], wu[gi % 2]
                                gi += 1
                                f0 = (fb0 + g0) * 128
                                self.S.dma("pool", g_, g_[:, :, 0:nb * 128], Wg, wgv[:, :, f0:f0 + nb * 128])
                                self.S.dma("pool", u_, u_[:, :, 0:nb * 128], Wu, wuv[:, :, f0:f0 + nb * 128])
                                for b in range(nb):
                                    for (o, n) in tchunks:
                                        psg = self.nps()
                                        for kc in range(8):
                                            self.mm(psg, psg[:, 0:n], g_, g_[:, kc, b * 128:(b + 1) * 128], hT, hT[:, kc, o:o + n], kc == 0, kc == 7)
                                        for kc in range(8):
                                            self.mm(psg, psg[:, 512:512 + n], u_, u_[:, kc, b * 128:(b + 1) * 128], hT, hT[:, kc, o:o + n], kc == 0, kc == 7)
                                        s_ = sg[si % 2]
                                        si += 1
                                        self.act(s_, s_[:, 0:n], psg, psg[:, 0:n], AF.Silu)
                                        self.tt("dve", actT, actT[:, g0 + b, o:o + n], s_, s_[:, 0:n], psg, psg[:, 512:512 + n], ALU.mult)
                            for i in range(ntl):
                                ps = self.nps()
                                for nh in range(2):
                                    for fbi in range(nfbu):
                                        self.mm(ps, ps[:, nh * 512:(nh + 1) * 512], actT, actT[:, fbi, i * 128:(i + 1) * 128], wd, wd[:, fbi, nh * 512:(nh + 1) * 512], fbi == 0, fbi == nfbu - 1)
                                if moe:
                                    if first:
                                        self.ts("dve", acc, acc[:, i, :], ps, ps[:], comb[:, i, e:e + 1], None, ALU.mult, ins=[comb])
                                    else:
                                        self.stt(acc, acc[:, i, :], ps, ps[:], comb[:, i, e:e + 1], acc, acc[:, i, :], ALU.mult, ALU.add, ins=[comb])
                                else:
                                    if first:
                                        self.cp("act", acc, acc[:, i, :], ps, ps[:])
                                    else:
                                        self.tt("dve", acc, acc[:, i, :], acc, acc[:, i, :], ps, ps[:], ALU.add)
                            first = False
                with S.scope() as Q:
                    xt = [Q.sb("rxt%d" % i, [128, D], F32) for i in range(2)]
                    tx = [Q.sb("rtx%d" % i, [128, D], F32) for i in range(2)]
                    if last:
                        fg = Q.sb("fgain", [128, D], F32)
                        self.ld(fg, fg[:], self.final_norm, self.final_norm[0].partition_broadcast(128))
                        sq = Q.sb("fsq", [128, D], F32)
                        ss = [Q.sb("fss%d" % i, [128, 1], F32) for i in range(2)]
                    for i, t in enumerate(tiles):
                        r = 1 if t < NCT else 0
                        x, y = xt[i % 2], tx[i % 2]
                        self.ld(x, x[:], self.xres, self.xres[t * 128:(t + 1) * 128, :])
                        self.tt("pool", y, y[:], acc, acc[:, i, :], GT[r], GT[r][:], ALU.mult)
                        self.tt("dve", y, y[:], y, y[:], x, x[:], ALU.add)
                        if not last:
                            self.ld(self.xres, self.xres[t * 128:(t + 1) * 128, :], y, y[:])
                        else:
                            s_ = ss[i % 2]
                            self.act(sq, sq[:], y, y[:], AF.Square)
                            self.S.op("dve", lambda e: e.tensor_reduce(s_[:], sq[:], mybir.AxisListType.X, ALU.add), outs=[s_], ins=[sq])
                            self.act(s_, s_[:], s_, s_[:], AF.Sqrt, bias=EPS, scale=1.0 / D)
                            self.recip(s_, s_[:], s_, s_[:])
                            self.stt(x, x[:], y, y[:], s_[:, 0:1], fg, fg[:], ALU.mult, ALU.mult, ins=[s_])
                            self.ld(self.y_out, self.y_out[(t - NCT) * 128:(t - NCT + 1) * 128, :], x, x[:])

_CONSTS = {}


def make_in_maps(inputs, n_cores=8):
    f = lambda a: np.ascontiguousarray(np.asarray(a, dtype=np.float32))
    shared = {
        "w_mod": f(inputs["w_mod"]), "b_mod": f(inputs["b_mod"]), "norm_mix": f(inputs["norm_mix"]), "norm_ffn": f(inputs["norm_ffn"]),
        "dn_norm": f(inputs["dn_norm"]),
        "lat_gain": f(np.concatenate([np.asarray(inputs["mla_q_norm"]).reshape(DEPTH, 3, 128), np.asarray(inputs["mla_kv_norm"]).reshape(DEPTH, 2, 128)], 1).transpose(0, 2, 1)),
        "mla_w_qup": f(inputs["mla_w_qup"]), "mla_w_kvup": f(inputs["mla_w_kvup"]), "w_branch": f(inputs["w_branch"]), "w_out": f(inputs["w_out"]),
        "ffn_w_gate": f(inputs["ffn_w_gate"]), "ffn_w_up": f(inputs["ffn_w_up"]), "ffn_w_down": f(inputs["ffn_w_down"]),
        "moe_router": f(inputs["moe_router"]),
        "moe_w_gate": f(inputs["moe_w_gate"]), "moe_w_up": f(inputs["moe_w_up"]), "moe_w_down": f(inputs["moe_w_down"]),
        "final_norm": f(np.asarray(inputs["final_norm"]).reshape(1, D)),
    }
    w_in = np.asarray(inputs["w_in"], np.float32)
    conv = np.asarray(inputs["dn_conv"], np.float32)
    alog = np.asarray(inputs["dn_a_log"], np.float32)
    dtb = np.asarray(inputs["dn_dt_bias"], np.float32)
    per = {}
    for flip in (False, True):
        d = {}
        if flip:
            perm = np.arange(INW)
            perm[O_A:O_A + 8] = np.r_[O_A + 4:O_A + 8, O_A:O_A + 4]
            perm[O_B:O_B + 8] = np.r_[O_B + 4:O_B + 8, O_B:O_B + 4]
            d["w_in"] = np.ascontiguousarray(w_in[:, :, perm])
            d["dn_convT"] = f(np.transpose(conv[:, ::-1, :], (0, 2, 1)))
            d["dn_a_log"] = f(alog[:, ::-1, :].reshape(DEPTH, 8))
            d["dn_dt_bias"] = f(dtb[:, ::-1, :].reshape(DEPTH, 8))
        else:
            d["w_in"] = f(w_in)
            d["dn_convT"] = f(np.transpose(conv, (0, 2, 1)))
            d["dn_a_log"] = f(alog.reshape(DEPTH, 8))
            d["dn_dt_bias"] = f(dtb.reshape(DEPTH, 8))
        if flip not in _CONSTS:
            _CONSTS[flip] = host_consts(flip)
        d.update(_CONSTS[flip])
        per[flip] = d
    maps = []
    x = np.asarray(inputs["x"], np.float32)
    c = np.asarray(inputs["c"], np.float32)
    ctx = np.asarray(inputs["ctx"], np.float32)
    cc = np.asarray(inputs["c_ctx"], np.float32)
    for core in range(n_cores):
        b = core % 4
        flip = core >= 4
        m = dict(shared)
        m.update(per[flip])
        m["x_in"] = np.ascontiguousarray(x[b][::-1] if flip else x[b])
        m["ctx_in"] = np.ascontiguousarray(ctx[b][::-1] if flip else ctx[b])
        c2 = np.stack([c[b], cc], 0)
        m["cT"] = np.ascontiguousarray(c2.reshape(2, 8, 128).transpose(2, 1, 0).reshape(128, 16))
        maps.append(m)
    return maps


_NC = None


def kernel(**inputs):
    global _NC
    if _NC is None:
        _NC = Builder().build()
    maps = make_in_maps(inputs, 8)
    res = run_bass_kernel_spmd(_NC, maps, core_ids=list(range(8)))
    out = np.empty((4, SEQ, D), np.float32)
    for core in range(8):
        y = np.asarray(res.results[core]["y_out"], np.float32)
        if core < 4:
            out[core, :LOC] = y
        else:
            out[core - 4, LOC:] = y[::-1]
    return out
```

```python
from contextlib import ExitStack, contextmanager
import numpy as np
import ml_dtypes
import concourse.bass as bass
import concourse.mybir as mybir
from concourse.bass_utils import run_bass_kernel_spmd

F32 = mybir.dt.float32
BF16 = mybir.dt.bfloat16
ALU = mybir.AluOpType
AF = mybir.ActivationFunctionType

D = 1024
SEQ = 4096
CTX = 256
TALL = SEQ + CTX
NT = TALL // 128
NCT = CTX // 128
DEPTH = 2
INW = 6320
O_Q, O_K, O_V, O_Z, O_A, O_B, O_CQ, O_CKV, O_KPE, O_F, O_G = 0, 512, 1024, 1536, 2048, 2056, 2064, 2448, 2704, 2736, 3248
DFF = 2816
NEXP = 8
EFF = 3584
EPS = 1e-6
NDSEM = 96
LOCT = 16
LOC = LOCT * 128


class Obj:
    __slots__ = ("name", "h", "w", "r", "ds", "is_sb")

    def __init__(self, name, h, is_sb):
        self.name = name
        self.h = h
        self.w = []
        self.r = []
        self.ds = None
        self.is_sb = is_sb

    def __getitem__(self, idx):
        return self.h[idx]


class Scope:
    def __init__(self, S):
        self.S = S
        self.stack = ExitStack()
        self.objs = []

    def sb(self, name, shape, dt):
        S = self.S
        S.uid += 1
        o = Obj(name, self.stack.enter_context(S.nc.sbuf_tensor("%s_%d" % (name, S.uid), list(shape), dt)), True)
        self.objs.append(o)
        return o


class Sched:
    def __init__(self, nc, stack):
        self.nc = nc
        self.E = {}
        for nm, h in (("pe", nc.tensor), ("act", nc.scalar), ("dve", nc.vector), ("pool", nc.gpsimd), ("sp", nc.sync)):
            sem = stack.enter_context(nc.semaphore("sem_" + nm))
            self.E[nm] = dict(h=h, sem=sem, cnt=0, seen={})
        self.dpool = [dict(sem=stack.enter_context(nc.semaphore("dsem%d" % i)), cnt=0, free=True) for i in range(NDSEM)]
        self.ninst = 0
        self.uid = 0
        self.stack = stack

    def ps(self, name, shape, dt=F32):
        return Obj(name, self.stack.enter_context(self.nc.psum_tensor(name, list(shape), dt)), True)

    def dram(self, name, shape, dt, kind="Internal"):
        t = self.nc.dram_tensor(name, list(shape), dt, kind=kind)
        return Obj(name, t.ap(), False)

    @contextmanager
    def scope(self):
        sc = Scope(self)
        try:
            yield sc
        finally:
            self.barrier()
            for o in sc.objs:
                if o.ds is not None:
                    self.dpool[o.ds]["free"] = True
                    o.ds = None
            sc.stack.close()

    def _wait(self, e, tok):
        sem, val = tok
        k = id(sem)
        pe = self.E["pe"]
        if sem is pe["sem"] and val > pe["cnt"]:
            pe["h"].nop().then_inc(pe["sem"], 1)
            pe["cnt"] += 1
            self.ninst += 1
            self.nforced = getattr(self, "nforced", 0) + 1
        E = self.E[e]
        if E["seen"].get(k, 0) >= val:
            return
        E["seen"][k] = val
        E["h"].wait_ge(sem, val)
        self.ninst += 1

    def barrier(self):
        toks = [(E["sem"], E["cnt"]) for E in self.E.values() if E["cnt"] > 0]
        toks += [(d["sem"], d["cnt"]) for d in self.dpool if d["cnt"] > 0]
        for e in self.E:
            for tok in toks:
                self._wait(e, tok)

    def _deps(self, e, ins, outs):
        mysem = id(self.E[e]["sem"])
        for o in ins:
            if not o.is_sb:
                continue
            for tok in o.w:
                if e == "pe" and id(tok[0]) == mysem:
                    continue
                self._wait(e, tok)
        for o in outs:
            if not o.is_sb:
                continue
            for tok in o.w:
                if e == "pe" and id(tok[0]) == mysem:
                    continue
                self._wait(e, tok)
            for tok in o.r:
                if id(tok[0]) == mysem:
                    continue
                self._wait(e, tok)

    def _prune(self, toks):
        best = {}
        for s, v in toks:
            k = id(s)
            if k not in best or best[k][1] < v:
                best[k] = (s, v)
        return list(best.values())

    def op(self, e, fn, outs=(), ins=(), inc=True):
        E = self.E[e]
        self._deps(e, ins, outs)
        inst = fn(E["h"])
        self.ninst += 1
        tok = (E["sem"], E["cnt"] + 1)
        if inc:
            inst.then_inc(E["sem"], 1)
            E["cnt"] += 1
        for o in ins:
            if o.is_sb:
                o.r.append(tok)
                if len(o.r) > 16:
                    o.r = self._prune(o.r)
        for o in outs:
            if o.is_sb:
                o.w = [tok]
                o.r = []
        return inst

    def dma(self, q, out_obj, out_ap, in_obj, in_ap):
        E = self.E[q]
        sbo = out_obj if out_obj.is_sb else in_obj
        assert sbo.is_sb
        self._deps(q, [in_obj], [out_obj])
        if sbo.ds is None:
            for i, d in enumerate(self.dpool):
                if d["free"]:
                    d["free"] = False
                    sbo.ds = i
                    break
            else:
                raise RuntimeError("out of dma semaphores")
        d = self.dpool[sbo.ds]
        if d["cnt"]:
            self._wait(q, (d["sem"], d["cnt"]))
        inst = E["h"].dma_start(out=out_ap, in_=in_ap)
        d["cnt"] += 16
        inst.then_inc(d["sem"], 16)
        self.ninst += 1
        tok = (d["sem"], d["cnt"])
        if in_obj.is_sb:
            in_obj.r.append(tok)
            if len(in_obj.r) > 16:
                in_obj.r = self._prune(in_obj.r)
        if out_obj.is_sb:
            out_obj.w = [tok]
            out_obj.r = []
        return inst


def host_consts(flip):
    C = {}
    i = np.arange(128)
    m, j = np.meshgrid(i, i, indexing="ij")
    NEG = -30000.0
    C["ident_f"] = np.eye(128)
    C["ULE"] = (m <= j)
    C["LGE"] = (m >= j)
    C["SGT"] = (m > j)
    C["SLT"] = (m < j)
    C["NEGF"] = np.where(j <= m, 0.0, NEG)
    C["NEGB"] = np.where(j >= m, 0.0, NEG)
    C["OFFD"] = -(1.0 - np.eye(128))
    cm = np.concatenate([np.asarray(C[k], np.float32) for k in ("ident_f", "ULE", "LGE", "SGT", "SLT", "NEGF", "NEGB", "OFFD")], 1)
    out = {"cmask": np.ascontiguousarray(cm)}
    out["ident_b"] = np.eye(128).astype(ml_dtypes.bfloat16)
    ang = 2 * np.pi * np.outer(i, i) / 128.0
    out["dft_c"] = (np.concatenate([np.cos(ang), np.sin(ang)], 1) / np.sqrt(128.0)).astype(ml_dtypes.bfloat16)
    for nm, T in (("x", SEQ), ("c", CTX)):
        t = np.arange(T)
        if flip:
            t = t[::-1]
        ph = ((np.outer(t, t) % T).astype(np.float64) * (2 * np.pi / T)).astype(np.float32)
        for tag in ("C", "S"):
            M = (np.cos(ph) if tag == "C" else -np.sin(ph)) / np.float32(np.sqrt(T))
            KC = 512 if nm == "x" else 256
            M = M.reshape(T // 128, 128, T // KC, KC).transpose(2, 1, 0, 3)
            out["dft%s_%s" % (tag, nm)] = np.ascontiguousarray(M).astype(ml_dtypes.bfloat16)
        del ph
    inv = 10000.0 ** (-np.arange(0, 16, 2, dtype=np.float32) / 16)
    pos = np.arange(SEQ)
    if flip:
        pos = pos[::-1]
    ar = np.outer(inv, (pos // 64).astype(np.float32))
    ac = np.outer(inv, (pos % 64).astype(np.float32))
    cosx = np.concatenate([np.cos(ar), np.cos(ar), np.cos(ac), np.cos(ac)], 0)
    sinx = np.concatenate([-np.sin(ar), np.sin(ar), -np.sin(ac), np.sin(ac)], 0)
    cos = np.concatenate([np.ones((32, CTX)), cosx], 1)
    sin = np.concatenate([np.zeros((32, CTX)), sinx], 1)
    out["rope_cs"] = np.stack([cos, sin], 0).astype(np.float32)
    return out


M_ID, M_ULE, M_LGE, M_SGT, M_SLT, M_NEGF, M_NEGB, M_OFFD = range(8)


class Builder:
    def __init__(self, stop_after=None, dbg=()):
        self.stop_after = stop_after
        self.dbg = set(dbg)
        self.nc = bass.Bass("TRN2", target_bir_lowering=False)
        self.in_names = []
        self.out_names = ["y_out"]

    def inp(self, name, shape, dt=F32):
        self.in_names.append(name)
        return self.S.dram(name, shape, dt, kind="ExternalInput")

    def scratch(self, name, shape, dt):
        if name in self.dbg:
            self.out_names.append(name)
            return self.S.dram(name, shape, dt, kind="ExternalOutput")
        return self.S.dram(name, shape, dt, kind="Internal")

    def build(self):
        with ExitStack() as st:
            self.S = Sched(self.nc, st)
            self._build()
        return self.nc

    def mk(self, i):
        return self.masks[:, i * 128:(i + 1) * 128]

    def nps(self, exclude=None):
        while True:
            p = self.PS[self.ps_i % len(self.PS)]
            self.ps_i += 1
            if p is not exclude:
                return p

    def npb(self):
        p = self.PB[self.pb_i % len(self.PB)]
        self.pb_i += 1
        return p

    def mm(self, ps, out_ap, a, lhsT, b, rhs, start=True, stop=True, inc=None):
        self.S.op("pe", lambda e: e.matmul(out_ap, lhsT, rhs, start=start, stop=stop), outs=[ps], ins=[a, b], inc=stop if inc is None else inc)

    def tr(self, ps, out_ap, a, in_ap, ident):
        self.S.op("pe", lambda e: e.transpose(out_ap, in_ap, ident), outs=[ps], ins=[a, self.identb, self.masks])

    def act(self, out_o, out_ap, in_o, in_ap, func, ins=(), **kw):
        self.S.op("act", lambda e: e.activation(out_ap, in_ap, func, **kw), outs=[out_o], ins=[in_o] + list(ins))

    def tt(self, eng, out_o, out_ap, a, a_ap, b, b_ap, op):
        self.S.op(eng, lambda e: e.tensor_tensor(out_ap, a_ap, b_ap, op), outs=[out_o], ins=[a, b])

    def ts(self, eng, out_o, out_ap, a, a_ap, s1, s2, op0, op1=None, ins=()):
        if op1 is None:
            self.S.op(eng, lambda e: e.tensor_scalar(out_ap, a_ap, s1, None, op0), outs=[out_o], ins=[a] + list(ins))
        else:
            self.S.op(eng, lambda e: e.tensor_scalar(out_ap, a_ap, s1, s2, op0, op1), outs=[out_o], ins=[a] + list(ins))

    def stt(self, out_o, out_ap, a, a_ap, scalar, b, b_ap, op0, op1, ins=()):
        self.S.op("dve", lambda e: e.scalar_tensor_tensor(out_ap, a_ap, scalar, b_ap, op0, op1), outs=[out_o], ins=[a, b] + list(ins))

    def cp(self, eng, out_o, out_ap, in_o, in_ap):
        if eng == "act":
            self.S.op("act", lambda e: e.copy(out_ap, in_ap), outs=[out_o], ins=[in_o])
        else:
            self.S.op(eng, lambda e: e.tensor_copy(out_ap, in_ap), outs=[out_o], ins=[in_o])

    def recip(self, out_o, out_ap, in_o, in_ap):
        self.S.op("dve", lambda e: e.reciprocal(out_ap, in_ap), outs=[out_o], ins=[in_o])

    def ld(self, out_o, out_ap, in_o, in_ap, q="sp"):
        self.S.dma(q, out_o, out_ap, in_o, in_ap)

    def run_pipe(self, items, fn, max_active=None):
        import os
        if os.environ.get("NOPIPE"):
            for it in items:
                for _ in fn(it):
                    pass
            return
        active = []
        items = list(items)
        k = 0
        while k < len(items) or active:
            if k < len(items) and (max_active is None or len(active) < max_active):
                active.append(fn(items[k]))
                k += 1
            for g in list(active):
                try:
                    next(g)
                except StopIteration:
                    active.remove(g)

    def stop(self, l, name):
        return self.stop_after is not None and self.stop_after == (l, name)

    def _build(self):
        S = self.S
        I = self.inp
        self.x_in = I("x_in", [SEQ, D])
        self.ctx_in = I("ctx_in", [CTX, D])
        self.cT = I("cT", [128, 16])
        self.w_mod = I("w_mod", [DEPTH, D, 6 * D])
        self.b_mod = I("b_mod", [DEPTH, 6 * D])
        self.norm_mix = I("norm_mix", [DEPTH, D])
        self.norm_ffn = I("norm_ffn", [DEPTH, D])
        self.w_in = I("w_in", [DEPTH, D, INW])
        self.dn_convT = I("dn_convT", [DEPTH, 1536, 5])
        self.dn_a_log = I("dn_a_log", [DEPTH, 8])
        self.dn_dt_bias = I("dn_dt_bias", [DEPTH, 8])
        self.dn_norm = I("dn_norm", [DEPTH, 128])
        self.lat_gain = I("lat_gain", [DEPTH, 128, 5])
        self.mla_w_qup = I("mla_w_qup", [DEPTH, 384, 768])
        self.mla_w_kvup = I("mla_w_kvup", [DEPTH, 256, 1024])
        self.w_branch = I("w_branch", [DEPTH, 3, 512, D])
        self.w_out = I("w_out", [DEPTH, D, D])
        self.ffn_w_gate = I("ffn_w_gate", [1, D, DFF])
        self.ffn_w_up = I("ffn_w_up", [1, D, DFF])
        self.ffn_w_down = I("ffn_w_down", [1, DFF, D])
        self.moe_router = I("moe_router", [1, D, NEXP])
        self.moe_w_gate = I("moe_w_gate", [1, NEXP, D, EFF])
        self.moe_w_up = I("moe_w_up", [1, NEXP, D, EFF])
        self.moe_w_down = I("moe_w_down", [1, NEXP, EFF, D])
        self.final_norm = I("final_norm", [1, D])
        self.c_mask = I("cmask", [128, 8 * 128])
        self.c_identb = I("ident_b", [128, 128], BF16)
        self.c_dftc = I("dft_c", [128, 256], BF16)
        self.c_dft = {("C", "x"): I("dftC_x", [8, 128, 32, 512], BF16), ("S", "x"): I("dftS_x", [8, 128, 32, 512], BF16),
                      ("C", "c"): I("dftC_c", [1, 128, 2, 256], BF16), ("S", "c"): I("dftS_c", [1, 128, 2, 256], BF16)}
        self.c_rope = I("rope_cs", [2, 32, TALL])
        self.y_out = S.dram("y_out", [LOC, D], F32, kind="ExternalOutput")
        sc = self.scratch
        self.xres = sc("xres", [TALL, D], F32)
        self.mod_d = sc("mod_d", [2, 6 * D], F32)
        self.qT_d = sc("qT_d", [128, NT, 4, 128], BF16)
        self.kT_d = sc("kT_d", [128, NT, 4, 128], BF16)
        self.ktm_d = sc("ktm_d", [NT, 128, 4, 128], BF16)
        self.vtm_d = sc("vtm_d", [NT, 128, 4, 128], BF16)
        self.z_d = sc("z_d", [TALL, 512], BF16)
        self.lat_d = sc("lat_d", [5, 128, TALL], BF16)
        self.kpe_d = sc("kpe_d", [32, TALL], BF16)
        self.uT_d = sc("uT_d", [4, 128, TALL], BF16)
        self.gT_d = sc("gT_d", [24, 128, TALL], BF16)
        self.o_d = [sc("of_d", [TALL, 512], F32), sc("ob_d", [TALL, 512], F32)]
        self.yT_d = sc("yT_d", [3, 4, 128, TALL], BF16)
        self.hx_d = sc("hx_d", [TALL, D], BF16) if "hx_d" in self.dbg else None
        self.gb_d = sc("gb_d", [128, NT * 16], F32) if "gb_d" in self.dbg else None

        with S.scope() as G:
            self.masks = G.sb("masks", [128, 8 * 128], F32)
            self.ld(self.masks, self.masks[:], self.c_mask, self.c_mask[:])
            self.identb = G.sb("identb", [128, 128], BF16)
            self.ld(self.identb, self.identb[:], self.c_identb, self.c_identb[:])
            self.ones_b = G.sb("ones_b", [128, 128], BF16)
            S.op("dve", lambda e: e.memset(self.ones_b[:], 1.0), outs=[self.ones_b])
            self.ones_f = G.sb("ones_f", [128, 128], F32)
            S.op("dve", lambda e: e.memset(self.ones_f[:], 1.0), outs=[self.ones_f])
            self.cact = G.sb("cact", [128, 16], F32)
            self.ld(self.cact, self.cact[:], self.cT, self.cT[:])
            self.act(self.cact, self.cact[:], self.cact, self.cact[:], AF.Silu)
            self.gb = G.sb("gb", [128, NT, 16], F32)
            self.PS = [S.ps("psum%d" % i, [128, 1024]) for i in range(3)]
            self.PB = [S.ps("psumb%d" % i, [128, 1024], BF16) for i in range(2)]
            self.ps_i = 0
            self.pb_i = 0
            for l in range(DEPTH):
                if self.layer(l):
                    break

    def xsrc(self, l, t):
        if l == 0:
            if t < NCT:
                return self.ctx_in, self.ctx_in[t * 128:(t + 1) * 128, :]
            return self.x_in, self.x_in[(t - NCT) * 128:(t - NCT + 1) * 128, :]
        return self.xres, self.xres[t * 128:(t + 1) * 128, :]

    def layer(self, l):
        ns = self.nc.named_scope
        last = l == DEPTH - 1
        need_ctx = not last
        with ns("L%d_mod" % l):
            self.phase_mod(l)
        if self.stop(l, "mod"):
            return True
        with self.S.scope() as P:
            self.hT = P.sb("hT", [128, 8, TALL], BF16)
            with ns("L%d_norm" % l):
                self.phase_norm(l, "mix", list(range(NT)), self.hT, 0)
            with ns("L%d_qkv" % l):
                self.phase_qkv(l)
            with ns("L%d_zab" % l):
                self.phase_zab(l)
            with ns("L%d_lat" % l):
                self.phase_lat(l)
            with ns("L%d_fg" % l):
                self.phase_fg(l)
            if self.stop(l, "proj"):
                return True
        with ns("L%d_dn" % l):
            self.phase_dn(l, need_ctx)
        if self.stop(l, "dn"):
            return True
        with ns("L%d_dnout" % l):
            self.phase_dnout(l, need_ctx)
        with ns("L%d_mla" % l):
            self.phase_mla(l, need_ctx)
        with ns("L%d_fft" % l):
            self.phase_fft(l, need_ctx)
        if self.stop(l, "mix"):
            return True
        with ns("L%d_merge" % l):
            self.phase_merge(l, need_ctx)
        if self.stop(l, "merge"):
            return True
        with ns("L%d_ffn" % l):
            self.phase_ffn(l)
        return False

    def out_tiles(self, l, need_ctx):
        if l == DEPTH - 1:
            return list(range(NCT, NCT + LOCT))
        return list(range(0 if need_ctx else NCT, NT))

    def phase_mod(self, l):
        S = self.S
        with S.scope() as P:
            wm = [P.sb("wm%d" % i, [128, 8, 512], F32) for i in range(2)]
            bm = P.sb("bm", [2, 6 * D], F32)
            rows = P.sb("mrows", [2, 6 * D], F32)
            self.ld(bm, bm[:], self.b_mod, self.b_mod[l].partition_broadcast(2))
            wv = self.w_mod[l].rearrange("(kc p) n -> p kc n", p=128)
            for n in range(12):
                w = wm[n % 2]
                self.ld(w, w[:], self.w_mod, wv[:, :, n * 512:(n + 1) * 512])
                ps = self.nps()
                for kc in range(8):
                    self.mm(ps, ps[0:2, 0:512], self.cact, self.cact[:, kc * 2:kc * 2 + 2], w, w[:, kc, :], kc == 0, kc == 7)
                self.tt("dve", rows, rows[0:2, n * 512:(n + 1) * 512], ps, ps[0:2, 0:512], bm, bm[0:2, n * 512:(n + 1) * 512], ALU.add)
            self.ld(self.mod_d, self.mod_d[:, :], rows, rows[:], q="sp")

    def load_bc(self, P, l, r, idx, gain=None):
        t = P.sb("bc%d_%d" % (idx, r), [128, D], F32)
        self.ld(t, t[:], self.mod_d, self.mod_d[r, idx * D:(idx + 1) * D].partition_broadcast(128))
        if gain is not None:
            g = P.sb("bcg%d_%d" % (idx, r), [128, D], F32)
            self.ld(g, g[:], gain, gain[l].partition_broadcast(128))
            self.stt(t, t[:], t, t[:], 1.0, g, g[:], ALU.add, ALU.mult)
        return t

    def phase_norm(self, l, kind, tiles, hT, col0, router=None):
        S = self.S
        gi, si, gain = (1, 0, self.norm_mix) if kind == "mix" else (4, 3, self.norm_ffn)
        with S.scope() as P:
            G = {}
            SH = {}
            for r in sorted(set(1 if t < NCT else 0 for t in tiles)):
                G[r] = self.load_bc(P, l, r, gi, gain)
                SH[r] = self.load_bc(P, l, r, si)
            NB = 6
            xt = [P.sb("xt%d" % i, [128, D], F32) for i in range(NB)]
            junks = [P.sb("junk%d" % i, [128, D], F32) for i in range(2)]
            ss = [P.sb("ss%d" % i, [128, 1], F32) for i in range(NB)]
            hf = [P.sb("hf%d" % i, [128, D], F32) for i in range(NB)]
            hb = [P.sb("hb%d" % i, [128, D], BF16) for i in range(NB)]
            def tile_gen(it):
                i, t = it
                r = 1 if t < NCT else 0
                x, s, f, b = xt[i % NB], ss[i % NB], hf[i % NB], hb[i % NB]
                junk = junks[i % 2]
                so, sap = self.xsrc(l, t) if kind == "mix" else (self.xres, self.xres[t * 128:(t + 1) * 128, :])
                self.ld(x, x[:], so, sap)
                yield
                self.act(junk, junk[:], x, x[:], AF.Square)
                yield
                self.S.op("dve", lambda e: e.tensor_reduce(s[:], junk[:], mybir.AxisListType.X, ALU.add), outs=[s], ins=[junk])
                yield
                self.act(s, s[:], s, s[:], AF.Sqrt, bias=EPS, scale=1.0 / D)
                yield
                self.recip(s, s[:], s, s[:])
                self.stt(f, f[:], x, x[:], s[:, 0:1], G[r], G[r][:], ALU.mult, ALU.mult, ins=[s])
                yield
                if router is not None:
                    self.tt("pool", f, f[:], f, f[:], SH[r], SH[r][:], ALU.add)
                    yield
                    self.cp("act", b, b[:], f, f[:])
                    router(i, t, f)
                else:
                    self.tt("pool", b, b[:], f, f[:], SH[r], SH[r][:], ALU.add)
                if self.hx_d is not None and kind == "mix":
                    self.ld(self.hx_d, self.hx_d[t * 128:(t + 1) * 128, :], b, b[:])
                yield
                pb = self.npb()
                for kc in range(8):
                    self.tr(pb, pb[:, kc * 128:(kc + 1) * 128], b, b[:, kc * 128:(kc + 1) * 128], self.identb[:])
                yield
                c0 = col0 + i * 128
                self.cp("act", hT, hT[:, :, c0:c0 + 128], pb, pb[:].rearrange("p (k t) -> p k t", k=8))

            self.run_pipe(list(enumerate(tiles)), tile_gen)

    def tok_chunks(self):
        return [(0, CTX)] + [(CTX + i * 512, 512) for i in range(SEQ // 512)]

    def load_w(self, wt, src_obj, src_ap):
        self.ld(wt, wt, src_obj, src_ap, q="pool")

    def linear_fm(self, P, l, col0, nblk, epilogue, per_block_done=None, chunks=None):
        wts = [P.sb("wfm%d" % i, [128, 8, 512], BF16) for i in range(2)]
        wv = self.w_in[l].rearrange("(kc p) n -> p kc n", p=128)
        gi = 0
        for g0 in range(0, nblk, 4):
            nb = min(4, nblk - g0)
            wt = wts[gi % 2]
            gi += 1
            self.S.dma("pool", wt, wt[:, :, 0:nb * 128], self.w_in, wv[:, :, col0 + g0 * 128:col0 + (g0 + nb) * 128])
            for b in range(nb):
                for ci, (t0, n) in enumerate(self.tok_chunks() if chunks is None else chunks):
                    ps = self.nps()
                    for kc in range(8):
                        self.mm(ps, ps[:, 0:n], wt, wt[:, kc, b * 128:(b + 1) * 128], self.hT, self.hT[:, kc, t0:t0 + n], kc == 0, kc == 7)
                    epilogue(g0 + b, ci, ps, t0, n)
                if per_block_done is not None:
                    per_block_done(g0 + b)

    def phase_qkv(self, l):
        S = self.S
        W = TALL + 4
        wv = self.w_in[l].rearrange("(kc p) n -> p kc n", p=128)
        with S.scope() as P:
            U = P.sb("convU", [128, W + 4], F32)
            accs = [P.sb("convA%d" % i, [128, W], F32) for i in range(2)]
            sqbs = [P.sb("sqb%d" % i, [128, W], BF16) for i in range(2)]
            stgs = [P.sb("stg%d" % i, [128, W], BF16) for i in range(2)]
            cw = P.sb("cw", [128, 12, 5], F32)
            rn = [P.sb("rn%d" % i, [128, 512], F32) for i in range(2)]
            tms = [P.sb("tms%d" % i, [128, 8, 128], BF16) for i in range(2)]
            wts = [P.sb("wfm%d" % i, [128, 8, 512], BF16) for i in range(2)]
            self.ld(cw, cw[:], self.dn_convT, self.dn_convT[l].rearrange("(b p) k -> p b k", p=128))
            S.op("dve", lambda e: e.memset(U[:], 0.0), outs=[U])
            cnt = [0, 0]

            def blk(b):
                acc, sqb, stg = accs[b % 2], sqbs[b % 2], stgs[b % 2]
                wt = wts[(b // 4) % 2]
                if b % 4 == 0:
                    self.S.dma("pool", wt, wt[:], self.w_in, wv[:, :, O_Q + b * 128:O_Q + (b + 4) * 128])
                bb = b % 4
                for ci, (t0, n) in enumerate(self.tok_chunks()):
                    ps = self.nps()
                    for kc in range(8):
                        self.mm(ps, ps[:, 0:n], wt, wt[:, kc, bb * 128:(bb + 1) * 128], self.hT, self.hT[:, kc, t0:t0 + n], kc == 0, kc == 7)
                    off = 2 + t0 if t0 < CTX else 2 + 4 + t0
                    self.cp("act", U, U[:, off:off + n], ps, ps[:, 0:n])
                yield
                self.ts("dve", acc, acc[:], U, U[:, 0:W], cw[:, b, 0:1], None, ALU.mult, ins=[cw])
                for k in range(1, 5):
                    self.stt(acc, acc[:], U, U[:, k:k + W], cw[:, b, k:k + 1], acc, acc[:], ALU.mult, ALU.add, ins=[cw])
                yield
                self.act(acc, acc[:], acc, acc[:], AF.Silu)
                kind, h = b // 4, b % 4
                if kind < 2:
                    self.tt("pool", sqb, sqb[:], acc, acc[:], acc, acc[:], ALU.mult)
                    yield
                    for c0 in range(0, W, 512):
                        n = min(512, W - c0)
                        ps = self.nps()
                        self.mm(ps, ps[:, 0:n], self.ones_b, self.ones_b[:], sqb, sqb[:, c0:c0 + n])
                        r = rn[cnt[0] % 2]
                        cnt[0] += 1
                        self.act(r, r[:, 0:n], ps, ps[:, 0:n], AF.Sqrt, bias=EPS, scale=1.0)
                        self.recip(r, r[:, 0:n], r, r[:, 0:n])
                        self.tt("dve", stg, stg[:, c0:c0 + n], acc, acc[:, c0:c0 + n], r, r[:, 0:n], ALU.mult)
                        if (c0 // 512) % 3 == 2:
                            yield
                    dst = self.qT_d if kind == 0 else self.kT_d
                    self.ld(dst, dst[:, 0:NCT, h, :], stg, stg[:, 0:CTX].rearrange("p (t k) -> p t k", k=128))
                    self.ld(dst, dst[:, NCT:NT, h, :], stg, stg[:, CTX + 4:W].rearrange("p (t k) -> p t k", k=128))
                else:
                    self.cp("pool", stg, stg[:], acc, acc[:])
                yield
                if kind >= 1:
                    dst = self.ktm_d if kind == 1 else self.vtm_d
                    for t0 in range(0, NT, 8):
                        nt = min(8, NT - t0)
                        pb = self.npb()
                        for i in range(nt):
                            t = t0 + i
                            c0 = t * 128 if t < NCT else t * 128 + 4
                            self.tr(pb, pb[:, i * 128:(i + 1) * 128], stg, stg[:, c0:c0 + 128], self.identb[:])
                        tm = tms[cnt[1] % 2]
                        cnt[1] += 1
                        self.cp("act", tm, tm[:, 0:nt, :], pb, pb[:, 0:nt * 128].rearrange("p (t k) -> p t k", k=128))
                        self.ld(dst, dst[t0:t0 + nt, :, h, :].rearrange("t p k -> p t k"), tm, tm[:, 0:nt, :])
                        yield

            self.run_pipe(list(range(12)), blk, max_active=2)

    def phase_zab(self, l):
        S = self.S
        wv = self.w_in[l].rearrange("(kc p) n -> p kc n", p=128)
        with S.scope() as P:
            wz = P.sb("wz", [128, 8, 512], BF16)
            wab = P.sb("wab", [128, 8, 16], BF16)
            self.S.dma("pool", wz, wz[:], self.w_in, wv[:, :, O_Z:O_Z + 512])
            self.S.dma("pool", wab, wab[:], self.w_in, wv[:, :, O_A:O_A + 16])
            zs = [P.sb("zs%d" % i, [128, 512], BF16) for i in range(2)]
            for t in range(NT):
                ps = self.nps()
                for kc in range(8):
                    self.mm(ps, ps[:, 0:512], self.hT, self.hT[:, kc, t * 128:(t + 1) * 128], wz, wz[:, kc, :], kc == 0, kc == 7)
                z = zs[t % 2]
                self.act(z, z[:], ps, ps[:, 0:512], AF.Silu)
                self.ld(self.z_d, self.z_d[t * 128:(t + 1) * 128, :], z, z[:])
            ps = self.nps()
            for t in range(NT):
                for kc in range(8):
                    self.mm(ps, ps[:, t * 16:(t + 1) * 16], self.hT, self.hT[:, kc, t * 128:(t + 1) * 128], wab, wab[:, kc, :], kc == 0, kc == 7)
            al = P.sb("alog", [128, 8], F32)
            dtb = P.sb("dtb", [128, 8], F32)
            self.ld(al, al[:], self.dn_a_log, self.dn_a_log[l].partition_broadcast(128))
            self.ld(dtb, dtb[:], self.dn_dt_bias, self.dn_dt_bias[l].partition_broadcast(128))
            self.act(al, al[:], al, al[:], AF.Exp)
            tmp = P.sb("abtmp", [128, NT, 8], F32)
            pv = ps[:, 0:NT * 16].rearrange("p (t c) -> p t c", c=16)
            self.tt("dve", tmp, tmp[:], ps, pv[:, :, 0:8], dtb, dtb[:].unsqueeze(1).to_broadcast([128, NT, 8]), ALU.add)
            self.act(tmp, tmp[:], tmp, tmp[:], AF.Exp)
            self.act(tmp, tmp[:], tmp, tmp[:], AF.Ln, bias=1.0, scale=1.0)
            self.stt(self.gb, self.gb[:, :, 0:8], tmp, tmp[:], -1.0, al, al[:].unsqueeze(1).to_broadcast([128, NT, 8]), ALU.mult, ALU.mult)
            self.act(self.gb, self.gb[:, :, 8:16], ps, pv[:, :, 8:16], AF.Sigmoid)
            if self.gb_d is not None:
                self.ld(self.gb_d, self.gb_d[:], self.gb, self.gb[:].rearrange("p t c -> p (t c)"))

    def phase_lat(self, l):
        S = self.S
        wv = self.w_in[l].rearrange("(kc p) n -> p kc n", p=128)
        with S.scope() as P:
            wl = P.sb("wl", [128, 8, 640], BF16)
            self.S.dma("pool", wl, wl[:], self.w_in, wv[:, :, O_CQ:O_CQ + 640])
            wk = P.sb("wkpe", [128, 8, 64], BF16)
            self.S.dma("pool", wk, wk[:, :, 0:32], self.w_in, wv[:, :, O_KPE:O_KPE + 32])
            for (d0, s0) in ((0, 8), (8, 0), (16, 24), (24, 16)):
                self.S.dma("pool", wk, wk[:, :, 32 + d0:32 + d0 + 8], self.w_in, wv[:, :, O_KPE + s0:O_KPE + s0 + 8])
            gn = P.sb("latg", [128, 5], F32)
            self.ld(gn, gn[:], self.lat_gain, self.lat_gain[l])
            sets = [dict(raw=[P.sb("raw%d" % i, [128, 512], F32) for i in range(3)], sq=[P.sb("sq%d" % i, [128, 512], BF16) for i in range(3)],
                         rs=P.sb("rs", [128, 512], F32), outb=[P.sb("lout%d" % i, [128, 512], BF16) for i in range(5)],
                         cs=P.sb("ropecs", [32, 2, 512], F32), t1=P.sb("kt1", [32, 512], F32), t2=P.sb("kt2", [32, 512], F32), kb=P.sb("kpeb", [32, 512], BF16)) for _ in range(2)]

            def chunk_gen(it):
                ci, (t0, n) = it
                B_ = sets[ci % 2]
                raw, sq, rs, outb, cs, t1, t2, kb = B_["raw"], B_["sq"], B_["rs"], B_["outb"], B_["cs"], B_["t1"], B_["t2"], B_["kb"]
                self.ld(cs, cs[:, :, 0:n], self.c_rope, self.c_rope[:, :, t0:t0 + n].rearrange("c p t -> p c t"))
                for (b0, nb, R) in ((0, 3, 384.0), (3, 2, 256.0)):
                    for b in range(nb):
                        ps = self.nps()
                        for kc in range(8):
                            self.mm(ps, ps[:, 0:n], wl, wl[:, kc, (b0 + b) * 128:(b0 + b + 1) * 128], self.hT, self.hT[:, kc, t0:t0 + n], kc == 0, kc == 7)
                        self.cp("act", raw[b], raw[b][:, 0:n], ps, ps[:, 0:n])
                        self.tt("pool", sq[b], sq[b][:, 0:n], raw[b], raw[b][:, 0:n], raw[b], raw[b][:, 0:n], ALU.mult)
                    yield
                    ps = self.nps()
                    for b in range(nb):
                        self.mm(ps, ps[:, 0:n], self.ones_b, self.ones_b[:], sq[b], sq[b][:, 0:n], b == 0, b == nb - 1)
                    self.act(rs, rs[:, 0:n], ps, ps[:, 0:n], AF.Sqrt, bias=EPS, scale=1.0 / R)
                    yield
                    self.recip(rs, rs[:, 0:n], rs, rs[:, 0:n])
                    for b in range(nb):
                        o = outb[b0 + b]
                        self.stt(o, o[:, 0:n], raw[b], raw[b][:, 0:n], gn[:, b0 + b:b0 + b + 1], rs, rs[:, 0:n], ALU.mult, ALU.mult, ins=[gn])
                        self.ld(self.lat_d, self.lat_d[b0 + b, :, t0:t0 + n], o, o[:, 0:n])
                    yield
                psA = self.nps()
                for kc in range(8):
                    self.mm(psA, psA[0:32, 0:n], wk, wk[:, kc, 0:32], self.hT, self.hT[:, kc, t0:t0 + n], kc == 0, kc == 7)
                for kc in range(8):
                    self.mm(psA, psA[0:32, 512:512 + n], wk, wk[:, kc, 32:64], self.hT, self.hT[:, kc, t0:t0 + n], kc == 0, kc == 7)
                self.tt("dve", t1, t1[:, 0:n], psA, psA[0:32, 0:n], cs, cs[:, 0, 0:n], ALU.mult)
                self.tt("dve", t2, t2[:, 0:n], psA, psA[0:32, 512:512 + n], cs, cs[:, 1, 0:n], ALU.mult)
                yield
                self.tt("pool", kb, kb[:, 0:n], t1, t1[:, 0:n], t2, t2[:, 0:n], ALU.add)
                self.ld(self.kpe_d, self.kpe_d[:, t0:t0 + n], kb, kb[:, 0:n])

            self.run_pipe(list(enumerate(self.tok_chunks())), chunk_gen, max_active=2)

    def phase_fg(self, l):
        with self.S.scope() as P:
            st = [P.sb("fgs%d" % i, [128, 512], BF16) for i in range(3)]
            cnt = [0]

            def epi_f(b, ci, ps, t0, n):
                s = st[cnt[0] % 3]
                cnt[0] += 1
                self.cp("act", s, s[:, 0:n], ps, ps[:, 0:n])
                self.ld(self.uT_d, self.uT_d[b, :, t0:t0 + n], s, s[:, 0:n])

            def epi_g(b, ci, ps, t0, n):
                s = st[cnt[0] % 3]
                cnt[0] += 1
                self.act(s, s[:, 0:n], ps, ps[:, 0:n], AF.Sigmoid)
                self.ld(self.gT_d, self.gT_d[b, :, t0:t0 + n], s, s[:, 0:n])

            self.linear_fm(P, l, O_F, 4, epi_f)
            gch = [c for c in self.tok_chunks() if CTX <= c[0] < CTX + LOC] if l == DEPTH - 1 else None
            self.linear_fm(P, l, O_G, 24, epi_g, chunks=gch)


    def phase_dn(self, l, need_ctx):
        S = self.S
        order = [list(range(NT)), [1, 0] + list(range(NT - 1, NCT - 1, -1))]
        otiles = set(self.out_tiles(l, need_ctx))
        B3 = [128, 4, 128]

        def hb(ap):
            return ap.unsqueeze(2).to_broadcast(B3)

        def mb(ap):
            return ap.unsqueeze(1).to_broadcast(B3)

        with S.scope() as P:
            St = [P.sb("St%d" % d, B3, F32) for d in range(2)]
            Sb = [P.sb("Sb%d" % d, B3, BF16) for d in range(2)]
            for d in range(2):
                S.op("dve", lambda e: e.memset(St[d][:], 0.0), outs=[St[d]])
                S.op("dve", lambda e: e.memset(Sb[d][:], 0.0), outs=[Sb[d]])
            L = {}
            for d in range(2):
                for par in range(2):
                    k = (d, par)
                    L[k] = dict(
                        qt=P.sb("qt", B3, BF16), kt=P.sb("kt", B3, BF16), km=P.sb("km", B3, BF16), vm=P.sb("vm", B3, BF16),
                        sm=P.sb("sm", [128, 20], F32), sm2=P.sb("sm2", [128, 8], F32), gm=P.sb("gm", B3, F32),
                        tD=P.sb("tD", B3, F32), Es=P.sb("Es", B3, F32), EsN=P.sb("EsN", B3, F32), ET=P.sb("ET", B3, F32),
                        tN=P.sb("tN", B3, F32), Na=P.sb("Na", B3, BF16), Nb=P.sb("Nb", B3, BF16), Nta=P.sb("Nta", B3, BF16), Ntb=P.sb("Ntb", B3, BF16),
                        Ra=P.sb("Ra", B3, BF16), Rb=P.sb("Rb", B3, BF16), aT=P.sb("aT", B3, BF16),
                        vb=P.sb("vb", B3, F32), kd=P.sb("kd", B3, BF16), r2t=P.sb("r2t", B3, F32), r2=P.sb("r2", B3, BF16),
                        vn=P.sb("vn", B3, BF16), tS=P.sb("tS", B3, F32), avs=P.sb("avs", B3, F32), to=P.sb("to", B3, F32), o=P.sb("o", B3, F32))
            def lane_step(s, d):
                t = order[d][s]
                b = L[(d, s % 2)]
                want_o = t in otiles
                if l == DEPTH - 1 and d == 0 and t >= NCT + LOCT:
                    return
                qt, kt, km, vm, sm, sm2 = b["qt"], b["kt"], b["km"], b["vm"], b["sm"], b["sm2"]
                self.ld(qt, qt[:], self.qT_d, self.qT_d[:, t, :, :])
                self.ld(kt, kt[:], self.kT_d, self.kT_d[:, t, :, :])
                self.ld(km, km[:], self.ktm_d, self.ktm_d[t])
                self.ld(vm, vm[:], self.vtm_d, self.vtm_d[t])
                g = self.gb[:, t, d * 4:(d + 1) * 4]
                beta = self.gb[:, t, 8 + d * 4:8 + (d + 1) * 4]
                Mle = self.mk(M_ULE if d == 0 else M_LGE)
                Msg = self.mk(M_SGT if d == 0 else M_SLT)
                NEGd = self.mk(M_NEGF if d == 0 else M_NEGB)
                NEGt = self.mk(M_NEGB if d == 0 else M_NEGF)
                psS = self.nps()
                self.mm(psS, psS[:, 0:4], self.masks, Mle, self.gb, g)
                self.mm(psS, psS[:, 4:8], self.ones_f, self.ones_f[:], self.gb, g)
                self.cp("act", sm2, sm2[:], psS, psS[:, 0:8])
                self.act(sm, sm[:, 0:8], sm2, sm2[:], AF.Exp)
                self.tt("dve", sm2, sm2[:, 0:4], sm2, sm2[:, 4:8], sm2, sm2[:, 0:4], ALU.subtract)
                self.act(sm, sm[:, 8:12], sm2, sm2[:, 0:4], AF.Exp)
                self.stt(sm, sm[:, 12:16], sm, sm[:, 0:4], -1.0, self.gb, beta, ALU.mult, ALU.mult)
                self.ts("dve", sm, sm[:, 16:20], sm, sm[:, 0:4], 128.0 ** -0.5, None, ALU.mult)
                yield
                gm = b["gm"]
                self.tt("pool", gm, gm[:], self.masks, mb(Msg), self.gb, hb(g), ALU.mult)
                psD = self.nps()
                for h in range(4):
                    self.mm(psD, psD[:, h * 128:(h + 1) * 128], self.masks, Mle, gm, gm[:, h, :])
                    self.mm(psD, psD[:, 512 + h * 128:512 + (h + 1) * 128], gm, gm[:, h, :], self.masks, Mle)
                tD, Es, EsN, ET = b["tD"], b["Es"], b["EsN"], b["ET"]
                self.tt("dve", tD, tD[:], psD, psD[:, 0:512].rearrange("p (h k) -> p h k", h=4), self.masks, mb(NEGd), ALU.add)
                self.act(Es, Es[:], tD, tD[:], AF.Exp)
                self.tt("pool", EsN, EsN[:], Es, Es[:], self.masks, mb(self.mk(M_OFFD)), ALU.mult)
                self.tt("dve", tD, tD[:], psD, psD[:, 512:1024].rearrange("p (h k) -> p h k", h=4), self.masks, mb(NEGt), ALU.add)
                self.act(ET, ET[:], tD, tD[:], AF.Exp)
                yield
                psG = self.nps()
                for h in range(4):
                    self.mm(psG, psG[:, h * 128:(h + 1) * 128], kt, kt[:, h, :], kt, kt[:, h, :])
                    self.mm(psG, psG[:, 512 + h * 128:512 + (h + 1) * 128], kt, kt[:, h, :], qt, qt[:, h, :])
                tN, N, Nt, R, aT = b["tN"], b["Na"], b["Nta"], b["Ra"], b["aT"]
                N2, Nt2, R2 = b["Nb"], b["Ntb"], b["Rb"]
                self.tt("dve", tN, tN[:], psG, psG[:, 0:512].rearrange("p (h k) -> p h k", h=4), self.gb, hb(beta), ALU.mult)
                self.tt("pool", N, N[:], tN, tN[:], EsN, EsN[:], ALU.mult)
                self.stt(aT, aT[:], psG, psG[:, 512:1024].rearrange("p (h k) -> p h k", h=4), 128.0 ** -0.5, ET, ET[:], ALU.mult, ALU.mult)
                yield
                pb = self.npb()
                for h in range(4):
                    self.tr(pb, pb[:, h * 128:(h + 1) * 128], N, N[:, h, :], self.identb[:])
                self.cp("act", Nt, Nt[:], pb, pb[:, 0:512].rearrange("p (h k) -> p h k", h=4))
                self.tt("pool", R, R[:], Nt, Nt[:], self.identb, mb(self.identb[:]), ALU.add)
                for lev in range(6):
                    yield
                    ps1 = self.nps()
                    for h in range(4):
                        self.mm(ps1, ps1[:, h * 128:(h + 1) * 128], Nt, Nt[:, h, :], N, N[:, h, :])
                        if lev < 5:
                            self.mm(ps1, ps1[:, 512 + h * 128:512 + (h + 1) * 128], N, N[:, h, :], Nt, Nt[:, h, :])
                    self.cp("act", N2, N2[:], ps1, ps1[:, 0:512].rearrange("p (h k) -> p h k", h=4))
                    if lev < 5:
                        self.cp("dve", Nt2, Nt2[:], ps1, ps1[:, 512:1024].rearrange("p (h k) -> p h k", h=4))
                    yield
                    ps2 = self.nps()
                    for h in range(4):
                        self.mm(ps2, ps2[:, h * 128:(h + 1) * 128], N2, N2[:, h, :], R, R[:, h, :])
                    self.tt("dve", R2, R2[:], ps2, ps2[:, 0:512].rearrange("p (h k) -> p h k", h=4), R, R[:], ALU.add)
                    N, N2 = N2, N
                    Nt, Nt2 = Nt2, Nt
                    R, R2 = R2, R
                b["Rfin"] = R

            def scan_step(s, d):
                t = order[d][s]
                b = L[(d, s % 2)]
                want_o = t in otiles
                if l == DEPTH - 1 and d == 0 and t >= NCT + LOCT:
                    return
                qt, kt, km, vm, sm, aT, R = b["qt"], b["kt"], b["km"], b["vm"], b["sm"], b["aT"], b["Rfin"]
                beta = self.gb[:, t, 8 + d * 4:8 + (d + 1) * 4]
                vb, kd, r2t, r2, vn, tS, avs, to, o = b["vb"], b["kd"], b["r2t"], b["r2"], b["vn"], b["tS"], b["avs"], b["to"], b["o"]
                self.tt("pool", vb, vb[:], vm, vm[:], self.gb, hb(beta), ALU.mult)
                self.tt("pool", kd, kd[:], km, km[:], sm, hb(sm[:, 8:12]), ALU.mult)
                psK = self.nps()
                for h in range(4):
                    self.mm(psK, psK[:, h * 128:(h + 1) * 128], kt, kt[:, h, :], Sb[d], Sb[d][:, h, :])
                    if want_o:
                        self.mm(psK, psK[:, 512 + h * 128:512 + (h + 1) * 128], qt, qt[:, h, :], Sb[d], Sb[d][:, h, :])
                self.tt("dve", r2t, r2t[:], psK, psK[:, 0:512].rearrange("p (h k) -> p h k", h=4), sm, hb(sm[:, 12:16]), ALU.mult)
                if want_o:
                    self.tt("dve", to, to[:], psK, psK[:, 512:1024].rearrange("p (h k) -> p h k", h=4), sm, hb(sm[:, 16:20]), ALU.mult)
                self.tt("pool", r2, r2[:], r2t, r2t[:], vb, vb[:], ALU.add)
                yield
                psV = self.nps()
                for h in range(4):
                    self.mm(psV, psV[:, h * 128:(h + 1) * 128], R, R[:, h, :], r2, r2[:, h, :])
                self.cp("act", vn, vn[:], psV, psV[:, 0:512].rearrange("p (h k) -> p h k", h=4))
                yield
                psU = self.nps()
                for h in range(4):
                    self.mm(psU, psU[:, h * 128:(h + 1) * 128], kd, kd[:, h, :], vn, vn[:, h, :])
                    if want_o:
                        self.mm(psU, psU[:, 512 + h * 128:512 + (h + 1) * 128], aT, aT[:, h, :], vn, vn[:, h, :])
                self.tt("pool", tS, tS[:], St[d], St[d][:], sm, hb(sm[:, 4:8]), ALU.mult)
                self.tt("dve", St[d], St[d][:], tS, tS[:], psU, psU[:, 0:512].rearrange("p (h k) -> p h k", h=4), ALU.add)
                self.cp("act", Sb[d], Sb[d][:], St[d], St[d][:])
                if want_o:
                    self.cp("act", avs, avs[:], psU, psU[:, 512:1024].rearrange("p (h k) -> p h k", h=4))
                    self.tt("pool", o, o[:], to, to[:], avs, avs[:], ALU.add)
                    self.ld(self.o_d[d], self.o_d[d][t * 128:(t + 1) * 128, :], o, o[:].rearrange("p h k -> p (h k)"))

            for s in range(NT + 1):
                gens = ([scan_step(s - 1, 0), scan_step(s - 1, 1)] if s > 0 else []) + ([lane_step(s, 0), lane_step(s, 1)] if s < NT else [])
                while gens:
                    for g_ in list(gens):
                        try:
                            next(g_)
                        except StopIteration:
                            gens.remove(g_)

    def phase_dnout(self, l, need_ctx):
        S = self.S
        tiles = self.out_tiles(l, need_ctx)
        with S.scope() as P:
            gn = P.sb("dng", [128, 128], F32)
            self.ld(gn, gn[:], self.dn_norm, self.dn_norm[l].partition_broadcast(128))
            bufs = [dict(of=P.sb("of", [128, 512], F32), ob=P.sb("ob", [128, 512], F32), z=P.sb("z", [128, 512], BF16), sq=P.sb("sq", [128, 512], F32),
                         ss=P.sb("ss", [128, 4], F32), gz=P.sb("gz", [128, 512], F32), y=P.sb("y", [128, 512], BF16), yT=P.sb("yT", [128, 4, 128], BF16)) for _ in range(10)]
            def tile_gen(it):
                i, t = it
                b = bufs[i % 10]
                of, ob, z, sq, ss, gz, y, yT = b["of"], b["ob"], b["z"], b["sq"], b["ss"], b["gz"], b["y"], b["yT"]
                rows = slice(t * 128, (t + 1) * 128)
                self.ld(of, of[:], self.o_d[0], self.o_d[0][rows, :])
                self.ld(ob, ob[:], self.o_d[1], self.o_d[1][rows, :])
                self.ld(z, z[:], self.z_d, self.z_d[rows, :])
                yield
                self.tt("pool", of, of[:], of, of[:], ob, ob[:], ALU.add)
                self.tt("pool", gz, gz[:].rearrange("p (h k) -> p h k", h=4), z, z[:].rearrange("p (h k) -> p h k", h=4), gn, gn[:].unsqueeze(1).to_broadcast([128, 4, 128]), ALU.mult)
                yield
                self.act(sq, sq[:], of, of[:], AF.Square)
                yield
                self.S.op("dve", lambda e: e.tensor_reduce(ss[:], sq[:].rearrange("p (h k) -> p h k", h=4), mybir.AxisListType.X, ALU.add), outs=[ss], ins=[sq])
                yield
                self.act(ss, ss[:], ss, ss[:], AF.Sqrt, bias=EPS, scale=1.0 / 128)
                yield
                self.recip(ss, ss[:], ss, ss[:])
                self.tt("dve", sq, sq[:].rearrange("p (h k) -> p h k", h=4), of, of[:].rearrange("p (h k) -> p h k", h=4), ss, ss[:].unsqueeze(2).to_broadcast([128, 4, 128]), ALU.mult)
                yield
                self.tt("pool", y, y[:], sq, sq[:], gz, gz[:], ALU.mult)
                yield
                pb = self.npb()
                for c in range(4):
                    self.tr(pb, pb[:, c * 128:(c + 1) * 128], y, y[:, c * 128:(c + 1) * 128], self.identb[:])
                yield
                self.cp("act", yT, yT[:], pb, pb[:, 0:512].rearrange("p (c k) -> p c k", c=4))
                self.ld(self.yT_d, self.yT_d[0, :, :, rows].rearrange("c p k -> p c k"), yT, yT[:])

            self.run_pipe(list(enumerate(tiles)), tile_gen)

    def phase_mla(self, l, need_ctx):
        S = self.S
        SCALE = 96.0 ** -0.5
        with S.scope() as P:
            lat = [P.sb("lat%d" % i, [128, TALL], BF16) for i in range(5)]
            for i in range(5):
                self.ld(lat[i], lat[i][:], self.lat_d, self.lat_d[i])
            KT = [P.sb("KT%d" % h, [96, TALL], BF16) for h in range(8)]
            VP = P.sb("VP", [128, NT, 512], BF16)
            wq = P.sb("wq", [128, 3, 768], BF16)
            wqs = P.sb("wqs", [128, 3, 768], BF16)
            wkv = P.sb("wkv", [128, 2, 1024], BF16)
            qv = self.mla_w_qup[l].rearrange("(rc p) n -> p rc n", p=128)
            self.S.dma("pool", wq, wq[:], self.mla_w_qup, qv)
            self.S.dma("pool", wqs, wqs[:], self.mla_w_qup, qv)
            q4 = self.mla_w_qup[l].rearrange("(rc p) (h d) -> p rc h d", p=128, d=96)
            w4 = wqs[:].rearrange("p rc (h d) -> p rc h d", d=96)
            for (d0, s0) in ((0, 8), (8, 0), (16, 24), (24, 16)):
                for rc in range(3):
                    self.S.dma("pool", wqs, w4[:, rc, :, 64 + d0:64 + d0 + 8], self.mla_w_qup, q4[:, rc, :, 64 + s0:64 + s0 + 8])
            self.S.dma("pool", wkv, wkv[:], self.mla_w_kvup, self.mla_w_kvup[l].rearrange("(rc p) n -> p rc n", p=128))
            for h in range(8):
                self.ld(KT[h], KT[h][64:96, :], self.kpe_d, self.kpe_d[:, :])
            for (t0, n) in self.tok_chunks():
                for h in range(8):
                    ps = self.nps()
                    for rc in range(2):
                        self.mm(ps, ps[0:64, 0:n], wkv, wkv[:, rc, h * 128:h * 128 + 64], lat[3 + rc], lat[3 + rc][:, t0:t0 + n], rc == 0, rc == 1)
                    self.cp("act" if h % 2 else "dve", KT[h], KT[h][0:64, t0:t0 + n], ps, ps[0:64, 0:n])
            wv4 = wkv[:].rearrange("p rc (h d) -> p rc h d", d=128)
            for t in range(NT):
                ps = self.nps()
                for rc in range(2):
                    self.mm(ps, ps[:, 0:512].rearrange("p (h d) -> p h d", d=64), lat[3 + rc], lat[3 + rc][:, t * 128:(t + 1) * 128], wkv, wv4[:, rc, :, 64:128], rc == 0, rc == 1)
                self.cp("act" if t % 2 else "dve", VP, VP[:, t, :], ps, ps[:, 0:512])
            self.S.barrier()
            H = [Obj("psh%d" % i, self.PS[i // 2].h[:, (i % 2) * 512:(i % 2 + 1) * 512], True) for i in range(6)]
            psOo, psOd = H[0], H[1]
            QP = [Obj("psq%d" % i, self.PB[i].h[:].bitcast(F32), True) for i in range(2)]
            rot = H[2:6]
            ri = [0]

            def nh():
                p = rot[ri[0] % 4]
                ri[0] += 1
                return p
            css = [P.sb("qcs%d" % i, [96, 2, 512], F32) for i in range(2)]
            QT = [P.sb("QT%d" % i, [96, 512], BF16) for i in range(2)]
            t1s = [P.sb("qt1_%d" % i, [96, 512], F32) for i in range(2)]
            t2s = [P.sb("qt2_%d" % i, [96, 512], F32) for i in range(2)]
            PT = [P.sb("PT%d" % i, [128, 512], BF16) for i in range(3)]
            rec = P.sb("rec", [128, 512], F32)
            yb = [P.sb("yb%d" % i, [128, 512], BF16) for i in range(2)]
            nq = (LOC if l == DEPTH - 1 else SEQ) // 512
            chunks = ([(0, CTX, list(range(NCT)))] if need_ctx else []) + [(CTX + i * 512, 512, list(range(NT))) for i in range(nq)]
            items = [(ci, t0, n, ktiles, c, j) for ci, (t0, n, ktiles) in enumerate(chunks) for c in range(4) for j in range(2)]

            def build_q(k):
                ci, t0, n, ktiles, c, j = items[k]
                cs = css[ci % 2]
                if c == 0 and j == 0:
                    self.ld(cs, cs[64:96, :, 0:n], self.c_rope, self.c_rope[:, :, t0:t0 + n].rearrange("c p t -> p c t"))
                h = 2 * c + j
                q, t1, t2 = QT[k % 2], t1s[k % 2], t2s[k % 2]
                pA, pB = QP
                for rc in range(3):
                    self.mm(pA, pA[0:96, 0:n], wq, wq[:, rc, h * 96:(h + 1) * 96], lat[rc], lat[rc][:, t0:t0 + n], rc == 0, rc == 2)
                for rc in range(3):
                    self.mm(pB, pB[0:96, 0:n], wqs, wqs[:, rc, h * 96:(h + 1) * 96], lat[rc], lat[rc][:, t0:t0 + n], rc == 0, rc == 2)
                self.cp("act", q, q[0:64, 0:n], pA, pA[0:64, 0:n])
                self.tt("dve", t1, t1[64:96, 0:n], pA, pA[64:96, 0:n], cs, cs[64:96, 0, 0:n], ALU.mult)
                self.tt("dve", t2, t2[64:96, 0:n], pB, pB[64:96, 0:n], cs, cs[64:96, 1, 0:n], ALU.mult)
                self.tt("pool", q, q[64:96, 0:n], t1, t1[64:96, 0:n], t2, t2[64:96, 0:n], ALU.add)

            build_q(0)
            for k, (ci, t0, n, ktiles, c, j) in enumerate(items):
                h = 2 * c + j
                q = QT[k % 2]
                y = yb[c % 2]
                sts = {}

                def score(i):
                    kt = ktiles[i]
                    p = nh()
                    self.mm(p, p[:, 0:n], KT[h], KT[h][:, kt * 128:(kt + 1) * 128], q, q[:, 0:n])
                    sts[i] = p
                AHEAD = 3
                for i in range(min(AHEAD, len(ktiles))):
                    score(i)
                if k + 1 < len(items):
                    build_q(k + 1)
                for i, kt in enumerate(ktiles):
                    if i + AHEAD < len(ktiles):
                        score(i + AHEAD)
                    p = sts.pop(i)
                    pt = PT[i % 3]
                    self.act(pt, pt[:, 0:n], p, p[:, 0:n], AF.Exp, scale=SCALE)
                    last = i == len(ktiles) - 1
                    self.mm(psOo, psOo[:, 0:n], VP, VP[:, kt, c * 128:(c + 1) * 128], pt, pt[:, 0:n], i == 0, last, inc=last)
                    self.mm(psOd, psOd[:, 0:n], self.ones_b, self.ones_b[:], pt, pt[:, 0:n], i == 0, last, inc=True)
                r0 = j * 64
                self.recip(rec, rec[r0:r0 + 64, 0:n], psOd, psOd[r0:r0 + 64, 0:n])
                self.tt("dve", y, y[r0:r0 + 64, 0:n], psOo, psOo[r0:r0 + 64, 0:n], rec, rec[r0:r0 + 64, 0:n], ALU.mult)
                if j == 1:
                    self.ld(self.yT_d, self.yT_d[1, c, :, t0:t0 + n], y, y[:, 0:n])

    def phase_fft(self, l, need_ctx):
        S = self.S
        with S.scope() as P:
            dc = P.sb("dftc", [128, 256], BF16)
            self.ld(dc, dc[:], self.c_dftc, self.c_dftc[:])
            AB = P.sb("AB", [128, NT, 1024], BF16)
            ut = [P.sb("ut%d" % i, [128, 4, 128], BF16) for i in range(2)]
            tiles = list(range(0 if need_ctx else NCT, NT))
            for i, t in enumerate(tiles):
                u = ut[i % 2]
                self.ld(u, u[:], self.uT_d, self.uT_d[:, :, t * 128:(t + 1) * 128].rearrange("g p k -> p g k"))
                ps = self.nps()
                for g in range(4):
                    self.mm(ps, ps[:, g * 256:(g + 1) * 256], u, u[:, g, :], dc, dc[:])
                self.cp("act" if i % 2 else "dve", AB, AB[:, t, :], ps, ps[:])
            self.S.barrier()
            H = [Obj("fph%d" % i, self.PS[i // 2].h[:, (i % 2) * 512:(i % 2 + 1) * 512], True) for i in range(6)]
            DB = [P.sb("dft%d" % i, [128, 32, 512], BF16) for i in range(3)]
            yc = [P.sb("yc%d" % i, [128, 512], BF16) for i in range(2)]
            nkc = (LOC if l == DEPTH - 1 else SEQ) // 512
            jobs = ([("c", 0, 0, NCT, 0, 256)] if need_ctx else []) + [("x", kc, NCT, SEQ // 128, CTX, 512) for kc in range(nkc)]
            passes = [(ji, pi) for ji in range(len(jobs)) for pi in range(2)]

            def load(p):
                ji, pi = passes[p]
                nm, kc, tb, ntt, tok0, w = jobs[ji]
                Mb = DB[p % 3]
                src = self.c_dft[("C" if pi == 0 else "S", nm)]
                self.ld(Mb, Mb[:, 0:ntt, 0:w], src, src[kc])

            oi = 0
            load(0)
            accs = None
            for p, (ji, pi) in enumerate(passes):
                nm, kc, tb, ntt, tok0, w = jobs[ji]
                if p + 1 < len(passes):
                    load(p + 1)
                if pi == 0:
                    accs = [H[(4 * ji + g) % 6] for g in range(4)]
                Mb = DB[p % 3]
                for tt in range(ntt):
                    for g in range(4):
                        o0 = g * 256 + (0 if pi == 0 else 128)
                        self.mm(accs[g], accs[g][:, 0:w], AB, AB[:, tb + tt, o0:o0 + 128], Mb, Mb[:, tt, 0:w], pi == 0 and tt == 0, pi == 1 and tt == ntt - 1)
                if pi == 1:
                    for g in range(4):
                        y = yc[oi % 2]
                        oi += 1
                        self.cp("act" if oi % 2 else "dve", y, y[:, 0:w], accs[g], accs[g][:, 0:w])
                        self.ld(self.yT_d, self.yT_d[2, g, :, tok0 + kc * w:tok0 + (kc + 1) * w], y, y[:, 0:w])

    def phase_merge(self, l, need_ctx):
        S = self.S
        with S.scope() as P:
            wb = P.sb("wb", [128, 12, D], BF16)
            self.S.dma("pool", wb, wb[:], self.w_branch, self.w_branch[l].rearrange("n (cc p) d -> p (n cc) d", p=128))
            wo = P.sb("wo", [128, 8, D], BF16)
            self.S.dma("pool", wo, wo[:], self.w_out, self.w_out[l].rearrange("(kc p) d -> p kc d", p=128))
            GT = {r: self.load_bc(P, l, r, 2) for r in ([0, 1] if need_ctx else [0])}
            yT = [P.sb("myT%d" % i, [128, 12, 512], BF16) for i in range(2)]
            gT = [P.sb("mgT%d" % i, [128, 24, 512], BF16) for i in range(2)]
            mT = [P.sb("mmT%d" % i, [128, 8, 512], BF16) for i in range(2)]
            tas = [[[P.sb("mta%d_%d_%d" % (c_, k, i), [128, 512], F32) for i in range(3)] for k in range(2)] for c_ in range(2)]
            xt = [P.sb("mxt%d" % i, [128, D], F32) for i in range(4)]
            tx = [P.sb("mtx%d" % i, [128, D], F32) for i in range(4)]
            chunks = [c for c in self.tok_chunks() if need_ctx or c[0] >= CTX]
            if l == DEPTH - 1:
                chunks = [c for c in chunks if CTX <= c[0] < CTX + LOC]
            def chunk_gen(it):
                ci, (t0, n) = it
                r = 1 if t0 < CTX else 0
                y, g, m = yT[ci % 2], gT[ci % 2], mT[ci % 2]
                self.ld(y, y[:, :, 0:n], self.yT_d, self.yT_d[:, :, :, t0:t0 + n].rearrange("n c p k -> p (n c) k"))
                self.ld(g, g[:, :, 0:n], self.gT_d, self.gT_d[:, :, t0:t0 + n].rearrange("b p k -> p b k"))
                yield
                for db in range(8):
                    ta = tas[ci % 2][db % 2]
                    for nb in range(3):
                        ps = self.nps()
                        for cc in range(4):
                            self.mm(ps, ps[:, 0:n], wb, wb[:, nb * 4 + cc, db * 128:(db + 1) * 128], y, y[:, nb * 4 + cc, 0:n], cc == 0, cc == 3)
                        self.tt("dve", ta[nb], ta[nb][:, 0:n], ps, ps[:, 0:n], g, g[:, nb * 8 + db, 0:n], ALU.mult)
                    self.tt("pool", ta[0], ta[0][:, 0:n], ta[0], ta[0][:, 0:n], ta[1], ta[1][:, 0:n], ALU.add)
                    self.tt("pool", m, m[:, db, 0:n], ta[0], ta[0][:, 0:n], ta[2], ta[2][:, 0:n], ALU.add)
                    yield
                for tl in range(n // 128):
                    t = t0 // 128 + tl
                    x, txx = xt[(ci % 2) * 2 + tl % 2], tx[(ci % 2) * 2 + tl % 2]
                    so, sap = self.xsrc(l, t)
                    self.ld(x, x[:], so, sap)
                    ps = self.nps()
                    for nh in range(2):
                        for db in range(8):
                            self.mm(ps, ps[:, nh * 512:(nh + 1) * 512], m, m[:, db, tl * 128:(tl + 1) * 128], wo, wo[:, db, nh * 512:(nh + 1) * 512], db == 0, db == 7)
                    self.tt("dve", txx, txx[:], ps, ps[:], GT[r], GT[r][:], ALU.mult)
                    self.tt("pool", txx, txx[:], txx, txx[:], x, x[:], ALU.add)
                    self.ld(self.xres, self.xres[t * 128:(t + 1) * 128, :], txx, txx[:])
                    yield

            self.run_pipe(list(enumerate(chunks)), chunk_gen, max_active=2)

    def phase_ffn(self, l):
        S = self.S
        moe = l % 2 == 1
        ei = l // 2
        last = l == DEPTH - 1
        NE, FF = (NEXP, EFF) if moe else (1, DFF)
        nfb = FF // 128
        nun = (nfb + 6) // 7
        units = []
        f_ = 0
        for i in range(nun):
            k_ = (nfb - f_ + (nun - i) - 1) // (nun - i)
            units.append((f_, k_))
            f_ += k_
        UMAX = max(u[1] for u in units)
        tiles_all = list(range(NCT, NCT + LOCT)) if last else list(range(NT))
        CH = 8 if moe else 12
        for c0 in range(0, len(tiles_all), CH):
            tiles = tiles_all[c0:c0 + CH]
            ntl = len(tiles)
            ntok = ntl * 128
            with S.scope() as P:
                hT = P.sb("fhT", [128, 8, CH * 128], BF16)
                acc = P.sb("facc", [128, CH, D], F32)
                comb = P.sb("comb", [128, CH, NEXP], F32)
                conds = sorted(set(1 if t < NCT else 0 for t in tiles))
                GT = {r: self.load_bc(P, l, r, 5) for r in conds}
                if moe:
                    wr = P.sb("wr", [128, 8, NEXP], F32)
                    self.ld(wr, wr[:], self.moe_router, self.moe_router[ei].rearrange("(kc p) e -> p kc e", p=128))
                    fT = P.sb("fT", [128, 8, 128], F32)
                    rt = P.sb("rt", [128, 8 * NEXP], F32)

                    def router(i, t, f):
                        ps = self.nps()
                        for kc in range(8):
                            self.S.op("pe", lambda e: e.transpose(ps[:, kc * 128:(kc + 1) * 128], f[:, kc * 128:(kc + 1) * 128], self.mk(M_ID)), outs=[ps], ins=[f, self.masks])
                        self.cp("act", fT, fT[:], ps, ps[:].rearrange("p (k t) -> p k t", k=8))
                        pl = self.nps()
                        for kc in range(8):
                            self.mm(pl, pl[:, 0:NEXP], fT, fT[:, kc, :], wr, wr[:, kc, :], kc == 0, kc == 7)
                        lg, m1, eq, l2, m2, sel, ex, sw = (rt[:, k * 8:(k + 1) * 8] for k in range(8))
                        self.cp("act", rt, lg, pl, pl[:, 0:NEXP])
                        self.S.op("dve", lambda e: e.tensor_reduce(m1[:, 0:1], lg, mybir.AxisListType.X, ALU.max), outs=[rt], ins=[rt])
                        self.ts("dve", rt, eq, rt, lg, m1[:, 0:1], None, ALU.is_equal)
                        self.stt(rt, l2, rt, eq, -1e30, rt, lg, ALU.mult, ALU.add)
                        self.S.op("dve", lambda e: e.tensor_reduce(m2[:, 0:1], l2, mybir.AxisListType.X, ALU.max), outs=[rt], ins=[rt])
                        self.ts("dve", rt, sel, rt, lg, m2[:, 0:1], None, ALU.is_ge)
                        self.ts("dve", rt, m1[:, 1:2], rt, m1[:, 0:1], -1.0, None, ALU.mult)
                        self.act(rt, ex, rt, lg, AF.Exp, bias=m1[:, 1:2], scale=1.0)
                        self.tt("dve", rt, ex, rt, ex, rt, sel, ALU.mult)
                        self.S.op("dve", lambda e: e.tensor_reduce(sw[:, 0:1], ex, mybir.AxisListType.X, ALU.add), outs=[rt], ins=[rt])
                        self.recip(rt, sw[:, 1:2], rt, sw[:, 0:1])
                        self.ts("dve", comb, comb[:, i, :], rt, ex, sw[:, 1:2], None, ALU.mult, ins=[rt])
                else:
                    router = None
                self.phase_norm(l, "ffn", tiles, hT, 0, router=router)
                tchunks = [(o, min(512, ntok - o)) for o in range(0, ntok, 512)]
                with S.scope() as Q:
                    actTs = [Q.sb("actT%d" % i, [128, UMAX, CH * 128], BF16) for i in range(2)]
                    wg = [Q.sb("wg%d" % i, [128, 8, 512], BF16) for i in range(2)]
                    wu = [Q.sb("wu%d" % i, [128, 8, 512], BF16) for i in range(2)]
                    wds = [Q.sb("wd%d" % i, [128, UMAX, D], BF16) for i in range(2)]
                    ui = 0
                    sg = [Q.sb("sg%d" % i, [128, 512], F32) for i in range(2)]
                    gi = 0
                    si = 0
                    first = True
                    for e in range(NE):
                        if moe:
                            Wg, Wu, Wd = self.moe_w_gate, self.moe_w_up, self.moe_w_down
                            wgv = Wg[ei, e].rearrange("(kc p) f -> p kc f", p=128)
                            wuv = Wu[ei, e].rearrange("(kc p) f -> p kc f", p=128)
                            wdv = Wd[ei, e]
                        else:
                            Wg, Wu, Wd = self.ffn_w_gate, self.ffn_w_up, self.ffn_w_down
                            wgv = Wg[ei].rearrange("(kc p) f -> p kc f", p=128)
                            wuv = Wu[ei].rearrange("(kc p) f -> p kc f", p=128)
                            wdv = Wd[ei]
                        for (fb0, nfbu) in units:
                            actT, wd = actTs[ui % 2], wds[ui % 2]
                            ui += 1
                            self.S.dma("pool", wd, wd[:, 0:nfbu, :], Wd, wdv[fb0 * 128:(fb0 + nfbu) * 128, :].rearrange("(fb p) d -> p fb d", p=128))
                            for g0 in range(0, nfbu, 4):
                                nb = min(4, nfbu - g0)
                                g_, u_ = wg[gi % 2], wu[gi % 2]
                                gi += 1
                                f0 = (fb0 + g0) * 128
                                self.S.dma("pool", g_, g_[:, :, 0:nb * 128], Wg, wgv[:, :, f0:f0 + nb * 128])
                                self.S.dma("pool", u_, u_[:, :, 0:nb * 128], Wu, wuv[:, :, f0:f0 + nb * 128])
                                for b in range(nb):
                                    for (o, n) in tchunks:
                                        psg = self.nps()
                                        for kc in range(8):
                                            self.mm(psg, psg[:, 0:n], g_, g_[:, kc, b * 128:(b + 1) * 128], hT, hT[:, kc, o:o + n], kc == 0, kc == 7)
                                        for kc in range(8):
                                            self.mm(psg, psg[:, 512:512 + n], u_, u_[:, kc, b * 128:(b + 1) * 128], hT, hT[:, kc, o:o + n], kc == 0, kc == 7)
                                        s_ = sg[si % 2]
                                        si += 1
                                        self.act(s_, s_[:, 0:n], psg, psg[:, 0:n], AF.Silu)
                                        self.tt("dve", actT, actT[:, g0 + b, o:o + n], s_, s_[:, 0:n], psg, psg[:, 512:512 + n], ALU.mult)
                            for i in range(ntl):
                                ps = self.nps()
                                for nh in range(2):
                                    for fbi in range(nfbu):
                                        self.mm(ps, ps[:, nh * 512:(nh + 1) * 512], actT, actT[:, fbi, i * 128:(i + 1) * 128], wd, wd[:, fbi, nh * 512:(nh + 1) * 512], fbi == 0, fbi == nfbu - 1)
                                if moe:
                                    if first:
                                        self.ts("dve", acc, acc[:, i, :], ps, ps[:], comb[:, i, e:e + 1], None, ALU.mult, ins=[comb])
                                    else:
                                        self.stt(acc, acc[:, i, :], ps, ps[:], comb[:, i, e:e + 1], acc, acc[:, i, :], ALU.mult, ALU.add, ins=[comb])
                                else:
                                    if first:
                                        self.cp("act", acc, acc[:, i, :], ps, ps[:])
                                    else:
                                        self.tt("dve", acc, acc[:, i, :], acc, acc[:, i, :], ps, ps[:], ALU.add)
                            first = False
                with S.scope() as Q:
                    xt = [Q.sb("rxt%d" % i, [128, D], F32) for i in range(2)]
                    tx = [Q.sb("rtx%d" % i, [128, D], F32) for i in range(2)]
                    if last:
                        fg = Q.sb("fgain", [128, D], F32)
                        self.ld(fg, fg[:], self.final_norm, self.final_norm[0].partition_broadcast(128))
                        sq = Q.sb("fsq", [128, D], F32)
                        ss = [Q.sb("fss%d" % i, [128, 1], F32) for i in range(2)]
                    for i, t in enumerate(tiles):
                        r = 1 if t < NCT else 0
                        x, y = xt[i % 2], tx[i % 2]
                        self.ld(x, x[:], self.xres, self.xres[t * 128:(t + 1) * 128, :])
                        self.tt("pool", y, y[:], acc, acc[:, i, :], GT[r], GT[r][:], ALU.mult)
                        self.tt("dve", y, y[:], y, y[:], x, x[:], ALU.add)
                        if not last:
                            self.ld(self.xres, self.xres[t * 128:(t + 1) * 128, :], y, y[:])
                        else:
                            s_ = ss[i % 2]
                            self.act(sq, sq[:], y, y[:], AF.Square)
                            self.S.op("dve", lambda e: e.tensor_reduce(s_[:], sq[:], mybir.AxisListType.X, ALU.add), outs=[s_], ins=[sq])
                            self.act(s_, s_[:], s_, s_[:], AF.Sqrt, bias=EPS, scale=1.0 / D)
                            self.recip(s_, s_[:], s_, s_[:])
                            self.stt(x, x[:], y, y[:], s_[:, 0:1], fg, fg[:], ALU.mult, ALU.mult, ins=[s_])
                            self.ld(self.y_out, self.y_out[(t - NCT) * 128:(t - NCT + 1) * 128, :], x, x[:])

_CONSTS = {}


def make_in_maps(inputs, n_cores=8):
    f = lambda a: np.ascontiguousarray(np.asarray(a, dtype=np.float32))
    shared = {
        "w_mod": f(inputs["w_mod"]), "b_mod": f(inputs["b_mod"]), "norm_mix": f(inputs["norm_mix"]), "norm_ffn": f(inputs["norm_ffn"]),
        "dn_norm": f(inputs["dn_norm"]),
        "lat_gain": f(np.concatenate([np.asarray(inputs["mla_q_norm"]).reshape(DEPTH, 3, 128), np.asarray(inputs["mla_kv_norm"]).reshape(DEPTH, 2, 128)], 1).transpose(0, 2, 1)),
        "mla_w_qup": f(inputs["mla_w_qup"]), "mla_w_kvup": f(inputs["mla_w_kvup"]), "w_branch": f(inputs["w_branch"]), "w_out": f(inputs["w_out"]),
        "ffn_w_gate": f(inputs["ffn_w_gate"]), "ffn_w_up": f(inputs["ffn_w_up"]), "ffn_w_down": f(inputs["ffn_w_down"]),
        "moe_router": f(inputs["moe_router"]),
        "moe_w_gate": f(inputs["moe_w_gate"]), "moe_w_up": f(inputs["moe_w_up"]), "moe_w_down": f(inputs["moe_w_down"]),
        "final_norm": f(np.asarray(inputs["final_norm"]).reshape(1, D)),
    }
    w_in = np.asarray(inputs["w_in"], np.float32)
    conv = np.asarray(inputs["dn_conv"], np.float32)
    alog = np.asarray(inputs["dn_a_log"], np.float32)
    dtb = np.asarray(inputs["dn_dt_bias"], np.float32)
    per = {}
    for flip in (False, True):
        d = {}
        if flip:
            perm = np.arange(INW)
            perm[O_A:O_A + 8] = np.r_[O_A + 4:O_A + 8, O_A:O_A + 4]
            perm[O_B:O_B + 8] = np.r_[O_B + 4:O_B + 8, O_B:O_B + 4]
            d["w_in"] = np.ascontiguousarray(w_in[:, :, perm])
            d["dn_convT"] = f(np.transpose(conv[:, ::-1, :], (0, 2, 1)))
            d["dn_a_log"] = f(alog[:, ::-1, :].reshape(DEPTH, 8))
            d["dn_dt_bias"] = f(dtb[:, ::-1, :].reshape(DEPTH, 8))
        else:
            d["w_in"] = f(w_in)
            d["dn_convT"] = f(np.transpose(conv, (0, 2, 1)))
            d["dn_a_log"] = f(alog.reshape(DEPTH, 8))
            d["dn_dt_bias"] = f(dtb.reshape(DEPTH, 8))
        if flip not in _CONSTS:
            _CONSTS[flip] = host_consts(flip)
        d.update(_CONSTS[flip])
        per[flip] = d
    maps = []
    x = np.asarray(inputs["x"], np.float32)
    c = np.asarray(inputs["c"], np.float32)
    ctx = np.asarray(inputs["ctx"], np.float32)
    cc = np.asarray(inputs["c_ctx"], np.float32)
    for core in range(n_cores):
        b = core % 4
        flip = core >= 4
        m = dict(shared)
        m.update(per[flip])
        m["x_in"] = np.ascontiguousarray(x[b][::-1] if flip else x[b])
        m["ctx_in"] = np.ascontiguousarray(ctx[b][::-1] if flip else ctx[b])
        c2 = np.stack([c[b], cc], 0)
        m["cT"] = np.ascontiguousarray(c2.reshape(2, 8, 128).transpose(2, 1, 0).reshape(128, 16))
        maps.append(m)
    return maps


_NC = None


def kernel(**inputs):
    global _NC
    if _NC is None:
        _NC = Builder().build()
    maps = make_in_maps(inputs, 8)
    res = run_bass_kernel_spmd(_NC, maps, core_ids=list(range(8)))
    out = np.empty((4, SEQ, D), np.float32)
    for core in range(8):
        y = np.asarray(res.results[core]["y_out"], np.float32)
        if core < 4:
            out[core, :LOC] = y
        else:
            out[core - 4, LOC:] = y[::-1]
    return out
```

```python
from contextlib import ExitStack, contextmanager
import numpy as np
import ml_dtypes
import concourse.bass as bass
import concourse.mybir as mybir
from concourse.bass_utils import run_bass_kernel_spmd

F32 = mybir.dt.float32
BF16 = mybir.dt.bfloat16
ALU = mybir.AluOpType
AF = mybir.ActivationFunctionType

D = 1024
SEQ = 4096
CTX = 256
TALL = SEQ + CTX
NT = TALL // 128
NCT = CTX // 128
DEPTH = 2
INW = 6320
O_Q, O_K, O_V, O_Z, O_A, O_B, O_CQ, O_CKV, O_KPE, O_F, O_G = 0, 512, 1024, 1536, 2048, 2056, 2064, 2448, 2704, 2736, 3248
DFF = 2816
NEXP = 8
EFF = 3584
EPS = 1e-6
NDSEM = 96
LOCT = 16
LOC = LOCT * 128


class Obj:
    __slots__ = ("name", "h", "w", "r", "ds", "is_sb")

    def __init__(self, name, h, is_sb):
        self.name = name
        self.h = h
        self.w = []
        self.r = []
        self.ds = None
        self.is_sb = is_sb

    def __getitem__(self, idx):
        return self.h[idx]


class Scope:
    def __init__(self, S):
        self.S = S
        self.stack = ExitStack()
        self.objs = []

    def sb(self, name, shape, dt):
        S = self.S
        S.uid += 1
        o = Obj(name, self.stack.enter_context(S.nc.sbuf_tensor("%s_%d" % (name, S.uid), list(shape), dt)), True)
        self.objs.append(o)
        return o


class Sched:
    def __init__(self, nc, stack):
        self.nc = nc
        self.E = {}
        for nm, h in (("pe", nc.tensor), ("act", nc.scalar), ("dve", nc.vector), ("pool", nc.gpsimd), ("sp", nc.sync)):
            sem = stack.enter_context(nc.semaphore("sem_" + nm))
            self.E[nm] = dict(h=h, sem=sem, cnt=0, seen={})
        self.dpool = [dict(sem=stack.enter_context(nc.semaphore("dsem%d" % i)), cnt=0, free=True) for i in range(NDSEM)]
        self.ninst = 0
        self.uid = 0
        self.stack = stack

    def ps(self, name, shape, dt=F32):
        return Obj(name, self.stack.enter_context(self.nc.psum_tensor(name, list(shape), dt)), True)

    def dram(self, name, shape, dt, kind="Internal"):
        t = self.nc.dram_tensor(name, list(shape), dt, kind=kind)
        return Obj(name, t.ap(), False)

    @contextmanager
    def scope(self):
        sc = Scope(self)
        try:
            yield sc
        finally:
            self.barrier()
            for o in sc.objs:
                if o.ds is not None:
                    self.dpool[o.ds]["free"] = True
                    o.ds = None
            sc.stack.close()

    def _wait(self, e, tok):
        sem, val = tok
        k = id(sem)
        pe = self.E["pe"]
        if sem is pe["sem"] and val > pe["cnt"]:
            pe["h"].nop().then_inc(pe["sem"], 1)
            pe["cnt"] += 1
            self.ninst += 1
            self.nforced = getattr(self, "nforced", 0) + 1
        E = self.E[e]
        if E["seen"].get(k, 0) >= val:
            return
        E["seen"][k] = val
        E["h"].wait_ge(sem, val)
        self.ninst += 1

    def barrier(self):
        toks = [(E["sem"], E["cnt"]) for E in self.E.values() if E["cnt"] > 0]
        toks += [(d["sem"], d["cnt"]) for d in self.dpool if d["cnt"] > 0]
        for e in self.E:
            for tok in toks:
                self._wait(e, tok)

    def _deps(self, e, ins, outs):
        mysem = id(self.E[e]["sem"])
        for o in ins:
            if not o.is_sb:
                continue
            for tok in o.w:
                if e == "pe" and id(tok[0]) == mysem:
                    continue
                self._wait(e, tok)
        for o in outs:
            if not o.is_sb:
                continue
            for tok in o.w:
                if e == "pe" and id(tok[0]) == mysem:
                    continue
                self._wait(e, tok)
            for tok in o.r:
                if id(tok[0]) == mysem:
                    continue
                self._wait(e, tok)

    def _prune(self, toks):
        best = {}
        for s, v in toks:
            k = id(s)
            if k not in best or best[k][1] < v:
                best[k] = (s, v)
        return list(best.values())

    def op(self, e, fn, outs=(), ins=(), inc=True):
        E = self.E[e]
        self._deps(e, ins, outs)
        inst = fn(E["h"])
        self.ninst += 1
        tok = (E["sem"], E["cnt"] + 1)
        if inc:
            inst.then_inc(E["sem"], 1)
            E["cnt"] += 1
        for o in ins:
            if o.is_sb:
                o.r.append(tok)
                if len(o.r) > 16:
                    o.r = self._prune(o.r)
        for o in outs:
            if o.is_sb:
                o.w = [tok]
                o.r = []
        return inst

    def dma(self, q, out_obj, out_ap, in_obj, in_ap):
        E = self.E[q]
        sbo = out_obj if out_obj.is_sb else in_obj
        assert sbo.is_sb
        self._deps(q, [in_obj], [out_obj])
        if sbo.ds is None:
            for i, d in enumerate(self.dpool):
                if d["free"]:
                    d["free"] = False
                    sbo.ds = i
                    break
            else:
                raise RuntimeError("out of dma semaphores")
        d = self.dpool[sbo.ds]
        if d["cnt"]:
            self._wait(q, (d["sem"], d["cnt"]))
        inst = E["h"].dma_start(out=out_ap, in_=in_ap)
        d["cnt"] += 16
        inst.then_inc(d["sem"], 16)
        self.ninst += 1
        tok = (d["sem"], d["cnt"])
        if in_obj.is_sb:
            in_obj.r.append(tok)
            if len(in_obj.r) > 16:
                in_obj.r = self._prune(in_obj.r)
        if out_obj.is_sb:
            out_obj.w = [tok]
            out_obj.r = []
        return inst


def host_consts(flip):
    C = {}
    i = np.arange(128)
    m, j = np.meshgrid(i, i, indexing="ij")
    NEG = -30000.0
    C["ident_f"] = np.eye(128)
    C["ULE"] = (m <= j)
    C["LGE"] = (m >= j)
    C["SGT"] = (m > j)
    C["SLT"] = (m < j)
    C["NEGF"] = np.where(j <= m, 0.0, NEG)
    C["NEGB"] = np.where(j >= m, 0.0, NEG)
    C["OFFD"] = -(1.0 - np.eye(128))
    cm = np.concatenate([np.asarray(C[k], np.float32) for k in ("ident_f", "ULE", "LGE", "SGT", "SLT", "NEGF", "NEGB", "OFFD")], 1)
    out = {"cmask": np.ascontiguousarray(cm)}
    out["ident_b"] = np.eye(128).astype(ml_dtypes.bfloat16)
    ang = 2 * np.pi * np.outer(i, i) / 128.0
    out["dft_c"] = (np.concatenate([np.cos(ang), np.sin(ang)], 1) / np.sqrt(128.0)).astype(ml_dtypes.bfloat16)
    for nm, T in (("x", SEQ), ("c", CTX)):
        t = np.arange(T)
        if flip:
            t = t[::-1]
        ph = ((np.outer(t, t) % T).astype(np.float64) * (2 * np.pi / T)).astype(np.float32)
        for tag in ("C", "S"):
            M = (np.cos(ph) if tag == "C" else -np.sin(ph)) / np.float32(np.sqrt(T))
            KC = 512 if nm == "x" else 256
            M = M.reshape(T // 128, 128, T // KC, KC).transpose(2, 1, 0, 3)
            out["dft%s_%s" % (tag, nm)] = np.ascontiguousarray(M).astype(ml_dtypes.bfloat16)
        del ph
    inv = 10000.0 ** (-np.arange(0, 16, 2, dtype=np.float32) / 16)
    pos = np.arange(SEQ)
    if flip:
        pos = pos[::-1]
    ar = np.outer(inv, (pos // 64).astype(np.float32))
    ac = np.outer(inv, (pos % 64).astype(np.float32))
    cosx = np.concatenate([np.cos(ar), np.cos(ar), np.cos(ac), np.cos(ac)], 0)
    sinx = np.concatenate([-np.sin(ar), np.sin(ar), -np.sin(ac), np.sin(ac)], 0)
    cos = np.concatenate([np.ones((32, CTX)), cosx], 1)
    sin = np.concatenate([np.zeros((32, CTX)), sinx], 1)
    out["rope_cs"] = np.stack([cos, sin], 0).astype(np.float32)
    return out


M_ID, M_ULE, M_LGE, M_SGT, M_SLT, M_NEGF, M_NEGB, M_OFFD = range(8)


class Builder:
    def __init__(self, stop_after=None, dbg=()):
        self.stop_after = stop_after
        self.dbg = set(dbg)
        self.nc = bass.Bass("TRN2", target_bir_lowering=False)
        self.in_names = []
        self.out_names = ["y_out"]

    def inp(self, name, shape, dt=F32):
        self.in_names.append(name)
        return self.S.dram(name, shape, dt, kind="ExternalInput")

    def scratch(self, name, shape, dt):
        if name in self.dbg:
            self.out_names.append(name)
            return self.S.dram(name, shape, dt, kind="ExternalOutput")
        return self.S.dram(name, shape, dt, kind="Internal")

    def build(self):
        with ExitStack() as st:
            self.S = Sched(self.nc, st)
            self._build()
        return self.nc

    def mk(self, i):
        return self.masks[:, i * 128:(i + 1) * 128]

    def nps(self, exclude=None):
        while True:
            p = self.PS[self.ps_i % len(self.PS)]
            self.ps_i += 1
            if p is not exclude:
                return p

    def npb(self):
        p = self.PB[self.pb_i % len(self.PB)]
        self.pb_i += 1
        return p

    def mm(self, ps, out_ap, a, lhsT, b, rhs, start=True, stop=True, inc=None):
        self.S.op("pe", lambda e: e.matmul(out_ap, lhsT, rhs, start=start, stop=stop), outs=[ps], ins=[a, b], inc=stop if inc is None else inc)

    def tr(self, ps, out_ap, a, in_ap, ident):
        self.S.op("pe", lambda e: e.transpose(out_ap, in_ap, ident), outs=[ps], ins=[a, self.identb, self.masks])

    def act(self, out_o, out_ap, in_o, in_ap, func, ins=(), **kw):
        self.S.op("act", lambda e: e.activation(out_ap, in_ap, func, **kw), outs=[out_o], ins=[in_o] + list(ins))

    def tt(self, eng, out_o, out_ap, a, a_ap, b, b_ap, op):
        self.S.op(eng, lambda e: e.tensor_tensor(out_ap, a_ap, b_ap, op), outs=[out_o], ins=[a, b])

    def ts(self, eng, out_o, out_ap, a, a_ap, s1, s2, op0, op1=None, ins=()):
        if op1 is None:
            self.S.op(eng, lambda e: e.tensor_scalar(out_ap, a_ap, s1, None, op0), outs=[out_o], ins=[a] + list(ins))
        else:
            self.S.op(eng, lambda e: e.tensor_scalar(out_ap, a_ap, s1, s2, op0, op1), outs=[out_o], ins=[a] + list(ins))

    def stt(self, out_o, out_ap, a, a_ap, scalar, b, b_ap, op0, op1, ins=()):
        self.S.op("dve", lambda e: e.scalar_tensor_tensor(out_ap, a_ap, scalar, b_ap, op0, op1), outs=[out_o], ins=[a, b] + list(ins))

    def cp(self, eng, out_o, out_ap, in_o, in_ap):
        if eng == "act":
            self.S.op("act", lambda e: e.copy(out_ap, in_ap), outs=[out_o], ins=[in_o])
        else:
            self.S.op(eng, lambda e: e.tensor_copy(out_ap, in_ap), outs=[out_o], ins=[in_o])

    def recip(self, out_o, out_ap, in_o, in_ap):
        self.S.op("dve", lambda e: e.reciprocal(out_ap, in_ap), outs=[out_o], ins=[in_o])

    def ld(self, out_o, out_ap, in_o, in_ap, q="sp"):
        self.S.dma(q, out_o, out_ap, in_o, in_ap)

    def run_pipe(self, items, fn, max_active=None):
        import os
        if os.environ.get("NOPIPE"):
            for it in items:
                for _ in fn(it):
                    pass
            return
        active = []
        items = list(items)
        k = 0
        while k < len(items) or active:
            if k < len(items) and (max_active is None or len(active) < max_active):
                active.append(fn(items[k]))
                k += 1
            for g in list(active):
                try:
                    next(g)
                except StopIteration:
                    active.remove(g)

    def stop(self, l, name):
        return self.stop_after is not None and self.stop_after == (l, name)

    def _build(self):
        S = self.S
        I = self.inp
        self.x_in = I("x_in", [SEQ, D])
        self.ctx_in = I("ctx_in", [CTX, D])
        self.cT = I("cT", [128, 16])
        self.w_mod = I("w_mod", [DEPTH, D, 6 * D])
        self.b_mod = I("b_mod", [DEPTH, 6 * D])
        self.norm_mix = I("norm_mix", [DEPTH, D])
        self.norm_ffn = I("norm_ffn", [DEPTH, D])
        self.w_in = I("w_in", [DEPTH, D, INW])
        self.dn_convT = I("dn_convT", [DEPTH, 1536, 5])
        self.dn_a_log = I("dn_a_log", [DEPTH, 8])
        self.dn_dt_bias = I("dn_dt_bias", [DEPTH, 8])
        self.dn_norm = I("dn_norm", [DEPTH, 128])
        self.lat_gain = I("lat_gain", [DEPTH, 128, 5])
        self.mla_w_qup = I("mla_w_qup", [DEPTH, 384, 768])
        self.mla_w_kvup = I("mla_w_kvup", [DEPTH, 256, 1024])
        self.w_branch = I("w_branch", [DEPTH, 3, 512, D])
        self.w_out = I("w_out", [DEPTH, D, D])
        self.ffn_w_gate = I("ffn_w_gate", [1, D, DFF])
        self.ffn_w_up = I("ffn_w_up", [1, D, DFF])
        self.ffn_w_down = I("ffn_w_down", [1, DFF, D])
        self.moe_router = I("moe_router", [1, D, NEXP])
        self.moe_w_gate = I("moe_w_gate", [1, NEXP, D, EFF])
        self.moe_w_up = I("moe_w_up", [1, NEXP, D, EFF])
        self.moe_w_down = I("moe_w_down", [1, NEXP, EFF, D])
        self.final_norm = I("final_norm", [1, D])
        self.c_mask = I("cmask", [128, 8 * 128])
        self.c_identb = I("ident_b", [128, 128], BF16)
        self.c_dftc = I("dft_c", [128, 256], BF16)
        self.c_dft = {("C", "x"): I("dftC_x", [8, 128, 32, 512], BF16), ("S", "x"): I("dftS_x", [8, 128, 32, 512], BF16),
                      ("C", "c"): I("dftC_c", [1, 128, 2, 256], BF16), ("S", "c"): I("dftS_c", [1, 128, 2, 256], BF16)}
        self.c_rope = I("rope_cs", [2, 32, TALL])
        self.y_out = S.dram("y_out", [LOC, D], F32, kind="ExternalOutput")
        sc = self.scratch
        self.xres = sc("xres", [TALL, D], F32)
        self.mod_d = sc("mod_d", [2, 6 * D], F32)
        self.qT_d = sc("qT_d", [128, NT, 4, 128], BF16)
        self.kT_d = sc("kT_d", [128, NT, 4, 128], BF16)
        self.ktm_d = sc("ktm_d", [NT, 128, 4, 128], BF16)
        self.vtm_d = sc("vtm_d", [NT, 128, 4, 128], BF16)
        self.z_d = sc("z_d", [TALL, 512], BF16)
        self.lat_d = sc("lat_d", [5, 128, TALL], BF16)
        self.kpe_d = sc("kpe_d", [32, TALL], BF16)
        self.uT_d = sc("uT_d", [4, 128, TALL], BF16)
        self.gT_d = sc("gT_d", [24, 128, TALL], BF16)
        self.o_d = [sc("of_d", [TALL, 512], F32), sc("ob_d", [TALL, 512], F32)]
        self.yT_d = sc("yT_d", [3, 4, 128, TALL], BF16)
        self.hx_d = sc("hx_d", [TALL, D], BF16) if "hx_d" in self.dbg else None
        self.gb_d = sc("gb_d", [128, NT * 16], F32) if "gb_d" in self.dbg else None

        with S.scope() as G:
            self.masks = G.sb("masks", [128, 8 * 128], F32)
            self.ld(self.masks, self.masks[:], self.c_mask, self.c_mask[:])
            self.identb = G.sb("identb", [128, 128], BF16)
            self.ld(self.identb, self.identb[:], self.c_identb, self.c_identb[:])
            self.ones_b = G.sb("ones_b", [128, 128], BF16)
            S.op("dve", lambda e: e.memset(self.ones_b[:], 1.0), outs=[self.ones_b])
            self.ones_f = G.sb("ones_f", [128, 128], F32)
            S.op("dve", lambda e: e.memset(self.ones_f[:], 1.0), outs=[self.ones_f])
            self.cact = G.sb("cact", [128, 16], F32)
            self.ld(self.cact, self.cact[:], self.cT, self.cT[:])
            self.act(self.cact, self.cact[:], self.cact, self.cact[:], AF.Silu)
            self.gb = G.sb("gb", [128, NT, 16], F32)
            self.PS = [S.ps("psum%d" % i, [128, 1024]) for i in range(3)]
            self.PB = [S.ps("psumb%d" % i, [128, 1024], BF16) for i in range(2)]
            self.ps_i = 0
            self.pb_i = 0
            for l in range(DEPTH):
                if self.layer(l):
                    break

    def xsrc(self, l, t):
        if l == 0:
            if t < NCT:
                return self.ctx_in, self.ctx_in[t * 128:(t + 1) * 128, :]
            return self.x_in, self.x_in[(t - NCT) * 128:(t - NCT + 1) * 128, :]
        return self.xres, self.xres[t * 128:(t + 1) * 128, :]

    def layer(self, l):
        ns = self.nc.named_scope
        last = l == DEPTH - 1
        need_ctx = not last
        with ns("L%d_mod" % l):
            self.phase_mod(l)
        if self.stop(l, "mod"):
            return True
        with self.S.scope() as P:
            self.hT = P.sb("hT", [128, 8, TALL], BF16)
            with ns("L%d_norm" % l):
                self.phase_norm(l, "mix", list(range(NT)), self.hT, 0)
            with ns("L%d_qkv" % l):
                self.phase_qkv(l)
            with ns("L%d_zab" % l):
                self.phase_zab(l)
            with ns("L%d_lat" % l):
                self.phase_lat(l)
            with ns("L%d_fg" % l):
                self.phase_fg(l)
            if self.stop(l, "proj"):
                return True
        with ns("L%d_dn" % l):
            self.phase_dn(l, need_ctx)
        if self.stop(l, "dn"):
            return True
        with ns("L%d_dnout" % l):
            self.phase_dnout(l, need_ctx)
        with ns("L%d_mla" % l):
            self.phase_mla(l, need_ctx)
        with ns("L%d_fft" % l):
            self.phase_fft(l, need_ctx)
        if self.stop(l, "mix"):
            return True
        with ns("L%d_merge" % l):
            self.phase_merge(l, need_ctx)
        if self.stop(l, "merge"):
            return True
        with ns("L%d_ffn" % l):
            self.phase_ffn(l)
        return False

    def out_tiles(self, l, need_ctx):
        if l == DEPTH - 1:
            return list(range(NCT, NCT + LOCT))
        return list(range(0 if need_ctx else NCT, NT))

    def phase_mod(self, l):
        S = self.S
        with S.scope() as P:
            wm = [P.sb("wm%d" % i, [128, 8, 512], F32) for i in range(2)]
            bm = P.sb("bm", [2, 6 * D], F32)
            rows = P.sb("mrows", [2, 6 * D], F32)
            self.ld(bm, bm[:], self.b_mod, self.b_mod[l].partition_broadcast(2))
            wv = self.w_mod[l].rearrange("(kc p) n -> p kc n", p=128)
            for n in range(12):
                w = wm[n % 2]
                self.ld(w, w[:], self.w_mod, wv[:, :, n * 512:(n + 1) * 512])
                ps = self.nps()
                for kc in range(8):
                    self.mm(ps, ps[0:2, 0:512], self.cact, self.cact[:, kc * 2:kc * 2 + 2], w, w[:, kc, :], kc == 0, kc == 7)
                self.tt("dve", rows, rows[0:2, n * 512:(n + 1) * 512], ps, ps[0:2, 0:512], bm, bm[0:2, n * 512:(n + 1) * 512], ALU.add)
            self.ld(self.mod_d, self.mod_d[:, :], rows, rows[:], q="sp")

    def load_bc(self, P, l, r, idx, gain=None):
        t = P.sb("bc%d_%d" % (idx, r), [128, D], F32)
        self.ld(t, t[:], self.mod_d, self.mod_d[r, idx * D:(idx + 1) * D].partition_broadcast(128))
        if gain is not None:
            g = P.sb("bcg%d_%d" % (idx, r), [128, D], F32)
            self.ld(g, g[:], gain, gain[l].partition_broadcast(128))
            self.stt(t, t[:], t, t[:], 1.0, g, g[:], ALU.add, ALU.mult)
        return t

    def phase_norm(self, l, kind, tiles, hT, col0, router=None):
        S = self.S
        gi, si, gain = (1, 0, self.norm_mix) if kind == "mix" else (4, 3, self.norm_ffn)
        with S.scope() as P:
            G = {}
            SH = {}
            for r in sorted(set(1 if t < NCT else 0 for t in tiles)):
                G[r] = self.load_bc(P, l, r, gi, gain)
                SH[r] = self.load_bc(P, l, r, si)
            NB = 6
            xt = [P.sb("xt%d" % i, [128, D], F32) for i in range(NB)]
            junks = [P.sb("junk%d" % i, [128, D], F32) for i in range(2)]
            ss = [P.sb("ss%d" % i, [128, 1], F32) for i in range(NB)]
            hf = [P.sb("hf%d" % i, [128, D], F32) for i in range(NB)]
            hb = [P.sb("hb%d" % i, [128, D], BF16) for i in range(NB)]
            def tile_gen(it):
                i, t = it
                r = 1 if t < NCT else 0
                x, s, f, b = xt[i % NB], ss[i % NB], hf[i % NB], hb[i % NB]
                junk = junks[i % 2]
                so, sap = self.xsrc(l, t) if kind == "mix" else (self.xres, self.xres[t * 128:(t + 1) * 128, :])
                self.ld(x, x[:], so, sap)
                yield
                self.act(junk, junk[:], x, x[:], AF.Square)
                yield
                self.S.op("dve", lambda e: e.tensor_reduce(s[:], junk[:], mybir.AxisListType.X, ALU.add), outs=[s], ins=[junk])
                yield
                self.act(s, s[:], s, s[:], AF.Sqrt, bias=EPS, scale=1.0 / D)
                yield
                self.recip(s, s[:], s, s[:])
                self.stt(f, f[:], x, x[:], s[:, 0:1], G[r], G[r][:], ALU.mult, ALU.mult, ins=[s])
                yield
                if router is not None:
                    self.tt("pool", f, f[:], f, f[:], SH[r], SH[r][:], ALU.add)
                    yield
                    self.cp("act", b, b[:], f, f[:])
                    router(i, t, f)
                else:
                    self.tt("pool", b, b[:], f, f[:], SH[r], SH[r][:], ALU.add)
                if self.hx_d is not None and kind == "mix":
                    self.ld(self.hx_d, self.hx_d[t * 128:(t + 1) * 128, :], b, b[:])
                yield
                pb = self.npb()
                for kc in range(8):
                    self.tr(pb, pb[:, kc * 128:(kc + 1) * 128], b, b[:, kc * 128:(kc + 1) * 128], self.identb[:])
                yield
                c0 = col0 + i * 128
                self.cp("act", hT, hT[:, :, c0:c0 + 128], pb, pb[:].rearrange("p (k t) -> p k t", k=8))

            self.run_pipe(list(enumerate(tiles)), tile_gen)

    def tok_chunks(self):
        return [(0, CTX)] + [(CTX + i * 512, 512) for i in range(SEQ // 512)]

    def load_w(self, wt, src_obj, src_ap):
        self.ld(wt, wt, src_obj, src_ap, q="pool")

    def linear_fm(self, P, l, col0, nblk, epilogue, per_block_done=None, chunks=None):
        wts = [P.sb("wfm%d" % i, [128, 8, 512], BF16) for i in range(2)]
        wv = self.w_in[l].rearrange("(kc p) n -> p kc n", p=128)
        gi = 0
        for g0 in range(0, nblk, 4):
            nb = min(4, nblk - g0)
            wt = wts[gi % 2]
            gi += 1
            self.S.dma("pool", wt, wt[:, :, 0:nb * 128], self.w_in, wv[:, :, col0 + g0 * 128:col0 + (g0 + nb) * 128])
            for b in range(nb):
                for ci, (t0, n) in enumerate(self.tok_chunks() if chunks is None else chunks):
                    ps = self.nps()
                    for kc in range(8):
                        self.mm(ps, ps[:, 0:n], wt, wt[:, kc, b * 128:(b + 1) * 128], self.hT, self.hT[:, kc, t0:t0 + n], kc == 0, kc == 7)
                    epilogue(g0 + b, ci, ps, t0, n)
                if per_block_done is not None:
                    per_block_done(g0 + b)

    def phase_qkv(self, l):
        S = self.S
        W = TALL + 4
        wv = self.w_in[l].rearrange("(kc p) n -> p kc n", p=128)
        with S.scope() as P:
            U = P.sb("convU", [128, W + 4], F32)
            accs = [P.sb("convA%d" % i, [128, W], F32) for i in range(2)]
            sqbs = [P.sb("sqb%d" % i, [128, W], BF16) for i in range(2)]
            stgs = [P.sb("stg%d" % i, [128, W], BF16) for i in range(2)]
            cw = P.sb("cw", [128, 12, 5], F32)
            rn = [P.sb("rn%d" % i, [128, 512], F32) for i in range(2)]
            tms = [P.sb("tms%d" % i, [128, 8, 128], BF16) for i in range(2)]
            wts = [P.sb("wfm%d" % i, [128, 8, 512], BF16) for i in range(2)]
            self.ld(cw, cw[:], self.dn_convT, self.dn_convT[l].rearrange("(b p) k -> p b k", p=128))
            S.op("dve", lambda e: e.memset(U[:], 0.0), outs=[U])
            cnt = [0, 0]

            def blk(b):
                acc, sqb, stg = accs[b % 2], sqbs[b % 2], stgs[b % 2]
                wt = wts[(b // 4) % 2]
                if b % 4 == 0:
                    self.S.dma("pool", wt, wt[:], self.w_in, wv[:, :, O_Q + b * 128:O_Q + (b + 4) * 128])
                bb = b % 4
                for ci, (t0, n) in enumerate(self.tok_chunks()):
                    ps = self.nps()
                    for kc in range(8):
                        self.mm(ps, ps[:, 0:n], wt, wt[:, kc, bb * 128:(bb + 1) * 128], self.hT, self.hT[:, kc, t0:t0 + n], kc == 0, kc == 7)
                    off = 2 + t0 if t0 < CTX else 2 + 4 + t0
                    self.cp("act", U, U[:, off:off + n], ps, ps[:, 0:n])
                yield
                self.ts("dve", acc, acc[:], U, U[:, 0:W], cw[:, b, 0:1], None, ALU.mult, ins=[cw])
                for k in range(1, 5):
                    self.stt(acc, acc[:], U, U[:, k:k + W], cw[:, b, k:k + 1], acc, acc[:], ALU.mult, ALU.add, ins=[cw])
                yield
                self.act(acc, acc[:], acc, acc[:], AF.Silu)
                kind, h = b // 4, b % 4
                if kind < 2:
                    self.tt("pool", sqb, sqb[:], acc, acc[:], acc, acc[:], ALU.mult)
                    yield
                    for c0 in range(0, W, 512):
                        n = min(512, W - c0)
                        ps = self.nps()
                        self.mm(ps, ps[:, 0:n], self.ones_b, self.ones_b[:], sqb, sqb[:, c0:c0 + n])
                        r = rn[cnt[0] % 2]
                        cnt[0] += 1
                        self.act(r, r[:, 0:n], ps, ps[:, 0:n], AF.Sqrt, bias=EPS, scale=1.0)
                        self.recip(r, r[:, 0:n], r, r[:, 0:n])
                        self.tt("pool", stg, stg[:, c0:c0 + n], acc, acc[:, c0:c0 + n], r, r[:, 0:n], ALU.mult)
                        if (c0 // 512) % 3 == 2:
                            yield
                    dst = self.qT_d if kind == 0 else self.kT_d
                    self.ld(dst, dst[:, 0:NCT, h, :], stg, stg[:, 0:CTX].rearrange("p (t k) -> p t k", k=128))
                    self.ld(dst, dst[:, NCT:NT, h, :], stg, stg[:, CTX + 4:W].rearrange("p (t k) -> p t k", k=128))
                else:
                    self.cp("pool", stg, stg[:], acc, acc[:])
                yield
                if kind >= 1:
                    dst = self.ktm_d if kind == 1 else self.vtm_d
                    for t0 in range(0, NT, 8):
                        nt = min(8, NT - t0)
                        pb = self.npb()
                        for i in range(nt):
                            t = t0 + i
                            c0 = t * 128 if t < NCT else t * 128 + 4
                            self.tr(pb, pb[:, i * 128:(i + 1) * 128], stg, stg[:, c0:c0 + 128], self.identb[:])
                        tm = tms[cnt[1] % 2]
                        cnt[1] += 1
                        self.cp("act", tm, tm[:, 0:nt, :], pb, pb[:, 0:nt * 128].rearrange("p (t k) -> p t k", k=128))
                        self.ld(dst, dst[t0:t0 + nt, :, h, :].rearrange("t p k -> p t k"), tm, tm[:, 0:nt, :])
                        yield

            self.run_pipe(list(range(12)), blk, max_active=2)

    def phase_zab(self, l):
        S = self.S
        wv = self.w_in[l].rearrange("(kc p) n -> p kc n", p=128)
        with S.scope() as P:
            wz = P.sb("wz", [128, 8, 512], BF16)
            wab = P.sb("wab", [128, 8, 16], BF16)
            self.S.dma("pool", wz, wz[:], self.w_in, wv[:, :, O_Z:O_Z + 512])
            self.S.dma("pool", wab, wab[:], self.w_in, wv[:, :, O_A:O_A + 16])
            zs = [P.sb("zs%d" % i, [128, 512], BF16) for i in range(2)]
            for t in range(NT):
                ps = self.nps()
                for kc in range(8):
                    self.mm(ps, ps[:, 0:512], self.hT, self.hT[:, kc, t * 128:(t + 1) * 128], wz, wz[:, kc, :], kc == 0, kc == 7)
                z = zs[t % 2]
                self.act(z, z[:], ps, ps[:, 0:512], AF.Silu)
                self.ld(self.z_d, self.z_d[t * 128:(t + 1) * 128, :], z, z[:])
            ps = self.nps()
            for t in range(NT):
                for kc in range(8):
                    self.mm(ps, ps[:, t * 16:(t + 1) * 16], self.hT, self.hT[:, kc, t * 128:(t + 1) * 128], wab, wab[:, kc, :], kc == 0, kc == 7)
            al = P.sb("alog", [128, 8], F32)
            dtb = P.sb("dtb", [128, 8], F32)
            self.ld(al, al[:], self.dn_a_log, self.dn_a_log[l].partition_broadcast(128))
            self.ld(dtb, dtb[:], self.dn_dt_bias, self.dn_dt_bias[l].partition_broadcast(128))
            self.act(al, al[:], al, al[:], AF.Exp)
            tmp = P.sb("abtmp", [128, NT, 8], F32)
            pv = ps[:, 0:NT * 16].rearrange("p (t c) -> p t c", c=16)
            self.tt("dve", tmp, tmp[:], ps, pv[:, :, 0:8], dtb, dtb[:].unsqueeze(1).to_broadcast([128, NT, 8]), ALU.add)
            self.act(tmp, tmp[:], tmp, tmp[:], AF.Exp)
            self.act(tmp, tmp[:], tmp, tmp[:], AF.Ln, bias=1.0, scale=1.0)
            self.stt(self.gb, self.gb[:, :, 0:8], tmp, tmp[:], -1.0, al, al[:].unsqueeze(1).to_broadcast([128, NT, 8]), ALU.mult, ALU.mult)
            self.act(self.gb, self.gb[:, :, 8:16], ps, pv[:, :, 8:16], AF.Sigmoid)
            if self.gb_d is not None:
                self.ld(self.gb_d, self.gb_d[:], self.gb, self.gb[:].rearrange("p t c -> p (t c)"))

    def phase_lat(self, l):
        S = self.S
        wv = self.w_in[l].rearrange("(kc p) n -> p kc n", p=128)
        with S.scope() as P:
            wl = P.sb("wl", [128, 8, 640], BF16)
            self.S.dma("pool", wl, wl[:], self.w_in, wv[:, :, O_CQ:O_CQ + 640])
            wk = P.sb("wkpe", [128, 8, 64], BF16)
            self.S.dma("pool", wk, wk[:, :, 0:32], self.w_in, wv[:, :, O_KPE:O_KPE + 32])
            for (d0, s0) in ((0, 8), (8, 0), (16, 24), (24, 16)):
                self.S.dma("pool", wk, wk[:, :, 32 + d0:32 + d0 + 8], self.w_in, wv[:, :, O_KPE + s0:O_KPE + s0 + 8])
            gn = P.sb("latg", [128, 5], F32)
            self.ld(gn, gn[:], self.lat_gain, self.lat_gain[l])
            sets = [dict(raw=[P.sb("raw%d" % i, [128, 512], F32) for i in range(3)], sq=[P.sb("sq%d" % i, [128, 512], BF16) for i in range(3)],
                         rs=P.sb("rs", [128, 512], F32), outb=[P.sb("lout%d" % i, [128, 512], BF16) for i in range(5)],
                         cs=P.sb("ropecs", [32, 2, 512], F32), t1=P.sb("kt1", [32, 512], F32), t2=P.sb("kt2", [32, 512], F32), kb=P.sb("kpeb", [32, 512], BF16)) for _ in range(2)]

            def chunk_gen(it):
                ci, (t0, n) = it
                B_ = sets[ci % 2]
                raw, sq, rs, outb, cs, t1, t2, kb = B_["raw"], B_["sq"], B_["rs"], B_["outb"], B_["cs"], B_["t1"], B_["t2"], B_["kb"]
                self.ld(cs, cs[:, :, 0:n], self.c_rope, self.c_rope[:, :, t0:t0 + n].rearrange("c p t -> p c t"))
                for (b0, nb, R) in ((0, 3, 384.0), (3, 2, 256.0)):
                    for b in range(nb):
                        ps = self.nps()
                        for kc in range(8):
                            self.mm(ps, ps[:, 0:n], wl, wl[:, kc, (b0 + b) * 128:(b0 + b + 1) * 128], self.hT, self.hT[:, kc, t0:t0 + n], kc == 0, kc == 7)
                        self.cp("act", raw[b], raw[b][:, 0:n], ps, ps[:, 0:n])
                        self.tt("pool", sq[b], sq[b][:, 0:n], raw[b], raw[b][:, 0:n], raw[b], raw[b][:, 0:n], ALU.mult)
                    yield
                    ps = self.nps()
                    for b in range(nb):
                        self.mm(ps, ps[:, 0:n], self.ones_b, self.ones_b[:], sq[b], sq[b][:, 0:n], b == 0, b == nb - 1)
                    self.act(rs, rs[:, 0:n], ps, ps[:, 0:n], AF.Sqrt, bias=EPS, scale=1.0 / R)
                    yield
                    self.recip(rs, rs[:, 0:n], rs, rs[:, 0:n])
                    for b in range(nb):
                        o = outb[b0 + b]
                        self.stt(o, o[:, 0:n], raw[b], raw[b][:, 0:n], gn[:, b0 + b:b0 + b + 1], rs, rs[:, 0:n], ALU.mult, ALU.mult, ins=[gn])
                        self.ld(self.lat_d, self.lat_d[b0 + b, :, t0:t0 + n], o, o[:, 0:n])
                    yield
                psA = self.nps()
                for kc in range(8):
                    self.mm(psA, psA[0:32, 0:n], wk, wk[:, kc, 0:32], self.hT, self.hT[:, kc, t0:t0 + n], kc == 0, kc == 7)
                for kc in range(8):
                    self.mm(psA, psA[0:32, 512:512 + n], wk, wk[:, kc, 32:64], self.hT, self.hT[:, kc, t0:t0 + n], kc == 0, kc == 7)
                self.tt("dve", t1, t1[:, 0:n], psA, psA[0:32, 0:n], cs, cs[:, 0, 0:n], ALU.mult)
                self.tt("dve", t2, t2[:, 0:n], psA, psA[0:32, 512:512 + n], cs, cs[:, 1, 0:n], ALU.mult)
                yield
                self.tt("pool", kb, kb[:, 0:n], t1, t1[:, 0:n], t2, t2[:, 0:n], ALU.add)
                self.ld(self.kpe_d, self.kpe_d[:, t0:t0 + n], kb, kb[:, 0:n])

            self.run_pipe(list(enumerate(self.tok_chunks())), chunk_gen, max_active=2)

    def phase_fg(self, l):
        with self.S.scope() as P:
            st = [P.sb("fgs%d" % i, [128, 512], BF16) for i in range(3)]
            cnt = [0]

            def epi_f(b, ci, ps, t0, n):
                s = st[cnt[0] % 3]
                cnt[0] += 1
                self.cp("act", s, s[:, 0:n], ps, ps[:, 0:n])
                self.ld(self.uT_d, self.uT_d[b, :, t0:t0 + n], s, s[:, 0:n])

            def epi_g(b, ci, ps, t0, n):
                s = st[cnt[0] % 3]
                cnt[0] += 1
                self.act(s, s[:, 0:n], ps, ps[:, 0:n], AF.Sigmoid)
                self.ld(self.gT_d, self.gT_d[b, :, t0:t0 + n], s, s[:, 0:n])

            self.linear_fm(P, l, O_F, 4, epi_f)
            gch = [c for c in self.tok_chunks() if CTX <= c[0] < CTX + LOC] if l == DEPTH - 1 else None
            self.linear_fm(P, l, O_G, 24, epi_g, chunks=gch)


    def phase_dn(self, l, need_ctx):
        S = self.S
        order = [list(range(NT)), [1, 0] + list(range(NT - 1, NCT - 1, -1))]
        otiles = set(self.out_tiles(l, need_ctx))
        B3 = [128, 4, 128]

        def hb(ap):
            return ap.unsqueeze(2).to_broadcast(B3)

        def mb(ap):
            return ap.unsqueeze(1).to_broadcast(B3)

        with S.scope() as P:
            St = [P.sb("St%d" % d, B3, F32) for d in range(2)]
            Sb = [P.sb("Sb%d" % d, B3, BF16) for d in range(2)]
            for d in range(2):
                S.op("dve", lambda e: e.memset(St[d][:], 0.0), outs=[St[d]])
                S.op("dve", lambda e: e.memset(Sb[d][:], 0.0), outs=[Sb[d]])
            L = {}
            for d in range(2):
                for par in range(2):
                    k = (d, par)
                    L[k] = dict(
                        qt=P.sb("qt", B3, BF16), kt=P.sb("kt", B3, BF16), km=P.sb("km", B3, BF16), vm=P.sb("vm", B3, BF16),
                        sm=P.sb("sm", [128, 20], F32), sm2=P.sb("sm2", [128, 8], F32), gm=P.sb("gm", B3, F32),
                        tD=P.sb("tD", B3, F32), Es=P.sb("Es", B3, F32), EsN=P.sb("EsN", B3, F32), ET=P.sb("ET", B3, F32),
                        tN=P.sb("tN", B3, F32), Na=P.sb("Na", B3, BF16), Nb=P.sb("Nb", B3, BF16), Nta=P.sb("Nta", B3, BF16), Ntb=P.sb("Ntb", B3, BF16),
                        Ra=P.sb("Ra", B3, BF16), Rb=P.sb("Rb", B3, BF16), aT=P.sb("aT", B3, BF16),
                        vb=P.sb("vb", B3, F32), kd=P.sb("kd", B3, BF16), r2t=P.sb("r2t", B3, F32), r2=P.sb("r2", B3, BF16),
                        vn=P.sb("vn", B3, BF16), tS=P.sb("tS", B3, F32), avs=P.sb("avs", B3, F32), to=P.sb("to", B3, F32), o=P.sb("o", B3, F32))
            def lane_step(s, d):
                t = order[d][s]
                b = L[(d, s % 2)]
                want_o = t in otiles
                if l == DEPTH - 1 and d == 0 and t >= NCT + LOCT:
                    return
                qt, kt, km, vm, sm, sm2 = b["qt"], b["kt"], b["km"], b["vm"], b["sm"], b["sm2"]
                self.ld(qt, qt[:], self.qT_d, self.qT_d[:, t, :, :])
                self.ld(kt, kt[:], self.kT_d, self.kT_d[:, t, :, :])
                self.ld(km, km[:], self.ktm_d, self.ktm_d[t])
                self.ld(vm, vm[:], self.vtm_d, self.vtm_d[t])
                g = self.gb[:, t, d * 4:(d + 1) * 4]
                beta = self.gb[:, t, 8 + d * 4:8 + (d + 1) * 4]
                Mle = self.mk(M_ULE if d == 0 else M_LGE)
                Msg = self.mk(M_SGT if d == 0 else M_SLT)
                NEGd = self.mk(M_NEGF if d == 0 else M_NEGB)
                NEGt = self.mk(M_NEGB if d == 0 else M_NEGF)
                psS = self.nps()
                self.mm(psS, psS[:, 0:4], self.masks, Mle, self.gb, g)
                self.mm(psS, psS[:, 4:8], self.ones_f, self.ones_f[:], self.gb, g)
                self.cp("act", sm2, sm2[:], psS, psS[:, 0:8])
                self.act(sm, sm[:, 0:8], sm2, sm2[:], AF.Exp)
                self.tt("dve", sm2, sm2[:, 0:4], sm2, sm2[:, 4:8], sm2, sm2[:, 0:4], ALU.subtract)
                self.act(sm, sm[:, 8:12], sm2, sm2[:, 0:4], AF.Exp)
                self.stt(sm, sm[:, 12:16], sm, sm[:, 0:4], -1.0, self.gb, beta, ALU.mult, ALU.mult)
                self.ts("dve", sm, sm[:, 16:20], sm, sm[:, 0:4], 128.0 ** -0.5, None, ALU.mult)
                yield
                gm = b["gm"]
                self.tt("pool", gm, gm[:], self.masks, mb(Msg), self.gb, hb(g), ALU.mult)
                psD = self.nps()
                for h in range(4):
                    self.mm(psD, psD[:, h * 128:(h + 1) * 128], self.masks, Mle, gm, gm[:, h, :])
                    self.mm(psD, psD[:, 512 + h * 128:512 + (h + 1) * 128], gm, gm[:, h, :], self.masks, Mle)
                tD, Es, EsN, ET = b["tD"], b["Es"], b["EsN"], b["ET"]
                self.tt("dve", tD, tD[:], psD, psD[:, 0:512].rearrange("p (h k) -> p h k", h=4), self.masks, mb(NEGd), ALU.add)
                self.act(Es, Es[:], tD, tD[:], AF.Exp)
                self.tt("pool", EsN, EsN[:], Es, Es[:], self.masks, mb(self.mk(M_OFFD)), ALU.mult)
                self.tt("dve", tD, tD[:], psD, psD[:, 512:1024].rearrange("p (h k) -> p h k", h=4), self.masks, mb(NEGt), ALU.add)
                self.act(ET, ET[:], tD, tD[:], AF.Exp)
                yield
                psG = self.nps()
                for h in range(4):
                    self.mm(psG, psG[:, h * 128:(h + 1) * 128], kt, kt[:, h, :], kt, kt[:, h, :])
                    self.mm(psG, psG[:, 512 + h * 128:512 + (h + 1) * 128], kt, kt[:, h, :], qt, qt[:, h, :])
                tN, N, Nt, R, aT = b["tN"], b["Na"], b["Nta"], b["Ra"], b["aT"]
                N2, Nt2, R2 = b["Nb"], b["Ntb"], b["Rb"]
                self.tt("dve", tN, tN[:], psG, psG[:, 0:512].rearrange("p (h k) -> p h k", h=4), self.gb, hb(beta), ALU.mult)
                self.tt("pool", N, N[:], tN, tN[:], EsN, EsN[:], ALU.mult)
                self.stt(aT, aT[:], psG, psG[:, 512:1024].rearrange("p (h k) -> p h k", h=4), 128.0 ** -0.5, ET, ET[:], ALU.mult, ALU.mult)
                yield
                pb = self.npb()
                for h in range(4):
                    self.tr(pb, pb[:, h * 128:(h + 1) * 128], N, N[:, h, :], self.identb[:])
                self.cp("act", Nt, Nt[:], pb, pb[:, 0:512].rearrange("p (h k) -> p h k", h=4))
                self.tt("pool", R, R[:], Nt, Nt[:], self.identb, mb(self.identb[:]), ALU.add)
                for lev in range(6):
                    yield
                    ps1 = self.nps()
                    for h in range(4):
                        self.mm(ps1, ps1[:, h * 128:(h + 1) * 128], Nt, Nt[:, h, :], N, N[:, h, :])
                        if lev < 5:
                            self.mm(ps1, ps1[:, 512 + h * 128:512 + (h + 1) * 128], N, N[:, h, :], Nt, Nt[:, h, :])
                    self.cp("act", N2, N2[:], ps1, ps1[:, 0:512].rearrange("p (h k) -> p h k", h=4))
                    if lev < 5:
                        self.cp("dve", Nt2, Nt2[:], ps1, ps1[:, 512:1024].rearrange("p (h k) -> p h k", h=4))
                    yield
                    ps2 = self.nps()
                    for h in range(4):
                        self.mm(ps2, ps2[:, h * 128:(h + 1) * 128], N2, N2[:, h, :], R, R[:, h, :])
                    self.tt("dve", R2, R2[:], ps2, ps2[:, 0:512].rearrange("p (h k) -> p h k", h=4), R, R[:], ALU.add)
                    N, N2 = N2, N
                    Nt, Nt2 = Nt2, Nt
                    R, R2 = R2, R
                b["Rfin"] = R

            def scan_step(s, d):
                t = order[d][s]
                b = L[(d, s % 2)]
                want_o = t in otiles
                if l == DEPTH - 1 and d == 0 and t >= NCT + LOCT:
                    return
                qt, kt, km, vm, sm, aT, R = b["qt"], b["kt"], b["km"], b["vm"], b["sm"], b["aT"], b["Rfin"]
                beta = self.gb[:, t, 8 + d * 4:8 + (d + 1) * 4]
                vb, kd, r2t, r2, vn, tS, avs, to, o = b["vb"], b["kd"], b["r2t"], b["r2"], b["vn"], b["tS"], b["avs"], b["to"], b["o"]
                self.tt("pool", vb, vb[:], vm, vm[:], self.gb, hb(beta), ALU.mult)
                self.tt("pool", kd, kd[:], km, km[:], sm, hb(sm[:, 8:12]), ALU.mult)
                psK = self.nps()
                for h in range(4):
                    self.mm(psK, psK[:, h * 128:(h + 1) * 128], kt, kt[:, h, :], Sb[d], Sb[d][:, h, :])
                    if want_o:
                        self.mm(psK, psK[:, 512 + h * 128:512 + (h + 1) * 128], qt, qt[:, h, :], Sb[d], Sb[d][:, h, :])
                self.tt("dve", r2t, r2t[:], psK, psK[:, 0:512].rearrange("p (h k) -> p h k", h=4), sm, hb(sm[:, 12:16]), ALU.mult)
                if want_o:
                    self.tt("dve", to, to[:], psK, psK[:, 512:1024].rearrange("p (h k) -> p h k", h=4), sm, hb(sm[:, 16:20]), ALU.mult)
                self.tt("pool", r2, r2[:], r2t, r2t[:], vb, vb[:], ALU.add)
                yield
                psV = self.nps()
                for h in range(4):
                    self.mm(psV, psV[:, h * 128:(h + 1) * 128], R, R[:, h, :], r2, r2[:, h, :])
                self.cp("act", vn, vn[:], psV, psV[:, 0:512].rearrange("p (h k) -> p h k", h=4))
                yield
                psU = self.nps()
                for h in range(4):
                    self.mm(psU, psU[:, h * 128:(h + 1) * 128], kd, kd[:, h, :], vn, vn[:, h, :])
                    if want_o:
                        self.mm(psU, psU[:, 512 + h * 128:512 + (h + 1) * 128], aT, aT[:, h, :], vn, vn[:, h, :])
                self.tt("pool", tS, tS[:], St[d], St[d][:], sm, hb(sm[:, 4:8]), ALU.mult)
                self.tt("dve", St[d], St[d][:], tS, tS[:], psU, psU[:, 0:512].rearrange("p (h k) -> p h k", h=4), ALU.add)
                self.cp("act", Sb[d], Sb[d][:], St[d], St[d][:])
                if want_o:
                    self.cp("act", avs, avs[:], psU, psU[:, 512:1024].rearrange("p (h k) -> p h k", h=4))
                    self.tt("pool", o, o[:], to, to[:], avs, avs[:], ALU.add)
                    self.ld(self.o_d[d], self.o_d[d][t * 128:(t + 1) * 128, :], o, o[:].rearrange("p h k -> p (h k)"))

            for s in range(NT + 1):
                gens = ([scan_step(s - 1, 0), scan_step(s - 1, 1)] if s > 0 else []) + ([lane_step(s, 0), lane_step(s, 1)] if s < NT else [])
                while gens:
                    for g_ in list(gens):
                        try:
                            next(g_)
                        except StopIteration:
                            gens.remove(g_)

    def phase_dnout(self, l, need_ctx):
        S = self.S
        tiles = self.out_tiles(l, need_ctx)
        with S.scope() as P:
            gn = P.sb("dng", [128, 128], F32)
            self.ld(gn, gn[:], self.dn_norm, self.dn_norm[l].partition_broadcast(128))
            bufs = [dict(of=P.sb("of", [128, 512], F32), ob=P.sb("ob", [128, 512], F32), z=P.sb("z", [128, 512], BF16), sq=P.sb("sq", [128, 512], F32),
                         ss=P.sb("ss", [128, 4], F32), gz=P.sb("gz", [128, 512], F32), y=P.sb("y", [128, 512], BF16), yT=P.sb("yT", [128, 4, 128], BF16)) for _ in range(10)]
            def tile_gen(it):
                i, t = it
                b = bufs[i % 10]
                of, ob, z, sq, ss, gz, y, yT = b["of"], b["ob"], b["z"], b["sq"], b["ss"], b["gz"], b["y"], b["yT"]
                rows = slice(t * 128, (t + 1) * 128)
                self.ld(of, of[:], self.o_d[0], self.o_d[0][rows, :])
                self.ld(ob, ob[:], self.o_d[1], self.o_d[1][rows, :])
                self.ld(z, z[:], self.z_d, self.z_d[rows, :])
                yield
                self.tt("pool", of, of[:], of, of[:], ob, ob[:], ALU.add)
                self.tt("pool", gz, gz[:].rearrange("p (h k) -> p h k", h=4), z, z[:].rearrange("p (h k) -> p h k", h=4), gn, gn[:].unsqueeze(1).to_broadcast([128, 4, 128]), ALU.mult)
                yield
                self.act(sq, sq[:], of, of[:], AF.Square)
                yield
                self.S.op("dve", lambda e: e.tensor_reduce(ss[:], sq[:].rearrange("p (h k) -> p h k", h=4), mybir.AxisListType.X, ALU.add), outs=[ss], ins=[sq])
                yield
                self.act(ss, ss[:], ss, ss[:], AF.Sqrt, bias=EPS, scale=1.0 / 128)
                yield
                self.recip(ss, ss[:], ss, ss[:])
                self.tt("dve", sq, sq[:].rearrange("p (h k) -> p h k", h=4), of, of[:].rearrange("p (h k) -> p h k", h=4), ss, ss[:].unsqueeze(2).to_broadcast([128, 4, 128]), ALU.mult)
                yield
                self.tt("pool", y, y[:], sq, sq[:], gz, gz[:], ALU.mult)
                yield
                pb = self.npb()
                for c in range(4):
                    self.tr(pb, pb[:, c * 128:(c + 1) * 128], y, y[:, c * 128:(c + 1) * 128], self.identb[:])
                yield
                self.cp("act", yT, yT[:], pb, pb[:, 0:512].rearrange("p (c k) -> p c k", c=4))
                self.ld(self.yT_d, self.yT_d[0, :, :, rows].rearrange("c p k -> p c k"), yT, yT[:])

            self.run_pipe(list(enumerate(tiles)), tile_gen)

    def phase_mla(self, l, need_ctx):
        S = self.S
        SCALE = 96.0 ** -0.5
        with S.scope() as P:
            lat = [P.sb("lat%d" % i, [128, TALL], BF16) for i in range(5)]
            for i in range(5):
                self.ld(lat[i], lat[i][:], self.lat_d, self.lat_d[i])
            KT = [P.sb("KT%d" % h, [96, TALL], BF16) for h in range(8)]
            VP = P.sb("VP", [128, NT, 512], BF16)
            wq = P.sb("wq", [128, 3, 768], BF16)
            wqs = P.sb("wqs", [128, 3, 768], BF16)
            wkv = P.sb("wkv", [128, 2, 1024], BF16)
            qv = self.mla_w_qup[l].rearrange("(rc p) n -> p rc n", p=128)
            self.S.dma("pool", wq, wq[:], self.mla_w_qup, qv)
            self.S.dma("pool", wqs, wqs[:], self.mla_w_qup, qv)
            q4 = self.mla_w_qup[l].rearrange("(rc p) (h d) -> p rc h d", p=128, d=96)
            w4 = wqs[:].rearrange("p rc (h d) -> p rc h d", d=96)
            for (d0, s0) in ((0, 8), (8, 0), (16, 24), (24, 16)):
                for rc in range(3):
                    self.S.dma("pool", wqs, w4[:, rc, :, 64 + d0:64 + d0 + 8], self.mla_w_qup, q4[:, rc, :, 64 + s0:64 + s0 + 8])
            self.S.dma("pool", wkv, wkv[:], self.mla_w_kvup, self.mla_w_kvup[l].rearrange("(rc p) n -> p rc n", p=128))
            for h in range(8):
                self.ld(KT[h], KT[h][64:96, :], self.kpe_d, self.kpe_d[:, :])
            for (t0, n) in self.tok_chunks():
                for h in range(8):
                    ps = self.nps()
                    for rc in range(2):
                        self.mm(ps, ps[0:64, 0:n], wkv, wkv[:, rc, h * 128:h * 128 + 64], lat[3 + rc], lat[3 + rc][:, t0:t0 + n], rc == 0, rc == 1)
                    self.cp("act" if h % 2 else "dve", KT[h], KT[h][0:64, t0:t0 + n], ps, ps[0:64, 0:n])
            wv4 = wkv[:].rearrange("p rc (h d) -> p rc h d", d=128)
            for t in range(NT):
                ps = self.nps()
                for rc in range(2):
                    self.mm(ps, ps[:, 0:512].rearrange("p (h d) -> p h d", d=64), lat[3 + rc], lat[3 + rc][:, t * 128:(t + 1) * 128], wkv, wv4[:, rc, :, 64:128], rc == 0, rc == 1)
                self.cp("act" if t % 2 else "dve", VP, VP[:, t, :], ps, ps[:, 0:512])
            self.S.barrier()
            H = [Obj("psh%d" % i, self.PS[i // 2].h[:, (i % 2) * 512:(i % 2 + 1) * 512], True) for i in range(6)]
            psOo, psOd = H[0], H[1]
            QP = [Obj("psq%d" % i, self.PB[i].h[:].bitcast(F32), True) for i in range(2)]
            rot = H[2:6]
            ri = [0]

            def nh():
                p = rot[ri[0] % 4]
                ri[0] += 1
                return p
            css = [P.sb("qcs%d" % i, [96, 2, 512], F32) for i in range(2)]
            QT = [P.sb("QT%d" % i, [96, 512], BF16) for i in range(2)]
            t1s = [P.sb("qt1_%d" % i, [96, 512], F32) for i in range(2)]
            t2s = [P.sb("qt2_%d" % i, [96, 512], F32) for i in range(2)]
            PT = [P.sb("PT%d" % i, [128, 512], BF16) for i in range(3)]
            rec = P.sb("rec", [128, 512], F32)
            yb = [P.sb("yb%d" % i, [128, 512], BF16) for i in range(2)]
            nq = (LOC if l == DEPTH - 1 else SEQ) // 512
            chunks = ([(0, CTX, list(range(NCT)))] if need_ctx else []) + [(CTX + i * 512, 512, list(range(NT))) for i in range(nq)]
            items = [(ci, t0, n, ktiles, c, j) for ci, (t0, n, ktiles) in enumerate(chunks) for c in range(4) for j in range(2)]

            def build_q(k):
                ci, t0, n, ktiles, c, j = items[k]
                cs = css[ci % 2]
                if c == 0 and j == 0:
                    self.ld(cs, cs[64:96, :, 0:n], self.c_rope, self.c_rope[:, :, t0:t0 + n].rearrange("c p t -> p c t"))
                h = 2 * c + j
                q, t1, t2 = QT[k % 2], t1s[k % 2], t2s[k % 2]
                pA, pB = QP
                for rc in range(3):
                    self.mm(pA, pA[0:96, 0:n], wq, wq[:, rc, h * 96:(h + 1) * 96], lat[rc], lat[rc][:, t0:t0 + n], rc == 0, rc == 2)
                for rc in range(3):
                    self.mm(pB, pB[0:96, 0:n], wqs, wqs[:, rc, h * 96:(h + 1) * 96], lat[rc], lat[rc][:, t0:t0 + n], rc == 0, rc == 2)
                self.cp("act", q, q[0:64, 0:n], pA, pA[0:64, 0:n])
                self.tt("dve", t1, t1[64:96, 0:n], pA, pA[64:96, 0:n], cs, cs[64:96, 0, 0:n], ALU.mult)
                self.tt("dve", t2, t2[64:96, 0:n], pB, pB[64:96, 0:n], cs, cs[64:96, 1, 0:n], ALU.mult)
                self.tt("pool", q, q[64:96, 0:n], t1, t1[64:96, 0:n], t2, t2[64:96, 0:n], ALU.add)

            build_q(0)
            for k, (ci, t0, n, ktiles, c, j) in enumerate(items):
                h = 2 * c + j
                q = QT[k % 2]
                y = yb[c % 2]
                sts = {}

                def score(i):
                    kt = ktiles[i]
                    p = nh()
                    self.mm(p, p[:, 0:n], KT[h], KT[h][:, kt * 128:(kt + 1) * 128], q, q[:, 0:n])
                    sts[i] = p
                AHEAD = 3
                for i in range(min(AHEAD, len(ktiles))):
                    score(i)
                if k + 1 < len(items):
                    build_q(k + 1)
                for i, kt in enumerate(ktiles):
                    if i + AHEAD < len(ktiles):
                        score(i + AHEAD)
                    p = sts.pop(i)
                    pt = PT[i % 3]
                    self.act(pt, pt[:, 0:n], p, p[:, 0:n], AF.Exp, scale=SCALE)
                    last = i == len(ktiles) - 1
                    self.mm(psOo, psOo[:, 0:n], VP, VP[:, kt, c * 128:(c + 1) * 128], pt, pt[:, 0:n], i == 0, last, inc=last)
                    self.mm(psOd, psOd[:, 0:n], self.ones_b, self.ones_b[:], pt, pt[:, 0:n], i == 0, last, inc=True)
                r0 = j * 64
                self.recip(rec, rec[r0:r0 + 64, 0:n], psOd, psOd[r0:r0 + 64, 0:n])
                self.tt("dve", y, y[r0:r0 + 64, 0:n], psOo, psOo[r0:r0 + 64, 0:n], rec, rec[r0:r0 + 64, 0:n], ALU.mult)
                if j == 1:
                    self.ld(self.yT_d, self.yT_d[1, c, :, t0:t0 + n], y, y[:, 0:n])

    def phase_fft(self, l, need_ctx):
        S = self.S
        with S.scope() as P:
            dc = P.sb("dftc", [128, 256], BF16)
            self.ld(dc, dc[:], self.c_dftc, self.c_dftc[:])
            AB = P.sb("AB", [128, NT, 1024], BF16)
            ut = [P.sb("ut%d" % i, [128, 4, 128], BF16) for i in range(2)]
            tiles = list(range(0 if need_ctx else NCT, NT))
            for i, t in enumerate(tiles):
                u = ut[i % 2]
                self.ld(u, u[:], self.uT_d, self.uT_d[:, :, t * 128:(t + 1) * 128].rearrange("g p k -> p g k"))
                ps = self.nps()
                for g in range(4):
                    self.mm(ps, ps[:, g * 256:(g + 1) * 256], u, u[:, g, :], dc, dc[:])
                self.cp("act" if i % 2 else "dve", AB, AB[:, t, :], ps, ps[:])
            self.S.barrier()
            H = [Obj("fph%d" % i, self.PS[i // 2].h[:, (i % 2) * 512:(i % 2 + 1) * 512], True) for i in range(6)]
            DB = [P.sb("dft%d" % i, [128, 32, 512], BF16) for i in range(3)]
            yc = [P.sb("yc%d" % i, [128, 512], BF16) for i in range(2)]
            nkc = (LOC if l == DEPTH - 1 else SEQ) // 512
            jobs = ([("c", 0, 0, NCT, 0, 256)] if need_ctx else []) + [("x", kc, NCT, SEQ // 128, CTX, 512) for kc in range(nkc)]
            passes = [(ji, pi) for ji in range(len(jobs)) for pi in range(2)]

            def load(p):
                ji, pi = passes[p]
                nm, kc, tb, ntt, tok0, w = jobs[ji]
                Mb = DB[p % 3]
                src = self.c_dft[("C" if pi == 0 else "S", nm)]
                self.ld(Mb, Mb[:, 0:ntt, 0:w], src, src[kc])

            oi = 0
            load(0)
            accs = None
            for p, (ji, pi) in enumerate(passes):
                nm, kc, tb, ntt, tok0, w = jobs[ji]
                if p + 1 < len(passes):
                    load(p + 1)
                if pi == 0:
                    accs = [H[(4 * ji + g) % 6] for g in range(4)]
                Mb = DB[p % 3]
                for tt in range(ntt):
                    for g in range(4):
                        o0 = g * 256 + (0 if pi == 0 else 128)
                        self.mm(accs[g], accs[g][:, 0:w], AB, AB[:, tb + tt, o0:o0 + 128], Mb, Mb[:, tt, 0:w], pi == 0 and tt == 0, pi == 1 and tt == ntt - 1)
                if pi == 1:
                    for g in range(4):
                        y = yc[oi % 2]
                        oi += 1
                        self.cp("act" if oi % 2 else "dve", y, y[:, 0:w], accs[g], accs[g][:, 0:w])
                        self.ld(self.yT_d, self.yT_d[2, g, :, tok0 + kc * w:tok0 + (kc + 1) * w], y, y[:, 0:w])

    def phase_merge(self, l, need_ctx):
        S = self.S
        with S.scope() as P:
            wb = P.sb("wb", [128, 12, D], BF16)
            self.S.dma("pool", wb, wb[:], self.w_branch, self.w_branch[l].rearrange("n (cc p) d -> p (n cc) d", p=128))
            wo = P.sb("wo", [128, 8, D], BF16)
            self.S.dma("pool", wo, wo[:], self.w_out, self.w_out[l].rearrange("(kc p) d -> p kc d", p=128))
            GT = {r: self.load_bc(P, l, r, 2) for r in ([0, 1] if need_ctx else [0])}
            yT = [P.sb("myT%d" % i, [128, 12, 512], BF16) for i in range(2)]
            gT = [P.sb("mgT%d" % i, [128, 24, 512], BF16) for i in range(2)]
            mT = [P.sb("mmT%d" % i, [128, 8, 512], BF16) for i in range(2)]
            tas = [[[P.sb("mta%d_%d_%d" % (c_, k, i), [128, 512], F32) for i in range(3)] for k in range(2)] for c_ in range(2)]
            xt = [P.sb("mxt%d" % i, [128, D], F32) for i in range(4)]
            tx = [P.sb("mtx%d" % i, [128, D], F32) for i in range(4)]
            chunks = [c for c in self.tok_chunks() if need_ctx or c[0] >= CTX]
            if l == DEPTH - 1:
                chunks = [c for c in chunks if CTX <= c[0] < CTX + LOC]
            def chunk_gen(it):
                ci, (t0, n) = it
                r = 1 if t0 < CTX else 0
                y, g, m = yT[ci % 2], gT[ci % 2], mT[ci % 2]
                self.ld(y, y[:, :, 0:n], self.yT_d, self.yT_d[:, :, :, t0:t0 + n].rearrange("n c p k -> p (n c) k"))
                self.ld(g, g[:, :, 0:n], self.gT_d, self.gT_d[:, :, t0:t0 + n].rearrange("b p k -> p b k"))
                yield
                for db in range(8):
                    ta = tas[ci % 2][db % 2]
                    for nb in range(3):
                        ps = self.nps()
                        for cc in range(4):
                            self.mm(ps, ps[:, 0:n], wb, wb[:, nb * 4 + cc, db * 128:(db + 1) * 128], y, y[:, nb * 4 + cc, 0:n], cc == 0, cc == 3)
                        self.tt("dve", ta[nb], ta[nb][:, 0:n], ps, ps[:, 0:n], g, g[:, nb * 8 + db, 0:n], ALU.mult)
                    self.tt("pool", ta[0], ta[0][:, 0:n], ta[0], ta[0][:, 0:n], ta[1], ta[1][:, 0:n], ALU.add)
                    self.tt("pool", m, m[:, db, 0:n], ta[0], ta[0][:, 0:n], ta[2], ta[2][:, 0:n], ALU.add)
                    yield
                for tl in range(n // 128):
                    t = t0 // 128 + tl
                    x, txx = xt[(ci % 2) * 2 + tl % 2], tx[(ci % 2) * 2 + tl % 2]
                    so, sap = self.xsrc(l, t)
                    self.ld(x, x[:], so, sap)
                    ps = self.nps()
                    for nh in range(2):
                        for db in range(8):
                            self.mm(ps, ps[:, nh * 512:(nh + 1) * 512], m, m[:, db, tl * 128:(tl + 1) * 128], wo, wo[:, db, nh * 512:(nh + 1) * 512], db == 0, db == 7)
                    self.tt("dve", txx, txx[:], ps, ps[:], GT[r], GT[r][:], ALU.mult)
                    self.tt("pool", txx, txx[:], txx, txx[:], x, x[:], ALU.add)
                    self.ld(self.xres, self.xres[t * 128:(t + 1) * 128, :], txx, txx[:])
                    yield

            self.run_pipe(list(enumerate(chunks)), chunk_gen, max_active=2)

    def phase_ffn(self, l):
        S = self.S
        moe = l % 2 == 1
        ei = l // 2
        last = l == DEPTH - 1
        NE, FF = (NEXP, EFF) if moe else (1, DFF)
        nfb = FF // 128
        nun = (nfb + 6) // 7
        units = []
        f_ = 0
        for i in range(nun):
            k_ = (nfb - f_ + (nun - i) - 1) // (nun - i)
            units.append((f_, k_))
            f_ += k_
        UMAX = max(u[1] for u in units)
        tiles_all = list(range(NCT, NCT + LOCT)) if last else list(range(NT))
        CH = 8 if moe else 12
        for c0 in range(0, len(tiles_all), CH):
            tiles = tiles_all[c0:c0 + CH]
            ntl = len(tiles)
            ntok = ntl * 128
            with S.scope() as P:
                hT = P.sb("fhT", [128, 8, CH * 128], BF16)
                acc = P.sb("facc", [128, CH, D], F32)
                comb = P.sb("comb", [128, CH, NEXP], F32)
                conds = sorted(set(1 if t < NCT else 0 for t in tiles))
                GT = {r: self.load_bc(P, l, r, 5) for r in conds}
                if moe:
                    wr = P.sb("wr", [128, 8, NEXP], F32)
                    self.ld(wr, wr[:], self.moe_router, self.moe_router[ei].rearrange("(kc p) e -> p kc e", p=128))
                    fT = P.sb("fT", [128, 8, 128], F32)
                    rt = P.sb("rt", [128, 8 * NEXP], F32)

                    def router(i, t, f):
                        ps = self.nps()
                        for kc in range(8):
                            self.S.op("pe", lambda e: e.transpose(ps[:, kc * 128:(kc + 1) * 128], f[:, kc * 128:(kc + 1) * 128], self.mk(M_ID)), outs=[ps], ins=[f, self.masks])
                        self.cp("act", fT, fT[:], ps, ps[:].rearrange("p (k t) -> p k t", k=8))
                        pl = self.nps()
                        for kc in range(8):
                            self.mm(pl, pl[:, 0:NEXP], fT, fT[:, kc, :], wr, wr[:, kc, :], kc == 0, kc == 7)
                        lg, m1, eq, l2, m2, sel, ex, sw = (rt[:, k * 8:(k + 1) * 8] for k in range(8))
                        self.cp("act", rt, lg, pl, pl[:, 0:NEXP])
                        self.S.op("dve", lambda e: e.tensor_reduce(m1[:, 0:1], lg, mybir.AxisListType.X, ALU.max), outs=[rt], ins=[rt])
                        self.ts("dve", rt, eq, rt, lg, m1[:, 0:1], None, ALU.is_equal)
                        self.stt(rt, l2, rt, eq, -1e30, rt, lg, ALU.mult, ALU.add)
                        self.S.op("dve", lambda e: e.tensor_reduce(m2[:, 0:1], l2, mybir.AxisListType.X, ALU.max), outs=[rt], ins=[rt])
                        self.ts("dve", rt, sel, rt, lg, m2[:, 0:1], None, ALU.is_ge)
                        self.ts("dve", rt, m1[:, 1:2], rt, m1[:, 0:1], -1.0, None, ALU.mult)
                        self.act(rt, ex, rt, lg, AF.Exp, bias=m1[:, 1:2], scale=1.0)
                        self.tt("dve", rt, ex, rt, ex, rt, sel, ALU.mult)
                        self.S.op("dve", lambda e: e.tensor_reduce(sw[:, 0:1], ex, mybir.AxisListType.X, ALU.add), outs=[rt], ins=[rt])
                        self.recip(rt, sw[:, 1:2], rt, sw[:, 0:1])
                        self.ts("dve", comb, comb[:, i, :], rt, ex, sw[:, 1:2], None, ALU.mult, ins=[rt])
                else:
                    router = None
                self.phase_norm(l, "ffn", tiles, hT, 0, router=router)
                tchunks = [(o, min(512, ntok - o)) for o in range(0, ntok, 512)]
                with S.scope() as Q:
                    actTs = [Q.sb("actT%d" % i, [128, UMAX, CH * 128], BF16) for i in range(2)]
                    wg = [Q.sb("wg%d" % i, [128, 8, 512], BF16) for i in range(2)]
                    wu = [Q.sb("wu%d" % i, [128, 8, 512], BF16) for i in range(2)]
                    wds = [Q.sb("wd%d" % i, [128, UMAX, D], BF16) for i in range(2)]
                    ui = 0
                    sg = [Q.sb("sg%d" % i, [128, 512], F32) for i in range(2)]
                    gi = 0
                    si = 0
                    first = True
                    for e in range(NE):
                        if moe:
                            Wg, Wu, Wd = self.moe_w_gate, self.moe_w_up, self.moe_w_down
                            wgv = Wg[ei, e].rearrange("(kc p) f -> p kc f", p=128)
                            wuv = Wu[ei, e].rearrange("(kc p) f -> p kc f", p=128)
                            wdv = Wd[ei, e]
                        else:
                            Wg, Wu, Wd = self.ffn_w_gate, self.ffn_w_up, self.ffn_w_down
                            wgv = Wg[ei].rearrange("(kc p) f -> p kc f", p=128)
                            wuv = Wu[ei].rearrange("(kc p) f -> p kc f", p=128)
                            wdv = Wd[ei]
                        for (fb0, nfbu) in units:
                            actT, wd = actTs[ui % 2], wds[ui % 2]
                            ui += 1
                            self.S.dma("pool", wd, wd[:, 0:nfbu, :], Wd, wdv[fb0 * 128:(fb0 + nfbu) * 128, :].rearrange("(fb p) d -> p fb d", p=128))
                            for g0 in range(0, nfbu, 4):
                                nb = min(4, nfbu - g0)
                                g_, u_ = wg[gi % 2], wu[gi % 2]
                                gi += 1
                                f0 = (fb0 + g0) * 128
                                self.S.dma("pool", g_, g_[:, :, 0:nb * 128], Wg, wgv[:, :, f0:f0 + nb * 128])
                                self.S.dma("pool", u_, u_[:, :, 0:nb * 128], Wu, wuv[:, :, f0:f0 + nb * 128])
                                for b in range(nb):
                                    for (o, n) in tchunks:
                                        psg = self.nps()
                                        for kc in range(8):
                                            self.mm(psg, psg[:, 0:n], g_, g_[:, kc, b * 128:(b + 1) * 128], hT, hT[:, kc, o:o + n], kc == 0, kc == 7)
                                        for kc in range(8):
                                            self.mm(psg, psg[:, 512:512 + n], u_, u_[:, kc, b * 128:(b + 1) * 128], hT, hT[:, kc, o:o + n], kc == 0, kc == 7)
                                        s_ = sg[si % 2]
                                        si += 1
                                        self.act(s_, s_[:, 0:n], psg, psg[:, 0:n], AF.Silu)
                                        self.tt("dve", actT, actT[:, g0 + b, o:o + n], s_, s_[:, 0:n], psg, psg[:, 512:512 + n], ALU.mult)
                            for i in range(ntl):
                                ps = self.nps()
                                for nh in range(2):
                                    for fbi in range(nfbu):
                                        self.mm(ps, ps[:, nh * 512:(nh + 1) * 512], actT, actT[:, fbi, i * 128:(i + 1) * 128], wd, wd[:, fbi, nh * 512:(nh + 1) * 512], fbi == 0, fbi == nfbu - 1)
                                if moe:
                                    if first:
                                        self.ts("dve", acc, acc[:, i, :], ps, ps[:], comb[:, i, e:e + 1], None, ALU.mult, ins=[comb])
                                    else:
                                        self.stt(acc, acc[:, i, :], ps, ps[:], comb[:, i, e:e + 1], acc, acc[:, i, :], ALU.mult, ALU.add, ins=[comb])
                                else:
                                    if first:
                                        self.cp("act", acc, acc[:, i, :], ps, ps[:])
                                    else:
                                        self.tt("dve", acc, acc[:, i, :], acc, acc[:, i, :], ps, ps[:], ALU.add)
                            first = False
                with S.scope() as Q:
                    xt = [Q.sb("rxt%d" % i, [128, D], F32) for i in range(2)]
                    tx = [Q.sb("rtx%d" % i, [128, D], F32) for i in range(2)]
                    if last:
                        fg = Q.sb("fgain", [128, D], F32)
                        self.ld(fg, fg[:], self.final_norm, self.final_norm[0].partition_broadcast(128))
                        sq = Q.sb("fsq", [128, D], F32)
                        ss = [Q.sb("fss%d" % i, [128, 1], F32) for i in range(2)]
                    for i, t in enumerate(tiles):
                        r = 1 if t < NCT else 0
                        x, y = xt[i % 2], tx[i % 2]
                        self.ld(x, x[:], self.xres, self.xres[t * 128:(t + 1) * 128, :])
                        self.tt("pool", y, y[:], acc, acc[:, i, :], GT[r], GT[r][:], ALU.mult)
                        self.tt("dve", y, y[:], y, y[:], x, x[:], ALU.add)
                        if not last:
                            self.ld(self.xres, self.xres[t * 128:(t + 1) * 128, :], y, y[:])
                        else:
                            s_ = ss[i % 2]
                            self.act(sq, sq[:], y, y[:], AF.Square)
                            self.S.op("dve", lambda e: e.tensor_reduce(s_[:], sq[:], mybir.AxisListType.X, ALU.add), outs=[s_], ins=[sq])
                            self.act(s_, s_[:], s_, s_[:], AF.Sqrt, bias=EPS, scale=1.0 / D)
                            self.recip(s_, s_[:], s_, s_[:])
                            self.stt(x, x[:], y, y[:], s_[:, 0:1], fg, fg[:], ALU.mult, ALU.mult, ins=[s_])
                            self.ld(self.y_out, self.y_out[(t - NCT) * 128:(t - NCT + 1) * 128, :], x, x[:])

_CONSTS = {}


def make_in_maps(inputs, n_cores=8):
    f = lambda a: np.ascontiguousarray(np.asarray(a, dtype=np.float32))
    shared = {
        "w_mod": f(inputs["w_mod"]), "b_mod": f(inputs["b_mod"]), "norm_mix": f(inputs["norm_mix"]), "norm_ffn": f(inputs["norm_ffn"]),
        "dn_norm": f(inputs["dn_norm"]),
        "lat_gain": f(np.concatenate([np.asarray(inputs["mla_q_norm"]).reshape(DEPTH, 3, 128), np.asarray(inputs["mla_kv_norm"]).reshape(DEPTH, 2, 128)], 1).transpose(0, 2, 1)),
        "mla_w_qup": f(inputs["mla_w_qup"]), "mla_w_kvup": f(inputs["mla_w_kvup"]), "w_branch": f(inputs["w_branch"]), "w_out": f(inputs["w_out"]),
        "ffn_w_gate": f(inputs["ffn_w_gate"]), "ffn_w_up": f(inputs["ffn_w_up"]), "ffn_w_down": f(inputs["ffn_w_down"]),
        "moe_router": f(inputs["moe_router"]),
        "moe_w_gate": f(inputs["moe_w_gate"]), "moe_w_up": f(inputs["moe_w_up"]), "moe_w_down": f(inputs["moe_w_down"]),
        "final_norm": f(np.asarray(inputs["final_norm"]).reshape(1, D)),
    }
    w_in = np.asarray(inputs["w_in"], np.float32)
    conv = np.asarray(inputs["dn_conv"], np.float32)
    alog = np.asarray(inputs["dn_a_log"], np.float32)
    dtb = np.asarray(inputs["dn_dt_bias"], np.float32)
    per = {}
    for flip in (False, True):
        d = {}
        if flip:
            perm = np.arange(INW)
            perm[O_A:O_A + 8] = np.r_[O_A + 4:O_A + 8, O_A:O_A + 4]
            perm[O_B:O_B + 8] = np.r_[O_B + 4:O_B + 8, O_B:O_B + 4]
            d["w_in"] = np.ascontiguousarray(w_in[:, :, perm])
            d["dn_convT"] = f(np.transpose(conv[:, ::-1, :], (0, 2, 1)))
            d["dn_a_log"] = f(alog[:, ::-1, :].reshape(DEPTH, 8))
            d["dn_dt_bias"] = f(dtb[:, ::-1, :].reshape(DEPTH, 8))
        else:
            d["w_in"] = f(w_in)
            d["dn_convT"] = f(np.transpose(conv, (0, 2, 1)))
            d["dn_a_log"] = f(alog.reshape(DEPTH, 8))
            d["dn_dt_bias"] = f(dtb.reshape(DEPTH, 8))
        if flip not in _CONSTS:
            _CONSTS[flip] = host_consts(flip)
        d.update(_CONSTS[flip])
        per[flip] = d
    maps = []
    x = np.asarray(inputs["x"], np.float32)
    c = np.asarray(inputs["c"], np.float32)
    ctx = np.asarray(inputs["ctx"], np.float32)
    cc = np.asarray(inputs["c_ctx"], np.float32)
    for core in range(n_cores):
        b = core % 4
        flip = core >= 4
        m = dict(shared)
        m.update(per[flip])
        m["x_in"] = np.ascontiguousarray(x[b][::-1] if flip else x[b])
        m["ctx_in"] = np.ascontiguousarray(ctx[b][::-1] if flip else ctx[b])
        c2 = np.stack([c[b], cc], 0)
        m["cT"] = np.ascontiguousarray(c2.reshape(2, 8, 128).transpose(2, 1, 0).reshape(128, 16))
        maps.append(m)
    return maps


_NC = None


def kernel(**inputs):
    global _NC
    if _NC is None:
        _NC = Builder().build()
    maps = make_in_maps(inputs, 8)
    res = run_bass_kernel_spmd(_NC, maps, core_ids=list(range(8)))
    out = np.empty((4, SEQ, D), np.float32)
    for core in range(8):
        y = np.asarray(res.results[core]["y_out"], np.float32)
        if core < 4:
            out[core, :LOC] = y
        else:
            out[core - 4, LOC:] = y[::-1]
    return out
```
